# Optimizing a Trainium2 kernel written in Bass

```python
import math
import jax
import jax.numpy as jnp
from jax import lax
import numpy as np

D_MODEL = 1024
BATCH = 2
SEQ = 16384
DEPTH = 2

GRID_W = 64
CTX_LEN = 256
N_AB_LAYERS = (DEPTH + 1) // 2
N_SSD_LAYERS = DEPTH // 2

HGRN_HEADS = 4
HGRN_DK = 128
HGRN_DV = 128
HGRN_WIDTH = HGRN_HEADS * HGRN_DK
GLA_CHUNK = 32

NA_HEADS = 8
NA_DH = 64
NA_WIDTH = NA_HEADS * NA_DH
NA_KR = 8
NA_KC = 16
NA_QCB = 16
NA_NCB = GRID_W // NA_QCB
NA_BAND_W = NA_QCB + NA_KC

AB_IN = 5 * HGRN_WIDTH + 3 * NA_WIDTH
AB_MIX = HGRN_HEADS * HGRN_DV + NA_WIDTH

SSD_INNER = 2 * D_MODEL
SSD_HEADDIM = 64
SSD_HEADS = SSD_INNER // SSD_HEADDIM
SSD_GROUPS = 8
SSD_HPG = SSD_HEADS // SSD_GROUPS
SSD_STATE = 128
SSD_CONV = 5
SSD_CHUNK = 64
SSD_CONV_DIM = SSD_INNER + 2 * SSD_GROUPS * SSD_STATE
SSD_IN = SSD_INNER + SSD_CONV_DIM + 2 * SSD_HEADS

N_EXPERTS = 16
EXPERT_FF = 1024
CAPACITY_FACTOR = 2

RMS_EPS = 1e-6
NEG_INF = -1e30
F32 = jnp.float32

kernel_name = 'hybrid_hgrn2_natten_ssd_ecmoe_dit'


def rmsnorm(x, g):
    xf = x.astype(F32)
    y = xf * lax.rsqrt(jnp.mean(xf * xf, axis=-1, keepdims=True) + RMS_EPS)
    return (y * g.astype(F32)).astype(x.dtype)


def modulate(h, shift, scale):
    return h * (1 + scale) + shift


def hgrn_heads(a):
    b, t, _ = a.shape
    return a.reshape(b, t, HGRN_HEADS, -1).transpose(0, 2, 1, 3)


def hgrn_gates(z, lb):
    f = lb + (1 - lb) * jax.nn.sigmoid(z)
    return hgrn_heads(jnp.log(f)), hgrn_heads(1 - f)


def gla_chunked(q, k, v, logf, s0):
    b, h, t, _ = q.shape
    n = t // GLA_CHUNK

    def chunks(a):
        return jnp.moveaxis(a.reshape(b, h, n, GLA_CHUNK, a.shape[-1]), 2, 0)

    qc, kc, vc, gc = chunks(q), chunks(k), chunks(v), chunks(logf)
    cum = jnp.cumsum(gc, axis=-2)
    cum_last = cum[..., -1:, :]
    q_in = qc * jnp.exp(cum)
    k_in = kc * jnp.exp(-cum)
    k_out = kc * jnp.exp(cum_last - cum)
    tril = np.tril(np.ones((GLA_CHUNK, GLA_CHUNK), bool))
    attn = jnp.where(tril, jnp.einsum('nbhld,nbhsd->nbhls', q_in, k_in), 0.0)
    intra = jnp.einsum('nbhls,nbhsv->nbhlv', attn, vc)

    def step(s, xs):
        q_i, ko_i, v_i, cl_i, intra_i = xs
        o = intra_i + jnp.einsum('bhld,bhdv->bhlv', q_i, s)
        s = jnp.exp(cl_i)[..., 0, :, None] * s + jnp.einsum('bhld,bhlv->bhdv', ko_i, v_i)
        return s, o

    s_fin, o = lax.scan(step, s0, (q_in, k_out, vc, cum_last, intra))
    return jnp.moveaxis(o, 0, 2).reshape(b, h, t, v.shape[-1]), s_fin


def hgrn_out_gate(o, g, gain):
    b, h, t, dv = o.shape
    o = o.transpose(0, 2, 1, 3)
    o = o * lax.rsqrt(jnp.mean(o * o, axis=-1, keepdims=True) + RMS_EPS) * gain.astype(F32)
    return o.reshape(b, t, h * dv) * jax.nn.silu(g)


def hgrn2_mixer(p_lat, p_ctx, lb, onorm_g, need_ctx):
    def split(p):
        q, zf, zb, i, g = jnp.split(p.astype(F32), 5, axis=-1)
        return hgrn_heads(jax.nn.silu(q)), zf, zb, hgrn_heads(i), g

    ql, zfl, zbl, il, gl = split(p_lat)
    qc, zfc, zbc, ic, gc = split(p_ctx)
    s0 = jnp.zeros((p_lat.shape[0], HGRN_HEADS, HGRN_DK, HGRN_DV), F32)

    def direction(zl, zc, lb_d, flip):
        lfl, kl = hgrn_gates(zl, lb_d)
        lfc, kc = hgrn_gates(zc, lb_d)
        lat = (ql, kl, il, lfl)
        ctx = (qc, kc, ic, lfc)
        if flip:
            lat = tuple(jnp.flip(a, axis=2) for a in lat)
            ctx = tuple(jnp.flip(a, axis=2) for a in ctx)
        oc, sc = gla_chunked(*ctx, s0)
        ol, _ = gla_chunked(*lat, sc)
        if flip:
            ol, oc = jnp.flip(ol, axis=2), jnp.flip(oc, axis=2)
        return ol, oc

    olf, ocf = direction(zfl, zfc, lb[0], False)
    olb, ocb = direction(zbl, zbc, lb[1], True)
    out_l = hgrn_out_gate(olf + olb, gl, onorm_g).astype(p_lat.dtype)
    out_c = hgrn_out_gate(ocf + ocb, gc, onorm_g).astype(p_ctx.dtype) if need_ctx else None
    return out_l, out_c


def na_heads(a):
    b, t, _ = a.shape
    return a.reshape(b, t, NA_HEADS, NA_DH).transpose(0, 2, 1, 3)


def na_merge(o):
    b, h, t, d = o.shape
    return o.transpose(0, 2, 1, 3).reshape(b, t, h * d)


def neighborhood_attention(q, k, v, kc, vc, rpb):
    b, h, s, dh = q.shape
    rows = s // GRID_W
    kr = min(NA_KR, rows)
    qcol = np.arange(GRID_W).reshape(NA_NCB, NA_QCB)
    win0 = np.clip(qcol - NA_KC // 2, 0, GRID_W - NA_KC)
    band0 = np.minimum(win0[:, 0], GRID_W - NA_BAND_W)
    band_cols = band0[:, None] + np.arange(NA_BAND_W)
    col_ok = (band_cols[:, None, :] >= win0[:, :, None]) & (band_cols[:, None, :] < win0[:, :, None] + NA_KC)
    dc_idx = np.clip(band_cols[:, None, :] - qcol[:, :, None] + NA_KC - 1, 0, 2 * NA_KC - 2)
    rpb_c = rpb[:, :, dc_idx].astype(F32)
    col_mask = col_ok[:, :, None, :]
    kg = k.reshape(b, h, rows, GRID_W, dh)
    vg = v.reshape(b, h, rows, GRID_W, dh)
    q_rows = jnp.moveaxis(q.reshape(b, h, rows, NA_NCB, NA_QCB, dh), 2, 0)
    n_win = kr * NA_BAND_W

    def row_block(args):
        r, qr = args
        r0 = jnp.clip(r - kr // 2, 0, rows - kr)
        k_band = lax.dynamic_slice_in_dim(kg, r0, kr, axis=2)[:, :, :, band_cols]
        v_band = lax.dynamic_slice_in_dim(vg, r0, kr, axis=2)[:, :, :, band_cols]
        s_win = jnp.einsum('bhjqd,bhkjcd->bhjqkc', qr, k_band).astype(F32)
        dr_idx = r0 + jnp.arange(kr) - r + NA_KR - 1
        bias = jnp.transpose(rpb_c[:, dr_idx], (0, 2, 3, 1, 4))
        s_win = jnp.where(col_mask, s_win + bias, NEG_INF)
        s_ctx = jnp.einsum('bhjqd,bhmd->bhjqm', qr, kc).astype(F32)
        logits = jnp.concatenate([s_win.reshape(b, h, NA_NCB, NA_QCB, n_win), s_ctx], axis=-1)
        p = jax.nn.softmax(logits, axis=-1).astype(v.dtype)
        p_win = p[..., :n_win].reshape(b, h, NA_NCB, NA_QCB, kr, NA_BAND_W)
        return (jnp.einsum('bhjqkc,bhkjcd->bhjqd', p_win, v_band)
                + jnp.einsum('bhjqm,bhmd->bhjqd', p[..., n_win:], vc))

    o = lax.map(row_block, (jnp.arange(rows), q_rows))
    return jnp.moveaxis(o, 0, 2).reshape(b, h, s, dh)


def context_attention(q, k, v):
    p = jax.nn.softmax(jnp.einsum('bhqd,bhkd->bhqk', q, k).astype(F32), axis=-1).astype(v.dtype)
    return jnp.einsum('bhqk,bhkd->bhqd', p, v)


def ab_mixer(h_lat, h_ctx, w_in, w_out, lb, onorm_g, rpb, need_ctx):
    p_lat = h_lat @ w_in
    p_ctx = h_ctx @ w_in
    hg = 5 * HGRN_WIDTH
    hl, hc = hgrn2_mixer(p_lat[..., :hg], p_ctx[..., :hg], lb, onorm_g, need_ctx)
    ql, kl, vl = (na_heads(a) for a in jnp.split(p_lat[..., hg:], 3, axis=-1))
    qc, kc, vc = (na_heads(a) for a in jnp.split(p_ctx[..., hg:], 3, axis=-1))
    scale = NA_DH ** -0.5
    nl = neighborhood_attention(ql * scale, kl, vl, kc, vc, rpb)
    y_lat = jnp.concatenate([hl, na_merge(nl)], axis=-1) @ w_out
    y_ctx = None
    if need_ctx:
        nc = context_attention(qc * scale, kc, vc)
        y_ctx = jnp.concatenate([hc, na_merge(nc)], axis=-1) @ w_out
    return y_lat, y_ctx


def dwconv_centred(u, w, bias):
    ch = u.shape[-1]
    y = lax.conv_general_dilated(u, w[:, None, :].astype(u.dtype), window_strides=(1,),
                                 padding=[(SSD_CONV // 2, SSD_CONV // 2)],
                                 dimension_numbers=('NWC', 'WIO', 'NWC'), feature_group_count=ch)
    return y + bias


def ssd_chunked(x, dt, a, bm, cm, h0):
    b, t = x.shape[:2]
    n = t // SSD_CHUNK

    def chunks(arr):
        return jnp.moveaxis(arr.reshape((b, n, SSD_CHUNK) + arr.shape[2:]), 1, 0)

    xc, dtc, bc, cc = chunks(x), chunks(dt), chunks(bm), chunks(cm)
    acum = jnp.cumsum(dtc * a, axis=2)
    causal = np.tril(np.ones((SSD_CHUNK, SSD_CHUNK), bool))[:, :, None, None]
    seg = acum[:, :, :, None] - acum[:, :, None, :]
    decay = jnp.exp(jnp.where(causal, seg, -jnp.inf))
    cb = jnp.einsum('nblgk,nbsgk->nblsg', cc, bc)
    y_diag = jnp.einsum('nblsgh,nbsghp->nblghp', cb[..., None] * decay * dtc[:, :, None], xc)
    w_end = jnp.exp(acum[:, :, -1:] - acum) * dtc

    def step(hs, xs):
        y_d, x_i, b_i, c_i, w_i, a_i = xs
        y = y_d + jnp.einsum('blgk,bghpk->blghp', c_i, hs) * jnp.exp(a_i)[..., None]
        hs = (jnp.exp(a_i[:, -1])[..., None, None] * hs
              + jnp.einsum('blgk,blghp->bghpk', b_i, x_i * w_i[..., None]))
        return hs, y

    h_fin, y = lax.scan(step, h0, (y_diag, xc, bc, cc, w_end, acum))
    return jnp.moveaxis(y, 0, 1).reshape(x.shape), h_fin


def ssd_mixer(h_lat, h_ctx, w_in, conv_w, conv_b, a_log, dt_bias, d_skip, norm_g, w_out, need_ctx):
    gn = SSD_GROUPS * SSD_STATE

    def prep(h):
        bsz, t, _ = h.shape
        p = h @ w_in
        z = p[..., :SSD_INNER]
        xbc = jax.nn.silu(dwconv_centred(p[..., SSD_INNER:SSD_INNER + SSD_CONV_DIM], conv_w, conv_b)).astype(F32)
        xs = xbc[..., :SSD_INNER].reshape(bsz, t, SSD_GROUPS, SSD_HPG, SSD_HEADDIM)
        bm = xbc[..., SSD_INNER:SSD_INNER + gn].reshape(bsz, t, SSD_GROUPS, SSD_STATE)
        cm = xbc[..., SSD_INNER + gn:].reshape(bsz, t, SSD_GROUPS, SSD_STATE)
        dt_raw = p[..., SSD_INNER + SSD_CONV_DIM:].astype(F32).reshape(bsz, t, 2, SSD_GROUPS, SSD_HPG)
        dt = jax.nn.softplus(dt_raw + dt_bias.astype(F32).reshape(2, SSD_GROUPS, SSD_HPG))
        return z, xs, bm, cm, dt

    zl, xl, bl, cl, dtl = prep(h_lat)
    zc, xc, bc, cc, dtc = prep(h_ctx)
    a = -jnp.exp(a_log.astype(F32)).reshape(2, SSD_GROUPS, SSD_HPG)
    h0 = jnp.zeros((h_lat.shape[0], SSD_GROUPS, SSD_HPG, SSD_HEADDIM, SSD_STATE), F32)

    def direction(d, flip):
        lat = (xl, dtl[:, :, d], bl, cl)
        ctx = (xc, dtc[:, :, d], bc, cc)
        if flip:
            lat = tuple(jnp.flip(arr, axis=1) for arr in lat)
            ctx = tuple(jnp.flip(arr, axis=1) for arr in ctx)
        yc, hc = ssd_chunked(ctx[0], ctx[1], a[d], ctx[2], ctx[3], h0)
        yl, _ = ssd_chunked(lat[0], lat[1], a[d], lat[2], lat[3], hc)
        if flip:
            yl, yc = jnp.flip(yl, axis=1), jnp.flip(yc, axis=1)
        return yl, yc

    ylf, ycf = direction(0, False)
    ylb, ycb = direction(1, True)
    dsk = d_skip.astype(F32).reshape(SSD_GROUPS, SSD_HPG, 1)

    def finish(yf, yb, xs, z, h):
        bsz, t = xs.shape[:2]
        y = (yf + yb + dsk * xs).reshape(bsz, t, SSD_INNER) * jax.nn.silu(z.astype(F32))
        yg = y.reshape(bsz, t, SSD_GROUPS, SSD_INNER // SSD_GROUPS)
        yg = yg * lax.rsqrt(jnp.mean(yg * yg, axis=-1, keepdims=True) + RMS_EPS)
        y = yg.reshape(bsz, t, SSD_INNER) * norm_g.astype(F32)
        return y.astype(h.dtype) @ w_out

    y_lat = finish(ylf, ylb, xl, zl, h_lat)
    y_ctx = finish(ycf, ycb, xc, zc, h_ctx) if need_ctx else None
    return y_lat, y_ctx


def expert_choice_ffn(h, w_router, w1, w3, w2):
    bsz, n, d = h.shape
    cap = max(1, CAPACITY_FACTOR * n // N_EXPERTS)
    aff = jax.nn.softmax(jnp.einsum('bnd,de->bne', h, w_router).astype(F32), axis=-1)
    gate, idx = lax.top_k(jnp.swapaxes(aff, 1, 2), cap)
    xs = jax.vmap(lambda hb, ib: hb[ib])(h, idx)
    hid = jax.nn.silu(jnp.einsum('becd,edf->becf', xs, w1)) * jnp.einsum('becd,edf->becf', xs, w3)
    ye = (jnp.einsum('becf,efd->becd', hid, w2) * gate[..., None]).astype(h.dtype)

    def combine(yb, ib):
        return jnp.zeros((n, d), h.dtype).at[ib.reshape(-1)].add(yb.reshape(-1, d))

    return jax.vmap(combine)(ye, idx)


def setup_inputs(seed: int = 0) -> dict:
    key = jax.random.key(seed)
    ks = iter(jax.random.split(key, 32))
    D = D_MODEL

    def nrm(shape, s):
        return jax.random.normal(next(ks), shape, F32) * s

    u_dt = jax.random.uniform(next(ks), (N_SSD_LAYERS, 2, SSD_HEADS), F32)
    dt0 = jnp.exp(u_dt * (math.log(0.1) - math.log(1e-3)) + math.log(1e-3))
    a_init = jax.random.uniform(next(ks), (N_SSD_LAYERS, 2, SSD_HEADS), F32, 1.0, 16.0)
    return {
        'x': nrm((BATCH, SEQ, D), 1.0),
        'c': nrm((BATCH, D), 1.0),
        'ctx': nrm((BATCH, CTX_LEN, D), 1.0),
        'c_ctx': nrm((D,), 1.0),
        'ada_w': nrm((DEPTH, D, 6 * D), 0.5 * D ** -0.5),
        'ada_b': nrm((DEPTH, 6 * D), 0.02),
        'norm_g': 1.0 + nrm((DEPTH, 2, D), 0.02),
        'final_g': 1.0 + nrm((D,), 0.02),
        'ab_w_in': nrm((N_AB_LAYERS, D, AB_IN), D ** -0.5),
        'ab_w_out': nrm((N_AB_LAYERS, AB_MIX, D), AB_MIX ** -0.5),
        'hgrn_lb_logits': nrm((2, N_AB_LAYERS + 1, HGRN_WIDTH), 0.1),
        'hgrn_onorm_g': 1.0 + nrm((N_AB_LAYERS, HGRN_DV), 0.02),
        'na_rpb': nrm((N_AB_LAYERS, NA_HEADS, 2 * NA_KR - 1, 2 * NA_KC - 1), 0.1),
        'ssd_w_in': nrm((N_SSD_LAYERS, D, SSD_IN), D ** -0.5),
        'ssd_conv_w': nrm((N_SSD_LAYERS, SSD_CONV, SSD_CONV_DIM), SSD_CONV ** -0.5),
        'ssd_conv_b': nrm((N_SSD_LAYERS, SSD_CONV_DIM), 0.02),
        'ssd_a_log': jnp.log(a_init),
        'ssd_dt_bias': dt0 + jnp.log(-jnp.expm1(-dt0)),
        'ssd_d': 1.0 + nrm((N_SSD_LAYERS, SSD_HEADS), 0.1),
        'ssd_norm_g': 1.0 + nrm((N_SSD_LAYERS, SSD_INNER), 0.02),
        'ssd_w_out': nrm((N_SSD_LAYERS, SSD_INNER, D), SSD_INNER ** -0.5),
        'moe_router': nrm((DEPTH, D, N_EXPERTS), D ** -0.5),
        'moe_w1': nrm((DEPTH, N_EXPERTS, D, EXPERT_FF), D ** -0.5),
        'moe_w3': nrm((DEPTH, N_EXPERTS, D, EXPERT_FF), D ** -0.5),
        'moe_w2': nrm((DEPTH, N_EXPERTS, EXPERT_FF, D), EXPERT_FF ** -0.5),
    }


def reference(x, c, ctx, c_ctx, ada_w, ada_b, norm_g, final_g, ab_w_in, ab_w_out, hgrn_lb_logits,
              hgrn_onorm_g, na_rpb, ssd_w_in, ssd_conv_w, ssd_conv_b, ssd_a_log, ssd_dt_bias, ssd_d,
              ssd_norm_g, ssd_w_out, moe_router, moe_w1, moe_w3, moe_w2):
    s_lat = jax.nn.silu(c)
    s_ctx = jax.nn.silu(c_ctx)
    lb_all = jnp.cumsum(jax.nn.softmax(hgrn_lb_logits.astype(F32), axis=1), axis=1)
    for l in range(DEPTH):
        need_ctx = l < DEPTH - 1
        mod_l = jnp.split((s_lat @ ada_w[l] + ada_b[l])[:, None, :], 6, axis=-1)
        mod_c = jnp.split(s_ctx @ ada_w[l] + ada_b[l], 6, axis=-1)
        h_lat = modulate(rmsnorm(x, norm_g[l, 0]), mod_l[0], mod_l[1])
        h_ctx = modulate(rmsnorm(ctx, norm_g[l, 0]), mod_c[0], mod_c[1])
        k = l // 2
        if l % 2 == 0:
            y_lat, y_ctx = ab_mixer(h_lat, h_ctx, ab_w_in[k], ab_w_out[k], lb_all[:, k],
                                    hgrn_onorm_g[k], na_rpb[k], need_ctx)
        else:
            y_lat, y_ctx = ssd_mixer(h_lat, h_ctx, ssd_w_in[k], ssd_conv_w[k], ssd_conv_b[k], ssd_a_log[k],
                                     ssd_dt_bias[k], ssd_d[k], ssd_norm_g[k], ssd_w_out[k], need_ctx)
        x = x + mod_l[2] * y_lat
        f_lat = modulate(rmsnorm(x, norm_g[l, 1]), mod_l[3], mod_l[4])
        x = x + mod_l[5] * expert_choice_ffn(f_lat, moe_router[l], moe_w1[l], moe_w3[l], moe_w2[l])
        if need_ctx:
            ctx = ctx + mod_c[2] * y_ctx
            f_ctx = modulate(rmsnorm(ctx, norm_g[l, 1]), mod_c[3], mod_c[4])
            ctx = ctx + mod_c[5] * expert_choice_ffn(f_ctx, moe_router[l], moe_w1[l], moe_w3[l], moe_w2[l])
    return rmsnorm(x, final_g)
```

```python
import contextlib
import numpy as np
import concourse.bass as bass
import concourse.mybir as mybir
from concourse.bass_utils import run_bass_kernel_spmd

F32 = mybir.dt.float32
BF16 = mybir.dt.bfloat16
I32 = mybir.dt.int32
AF = mybir.ActivationFunctionType
ALU = mybir.AluOpType
AX = mybir.AxisListType

D = 1024
CT = 256
GW = 64
EPS = 1e-6
NEG = -1e30


class Buf:
    __slots__ = ("name", "t", "w", "r")

    def __init__(self, name, t=None):
        self.name = name
        self.t = t
        self.w = None
        self.r = {}

    def __getitem__(self, idx):
        return self.t[idx]


class K:
    NQ = 8

    def __init__(self, nc, stack):
        self.nc = nc
        self.stack = stack
        self.cur = stack
        self.eng = {"pe": nc.tensor, "dve": nc.vector, "act": nc.scalar,
                    "pool": nc.gpsimd, "sp": nc.sync}
        self.sem = {}
        self.cnt = {}
        for e in ("pe", "dve", "act", "pool"):
            self.sem[e] = stack.enter_context(nc.semaphore("s_" + e))
            self.cnt[e] = 0
        for i in range(self.NQ):
            self.sem[("d", i)] = stack.enter_context(nc.semaphore("s_d%d" % i))
            self.cnt[("d", i)] = 0
        for i in range(self.NQ):
            self.sem[("g", i)] = stack.enter_context(nc.semaphore("s_g%d" % i))
            self.cnt[("g", i)] = 0
        for i in range(self.NQ):
            self.sem[("a", i)] = stack.enter_context(nc.semaphore("s_a%d" % i))
            self.cnt[("a", i)] = 0
        self.known = {e: {} for e in self.eng}
        self.adma_i = 0
        self.idma_i = 0
        self.dma_i = 0
        self.n_instr = 0
        self.n_wait = 0
        self._uid = 0
        self.marks = []

    def sb(self, name, shape, dt=F32):
        self._uid += 1
        t = self.cur.enter_context(self.nc.sbuf_tensor("%s_%d" % (name, self._uid), list(shape), dt))
        return Buf(name, t)

    def ps(self, name, shape, dt=F32):
        self._uid += 1
        t = self.cur.enter_context(self.nc.psum_tensor("%s_%d" % (name, self._uid), list(shape), dt))
        return Buf(name, t)

    def mark(self, name):
        self.marks.append((name, self.cnt["pe"], self.cnt["act"]))

    @contextlib.contextmanager
    def scope(self):
        prev = self.cur
        with contextlib.ExitStack() as st:
            self.cur = st
            yield
            self.barrier()
        self.cur = prev

    def barrier(self):
        for e in self.eng:
            kn = self.known[e]
            for key, v in self.cnt.items():
                if v > 0 and kn.get(key, 0) < v and not (key == e):
                    self.eng[e].wait_ge(self.sem[key], v)
                    kn[key] = v
                    self.n_wait += 1

    def _need(self, e, R, W):
        need = {}

        def add(ev):
            if ev is None:
                return
            k, v = ev
            if need.get(k, 0) < v:
                need[k] = v
        for b in R:
            add(b.w)
        for b in W:
            add(b.w)
            for k, v in b.r.items():
                add((k, v))
        kn = self.known[e]
        for k, v in need.items():
            if k == e and e == "pe":
                continue
            if kn.get(k, 0) >= v:
                continue
            self.eng[e].wait_ge(self.sem[k], v)
            self.n_wait += 1
            kn[k] = v

    def _done(self, ev, R, W):
        k, v = ev
        for b in R:
            if b.r.get(k, 0) < v:
                b.r[k] = v
        for b in W:
            b.w = ev
            b.r = {}

    def I(self, e, fn, R=(), W=()):
        self._need(e, R, W)
        ins = fn(self.eng[e])
        self.cnt[e] += 1
        ins.then_inc(self.sem[e], 1)
        self.n_instr += 1
        self._done((e, self.cnt[e]), R, W)
        return ins

    def dma(self, out, in_, R=(), W=(), q="sp", **kw):
        e = q
        if q == "sp":
            i = self.dma_i % self.NQ
            self.dma_i += 1
            key = ("d", i)
        else:
            i = self.adma_i % self.NQ
            self.adma_i += 1
            key = ("a", i)
        kn = self.known[e]
        if kn.get(key, 0) < self.cnt[key]:
            self.eng[e].wait_ge(self.sem[key], self.cnt[key])
            kn[key] = self.cnt[key]
        self._need(e, R, W)
        ins = self.eng[e].dma_start(out=out, in_=in_, **kw)
        self.cnt[key] += 16
        ins.then_inc(self.sem[key], 16)
        self.n_instr += 1
        self._done((key, self.cnt[key]), R, W)
        return ins

    def idma(self, R=(), W=(), **kw):
        e = "pool"
        i = self.idma_i % self.NQ
        self.idma_i += 1
        key = ("g", i)
        kn = self.known[e]
        if kn.get(key, 0) < self.cnt[key]:
            self.eng[e].wait_ge(self.sem[key], self.cnt[key])
            kn[key] = self.cnt[key]
        self._need(e, R, W)
        ins = self.eng[e].indirect_dma_start(**kw)
        self.cnt[key] += 16
        ins.then_inc(self.sem[key], 16)
        self.n_instr += 1
        self._done((key, self.cnt[key]), R, W)
        return ins

    def gdma(self, out, in_, R=(), W=()):
        e = "pool"
        i = self.idma_i % self.NQ
        self.idma_i += 1
        key = ("g", i)
        kn = self.known[e]
        if kn.get(key, 0) < self.cnt[key]:
            self.eng[e].wait_ge(self.sem[key], self.cnt[key])
            kn[key] = self.cnt[key]
        self._need(e, R, W)
        ins = self.eng[e].dma_start(out=out, in_=in_)
        self.cnt[key] += 16
        ins.then_inc(self.sem[key], 16)
        self.n_instr += 1
        self._done((key, self.cnt[key]), R, W)
        return ins

    def mm(self, ps, out, lb, lhsT, rb, rhs, start=True, stop=True):
        R = [lb, rb] if lb is not rb else [lb]
        return self.I("pe", lambda g: g.matmul(out, lhsT, rhs, start=start, stop=stop), R=R, W=[ps])

    def tr(self, ps, out, ib, in_, idb, ident):
        return self.I("pe", lambda g: g.transpose(out, in_, ident), R=[ib, idb], W=[ps])


class DT:
    def __init__(self, nc, name, shape, dt, ntile, kind="Internal"):
        self.t = nc.dram_tensor(name, list(shape), dt, kind=kind)
        self.ap = self.t.ap()
        self.b = [Buf("%s@%d" % (name, i)) for i in range(ntile)]
        self.all = self.b


def col_layout(v):
    v = np.asarray(v, np.float32)
    return np.ascontiguousarray(v.reshape(-1, 128).T)


CST = {}
MARKS = []


def make_consts():
    s = np.arange(128)[:, None]
    l = np.arange(128)[None, :]
    c = {}
    c["ident"] = np.eye(128, dtype=np.float32)
    c["Uf"] = (s <= l).astype(np.float32)
    c["Ub"] = (s >= l).astype(np.float32)
    c["Usf"] = (s > l).astype(np.float32)
    c["Usb"] = (s < l).astype(np.float32)
    midf = (s <= 63).astype(np.float32) * np.ones((1, 128), np.float32)
    midb = (s >= 64).astype(np.float32) * np.ones((1, 128), np.float32)
    c["R1f"] = np.concatenate([c["Uf"] - midf, midf[:, :1], 1.0 - midf[:, :1], np.zeros((128, 126), np.float32)], 1)
    c["R1b"] = np.concatenate([c["Ub"] - midb, midb[:, :1], 1.0 - midb[:, :1], np.zeros((128, 126), np.float32)], 1)
    c["ones"] = np.ones((128, 128), np.float32)
    c["G16"] = ((s % 16) == (l % 16)).astype(np.float32)
    names = list(c)
    off = {}
    o = 0
    for n in names:
        off[n] = (o, c[n].shape[1])
        o += c[n].shape[1]
    arr = np.concatenate([c[n] for n in names], 1)
    return arr, off


CONST_ARR, CONST_OFF = make_consts()


def na_bias_tables(rpb, rows):
    nt = rows // 2
    out = np.full((5, 8, 128, 896), NEG, np.float32)
    out[:, :, :, 640:] = 0.0
    qcol = np.arange(64)
    win0 = np.clip(qcol - 8, 0, 48)
    for cls, ti in enumerate([2, 0, 1, nt - 2, nt - 1]):
        c0 = min(max(ti - 2, 0), nt - 5)
        for rr in range(2):
            r = 2 * ti + rr
            r0 = min(max(r - 4, 0), rows - 8)
            for kr in range(10):
                gr = 2 * c0 + kr
                if gr < r0 or gr >= r0 + 8:
                    continue
                dr = gr - r + 7
                for c in range(64):
                    cc = np.arange(win0[c], win0[c] + 16)
                    dc = cc - c + 15
                    out[cls][:, rr * 64 + c, kr * 64 + cc] = rpb[:, dr][:, dc]
    return out


def build(S, stop="end", dbg=()):
    NT = S // 128
    NTT = NT + 2
    TOK = CT + S
    ROWS = S // GW
    nc = bass.Bass("TRN2", target_bir_lowering=False)
    ext = {}

    def ein(name, shape, dt=F32):
        ext[name] = nc.dram_tensor(name, list(shape), dt, kind="ExternalInput")
        return ext[name].ap()

    xin = ein("xin", [TOK, D])
    cst_d = ein("cst", list(CONST_ARR.shape))
    scol_d = ein("scol", [128, 8, 2])
    adaw_d = ein("ada_w", [2, D, 6 * D])
    adab_d = ein("adab_col", [128, 2, 48])
    ng_d = ein("ng_col", [128, 2, 2, 8])
    fing_d = ein("final_g", [D])
    abwin_d = ein("ab_w_in", [D, 4096])
    abwout_d = ein("ab_w_out", [D, D])
    lbl_d = ein("lb_logits", [2, 2, 512])
    ong_d = ein("onorm_g", [128])
    nab_d = ein("na_bias", [5, 8, 128, 896])
    swin_d = ein("ssd_w_in", [D, 6208])
    scw_d = ein("convw_col", [128, 32, 5])
    scb_d = ein("convb_col", [128, 32])
    salog_d = ein("ssd_a_log", [64])
    sdtb_d = ein("ssd_dt_bias", [64])
    sd_d = ein("ssd_d", [32])
    sng_d = ein("ssd_norm_g", [2048])
    swout_d = ein("ssd_w_out", [2048, D])
    rw_d = ein("moe_router", [2, D, 16])
    w1_d = ein("moe_w1", [2, 16, D, D])
    w3_d = ein("moe_w3", [2, 16, D, D])
    w2_d = ein("moe_w2", [2, 16, D, D])
    sel32_d = ein("sel32", [32, 32 * 128])
    ngnat_d = ein("norm_g_nat", [2, 2, D])

    outs = {}

    def dt_(name, shape, dt, ntile=NTT):
        kind = "ExternalOutput" if (name in dbg or name == "out") else "Internal"
        d = DT(nc, name, shape, dt, ntile, kind=kind)
        outs[name] = d
        return d

    out_d = dt_("out", [S, D], F32, NT)
    xres = dt_("xres", [TOK, D], F32)
    qh = dt_("qh", [TOK, 512], BF16)
    vh = dt_("vh", [TOK, 512], BF16)
    gs = dt_("gs", [TOK, 512], BF16)
    kd = [dt_("kf", [TOK, 512], BF16), dt_("kb", [TOK, 512], BF16)]
    gd = [dt_("gf", [TOK, 512], F32), dt_("gb", [TOK, 512], F32)]
    qnT = dt_("qnT", [512, TOK], BF16)
    knT = dt_("knT", [512, TOK], BF16)
    vn = dt_("vn", [TOK, 512], BF16)
    of_ = dt_("of", [TOK, 512], F32)
    mix = dt_("mix", [TOK, D], BF16)
    fTd = dt_("fTd", [D, TOK], BF16)
    aff = dt_("aff", [TOK, 16], F32)
    affT = dt_("affT", [16, TOK], F32)
    modc = dt_("modc", [128, 2 * 48 * 2], F32, 1)
    U32 = mybir.dt.uint32
    CAPL = (2 * S) // 16
    CB = CAPL + 256
    NR = CB + 128
    ftok = dt_("ftok", [TOK, D], BF16)
    Xsel = [nc.dram_tensor("Xsel%d" % e, [NR, D], BF16, kind="Internal") for e in range(16)]
    Ysel = [nc.dram_tensor("Ysel%d" % e, [NR, D], BF16, kind="Internal") for e in range(16)]
    uT = dt_("uT", [4096, TOK], BF16)
    zs = dt_("zs", [TOK, 2048], BF16)
    dtd = dt_("dtd", [TOK, 64], F32)
    dtad = dt_("dtad", [TOK, 64], F32)
    bcT = dt_("bcT", [2048, TOK], BF16)
    xB = dt_("xB", [TOK, 3072], BF16)
    yf = dt_("yf", [TOK, 2048], F32)
    if "coef" in dbg:
        dt_("coef", [TOK, 16], F32)
    if "slotsd" in dbg:
        dt_("slotsd", [TOK, 16], mybir.dt.uint32)

    with contextlib.ExitStack() as st:
        k = K(nc, st)
        NCC = CONST_ARR.shape[1]
        cst = k.sb("cst", [128, NCC])
        k.dma(cst[:], cst_d, W=[cst])

        def C(name, lo=0, hi=None):
            o, n = CONST_OFF[name]
            hi = n if hi is None else hi
            return cst[:, o + lo:o + hi]
        identb = k.sb("identb", [128, 128], BF16)
        k.I("dve", lambda g: g.tensor_copy(out=identb[:], in_=C("ident")), R=[cst], W=[identb])
        epsT = k.sb("eps", [128, 1])
        k.I("dve", lambda g: g.memset(epsT[:], EPS), W=[epsT])
        mod = k.sb("mod", [128, 2, 48, 2])
        AB = k.sb("AB", [128, 2, 4, 8, 2])
        g2row = [[None, None], [None, None]]
        g5row = [[None, None], [None, None]]

        with k.scope():
            scol = k.sb("scol", [128, 8, 2])
            k.dma(scol[:], scol_d, W=[scol])
            ssl = k.sb("ssl", [128, 8, 2])
            k.I("act", lambda g: g.activation(out=ssl[:], in_=scol[:], func=AF.Silu), R=[scol], W=[ssl])
            adab = k.sb("adab", [128, 2, 48])
            k.dma(adab[:], adab_d, W=[adab])
            ngc = k.sb("ngc", [128, 2, 2, 8])
            k.dma(ngc[:], ng_d, W=[ngc])
            wbuf = [k.sb("adaw", [128, 6 * D]) for _ in range(2)]
            pp = [k.ps("pp", [128, 512]) for _ in range(2)]
            n = 0
            for l in range(2):
                for j in range(8):
                    wb = wbuf[n % 2]
                    p = pp[n % 2]
                    n += 1
                    k.dma(wb[:], adaw_d[l, j * 128:(j + 1) * 128, :], W=[wb])
                    for fb in range(48):
                        k.mm(p, p[:, 2 * fb:2 * fb + 2], wb, wb[:, fb * 128:(fb + 1) * 128], ssl, ssl[:, j, :])
                    mv = mod[:, l, :, :]
                    pv = p[:, 0:96]
                    if j == 0:
                        k.I("dve", lambda g: g.tensor_copy(out=mv, in_=pv), R=[p], W=[mod])
                    else:
                        k.I("dve", lambda g: g.tensor_tensor(out=mv, in0=mv, in1=pv, op=ALU.add), R=[p, mod], W=[mod])
            for l in range(2):
                for r in range(2):
                    mv = mod[:, l, :, r]
                    k.I("dve", lambda g: g.tensor_tensor(out=mv, in0=mv, in1=adab[:, l, :], op=ALU.add),
                        R=[mod, adab], W=[mod])
            for l in range(2):
                for r in range(2):
                    for wi, (sh, sc) in enumerate([(0, 1), (3, 4)]):
                        Aout = AB[:, l, 2 * wi, :, r]
                        Bout = AB[:, l, 2 * wi + 1, :, r]
                        scv = mod[:, l, sc * 8:(sc + 1) * 8, r]
                        shv = mod[:, l, sh * 8:(sh + 1) * 8, r]
                        gv = ngc[:, l, wi, :]
                        k.I("dve", lambda g: g.scalar_tensor_tensor(out=Aout, in0=scv, scalar=1.0, in1=gv,
                                                                   op0=ALU.add, op1=ALU.mult),
                            R=[mod, ngc], W=[AB])
                        k.I("dve", lambda g: g.tensor_copy(out=Bout, in_=shv), R=[mod], W=[AB])
            if "modc" in dbg:
                k.dma(modc.ap, mod[:], R=[mod], W=modc.all)

        def gate_rows(l, chunk, r, dst):
            with k.scope():
                tp = k.ps("gtp", [128, 128])
                mT = k.sb("mT", [128, 128])
                bc = k.ps("gbc", [128, 512])
                src = k.sb("gsrc", [128, 8])
                k.I("dve", lambda g: g.tensor_copy(out=src[:], in_=mod[:, l, chunk * 8:(chunk + 1) * 8, r]), R=[mod], W=[src])
                k.tr(tp, tp[0:8, :], src, src[:], cst, C("ident"))
                k.I("act", lambda g: g.activation(out=mT[0:8, :], in_=tp[0:8, :], func=AF.Copy), R=[tp], W=[mT])
                for hf in range(2):
                    for q in range(4):
                        fb = hf * 4 + q
                        selb = k.sb("selb", [128, 128])
                        k.I("dve", lambda g: g.tensor_scalar(out=selb[0:8, :], in0=C("ones")[0:8, :],
                                                             scalar1=C("ident")[0:8, fb:fb + 1], scalar2=None,
                                                             op0=ALU.mult), R=[cst], W=[selb])
                        k.mm(bc, bc[:, q * 128:(q + 1) * 128], selb, selb[0:8, :], mT, mT[0:8, :])
                    k.I("act", lambda g: g.activation(out=dst[:, hf * 512:(hf + 1) * 512], in_=bc[:], func=AF.Copy),
                        R=[bc], W=[dst])

        def norm_tile(xt, l, wi, r, hT, col0, sc, outdt_ps, ident, identb_):
            sq, ss, xn, tps = sc
            k.I("act", lambda g: g.activation(out=sq[:], in_=xt[:], func=AF.Square, accum_out=ss[:]), R=[xt], W=[sq, ss])
            k.I("act", lambda g: g.activation(out=ss[:], in_=ss[:], func=AF.Sqrt, bias=epsT[:], scale=1.0 / D),
                R=[ss, epsT], W=[ss])
            k.I("dve", lambda g: g.reciprocal(out=ss[:], in_=ss[:]), R=[ss], W=[ss])
            k.I("dve", lambda g: g.tensor_scalar(out=xn[:], in0=xt[:], scalar1=ss[:], scalar2=None, op0=ALU.mult),
                R=[xt, ss], W=[xn])
            for j in range(8):
                k.tr(tps, tps[:, j * 128:(j + 1) * 128], xn, xn[:, j * 128:(j + 1) * 128], ident, identb_)
            for j in range(8):
                k.I("act", lambda g: g.activation(out=hT[:, j, col0:col0 + 128], in_=tps[:, j * 128:(j + 1) * 128],
                                                  func=AF.Identity, scale=AB[:, l, 2 * wi, j, r:r + 1],
                                                  bias=AB[:, l, 2 * wi + 1, j, r:r + 1]),
                    R=[tps, AB], W=[hT])

        def groups():
            yield 0, 2, 1
            for t in range(2, NTT, 4):
                yield t, min(4, NTT - t), 0

        def load_w_bf16(dst, src_ap, rows_chunks, cols, eng="pool", piece=2048):
            stg = [k.sb("wstg", [128, piece]) for _ in range(2)]
            n = 0
            for j in range(rows_chunks):
                for c0 in range(0, cols, piece):
                    c1 = min(cols, c0 + piece)
                    sb_ = stg[n % 2]
                    n += 1
                    k.dma(sb_[:, 0:c1 - c0], src_ap[j * 128:(j + 1) * 128, c0:c1], W=[sb_])
                    k.I(eng, lambda g: g.tensor_copy(out=dst[:, j, c0:c1], in_=sb_[:, 0:c1 - c0]), R=[sb_], W=[dst])

        k.mark("prelude")
        if stop == "prelude":
            k.barrier()
            return nc, outs

        with k.scope():
            W = k.sb("abwin", [128, 8, 4096], BF16)
            load_w_bf16(W, abwin_d, 8, 4096)
            lbr = k.sb("lbr", [128, 2, 2, 512])
            k.dma(lbr[:], lbl_d.partition_broadcast(128), W=[lbr])
            lb = k.sb("lb", [128, 2, 512])
            oml = k.sb("oml", [128, 2, 512])
            k.I("dve", lambda g: g.tensor_tensor(out=lb[:], in0=lbr[:, :, 0, :], in1=lbr[:, :, 1, :], op=ALU.subtract),
                R=[lbr], W=[lb])
            k.I("act", lambda g: g.activation(out=lb[:], in_=lb[:], func=AF.Sigmoid), R=[lb], W=[lb])
            k.I("dve", lambda g: g.tensor_scalar(out=oml[:], in0=lb[:], scalar1=-1.0, scalar2=1.0, op0=ALU.mult, op1=ALU.add),
                R=[lb], W=[oml])
            xt_ = [k.sb("xt", [128, D]) for _ in range(2)]
            sq = k.sb("sq", [128, D])
            ss_ = [k.sb("ss", [128, 1]) for _ in range(2)]
            xn_ = [k.sb("xn", [128, D], BF16) for _ in range(2)]
            tps_ = [k.ps("tps", [128, D], BF16) for _ in range(2)]
            hT_ = [k.sb("hT", [128, 8, 512], BF16) for _ in range(2)]
            pj = [k.ps("pj", [128, 512]) for _ in range(4)]
            ob16 = [k.sb("ob16", [128, 512], BF16) for _ in range(4)]
            o32 = [k.sb("o32", [128, 512]) for _ in range(2)]
            sg_ = [k.sb("sg", [128, 512]) for _ in range(2)]
            fT2 = [k.sb("fT2", [128, 512], BF16) for _ in range(2)]
            cnt = {"x": 0, "pj": 0, "ob": 0, "o32": 0, "ft": 0}

            def nxt(lst, key):
                v = lst[cnt[key] % len(lst)]
                cnt[key] += 1
                return v
            for gi, (t0, ng, r) in enumerate(groups()):
                hT = hT_[gi % 2]
                T = ng * 128
                for ti in range(ng):
                    t = t0 + ti
                    xt = nxt(xt_, "x")
                    i2 = cnt["x"] % 2
                    k.dma(xt[:], xin[t * 128:(t + 1) * 128, :], W=[xt])
                    norm_tile(xt, 0, 0, r, hT, ti * 128, (sq, ss_[i2], xn_[i2], tps_[i2]), None, identb, identb[:])
                for fb in range(8):
                    p = nxt(pj, "pj")
                    c0 = 2560 + fb * 128
                    for j in range(8):
                        k.mm(p, p[:, 0:T], W, W[:, j, c0:c0 + 128], hT, hT[:, j, 0:T], start=(j == 0), stop=(j == 7))
                    o = nxt(fT2, "ft")
                    k.I("act", lambda g: g.activation(out=o[:, 0:T], in_=p[:, 0:T], func=AF.Copy), R=[p], W=[o])
                    dst = qnT if fb < 4 else knT
                    rr = (fb % 4) * 128
                    k.dma(dst.ap[rr:rr + 128, t0 * 128:t0 * 128 + T], o[:, 0:T], R=[o], W=dst.b[t0:t0 + ng])
                for ti in range(ng):
                    t = t0 + ti
                    rows = slice(t * 128, (t + 1) * 128)
                    for cb in (0, 1, 2, 3, 4, 7):
                        p = nxt(pj, "pj")
                        for j in range(8):
                            k.mm(p, p[:], hT, hT[:, j, ti * 128:(ti + 1) * 128], W, W[:, j, cb * 512:(cb + 1) * 512],
                                 start=(j == 0), stop=(j == 7))
                        if cb in (0, 4):
                            o = nxt(ob16, "ob")
                            k.I("act", lambda g: g.activation(out=o[:], in_=p[:], func=AF.Silu), R=[p], W=[o])
                            dst = qh if cb == 0 else gs
                            k.dma(dst.ap[rows, :], o[:], R=[o], W=[dst.b[t]])
                        elif cb in (3, 7):
                            o = nxt(ob16, "ob")
                            k.I("act", lambda g: g.activation(out=o[:], in_=p[:], func=AF.Copy), R=[p], W=[o])
                            dst = vh if cb == 3 else vn
                            k.dma(dst.ap[rows, :], o[:], R=[o], W=[dst.b[t]])
                        else:
                            d = cb - 1
                            s_ = sg_[d]
                            k.I("act", lambda g: g.activation(out=s_[:], in_=p[:], func=AF.Sigmoid), R=[p], W=[s_])
                            k.I("dve", lambda g: g.tensor_tensor(out=s_[:], in0=s_[:], in1=oml[:, d, :], op=ALU.mult),
                                R=[s_, oml], W=[s_])
                            k.I("dve", lambda g: g.tensor_tensor(out=s_[:], in0=s_[:], in1=lb[:, d, :], op=ALU.add),
                                R=[s_, lb], W=[s_])
                            ko = nxt(ob16, "ob")
                            k.I("dve", lambda g: g.tensor_scalar(out=ko[:], in0=s_[:], scalar1=-1.0, scalar2=1.0,
                                                                 op0=ALU.mult, op1=ALU.add), R=[s_], W=[ko])
                            k.dma(kd[d].ap[rows, :], ko[:], R=[ko], W=[kd[d].b[t]])
                            go = nxt(o32, "o32")
                            k.I("act", lambda g: g.activation(out=go[:], in_=s_[:], func=AF.Ln), R=[s_], W=[go])
                            k.dma(gd[d].ap[rows, :], go[:], R=[go], W=[gd[d].b[t]])
        k.mark("A")
        if stop == "A":
            k.barrier()
            return nc, outs

        def hgrn_pass(d):
            with k.scope():
                R1 = C("R1f" if d == 0 else "R1b", 0, 130)
                Ust = C("Usf" if d == 0 else "Usb")
                Um = C("Uf" if d == 0 else "Ub")
                Sst = [k.sb("S%d" % h, [128, 128]) for h in range(4)]
                for h in range(4):
                    k.I("pool", lambda g: g.memset(Sst[h][:], 0.0), W=[Sst[h]])
                gain = k.sb("gain", [128, 128])
                k.dma(gain[:], ong_d.partition_broadcast(128), W=[gain])
                gt_ = [k.sb("gt", [128, 512]) for _ in range(2)]
                kt_ = [k.sb("kt", [128, 512], BF16) for _ in range(2)]
                qt_ = [k.sb("qt", [128, 512], BF16) for _ in range(2)]
                vt_ = [k.sb("vt", [128, 512], BF16) for _ in range(2)]
                ot_ = [k.sb("ot", [128, 512]) for _ in range(2)]
                oft_ = [k.sb("oft", [128, 512]) for _ in range(2)]
                gst_ = [k.sb("gst", [128, 512], BF16) for _ in range(2)]
                mixt_ = [k.sb("mixt", [128, 512], BF16) for _ in range(2)]
                psE = [k.ps("psE", [128, 258]) for _ in range(2)]
                psT = [k.ps("psT", [128, 256], BF16) for _ in range(2)]
                psA = k.ps("psA", [128, 128])
                psO = [k.ps("psO", [128, 128]) for _ in range(2)]
                psS = k.ps("psS", [128, 128])
                eq_ = [k.sb("eq", [128, 128]) for _ in range(2)]
                ek_ = [k.sb("ek", [128, 128]) for _ in range(2)]
                e2_ = [k.sb("e2", [128, 128]) for _ in range(2)]
                cmr_ = [k.sb("cmr", [128, 2]) for _ in range(2)]
                ct_ = [k.sb("ct", [128, 1]) for _ in range(2)]
                qin_ = [k.sb("qin", [128, 128], BF16) for _ in range(2)]
                kin_ = [k.sb("kin", [128, 128], BF16) for _ in range(2)]
                kout_ = [k.sb("kout", [128, 128], BF16) for _ in range(2)]
                at_ = [k.sb("at", [128, 128], BF16) for _ in range(2)]
                sm_ = [k.sb("smid", [128, 128], BF16) for _ in range(2)]
                junk = k.sb("junk", [128, 128])
                ssq_ = [k.sb("ssq", [128, 4]) for _ in range(2)]
                tmp_ = [k.sb("tmpo", [128, 512]) for _ in range(2)]
                order = list(range(NTT)) if d == 0 else [1, 0] + list(range(NTT - 1, 1, -1))

                def pre(it, t):
                    b2 = it % 2
                    rows = slice(t * 128, (t + 1) * 128)
                    gt, kt, qt, vt = gt_[b2], kt_[b2], qt_[b2], vt_[b2]
                    k.dma(gt[:], gd[d].ap[rows, :], R=[gd[d].b[t]], W=[gt])
                    k.dma(kt[:], kd[d].ap[rows, :], R=[kd[d].b[t]], W=[kt])
                    k.dma(qt[:], qh.ap[rows, :], R=[qh.b[t]], W=[qt])
                    k.dma(vt[:], vh.ap[rows, :], R=[vh.b[t]], W=[vt])
                    if d == 1:
                        k.dma(oft_[b2][:], of_.ap[rows, :], R=[of_.b[t]], W=[oft_[b2]])
                        k.dma(gst_[b2][:], gs.ap[rows, :], R=[gs.b[t]], W=[gst_[b2]])

                def s1(u, it, t, h):
                    b2, i2 = it % 2, u % 2
                    hs = slice(h * 128, (h + 1) * 128)
                    gt, kt, qt = gt_[b2], kt_[b2], qt_[b2]
                    pE, pT = psE[i2], psT[i2]
                    k.mm(pE, pE[:, 0:130], gt, gt[:, hs], cst, R1)
                    k.mm(pE, pE[:, 130:258], cst, Ust, gt, gt[:, hs])
                    k.tr(pT, pT[:, 0:128], qt, qt[:, hs], identb, identb[:])
                    k.tr(pT, pT[:, 128:256], kt, kt[:, hs], identb, identb[:])

                def s2(u, it, t, h):
                    b2, i2 = it % 2, u % 2
                    rows = slice(t * 128, (t + 1) * 128)
                    hs = slice(h * 128, (h + 1) * 128)
                    kt, vt, ot = kt_[b2], vt_[b2], ot_[b2]
                    pE, pT, pO = psE[i2], psT[i2], psO[i2]
                    eq, ek, e2, cmr, ct = eq_[i2], ek_[i2], e2_[i2], cmr_[i2], ct_[i2]
                    qin, kin, kout, at, smid = qin_[i2], kin_[i2], kout_[i2], at_[i2], sm_[i2]
                    Sh = Sst[h]
                    k.I("act", lambda g: g.activation(out=eq[:], in_=pE[:, 0:128], func=AF.Exp), R=[pE], W=[eq])
                    k.I("act", lambda g: g.activation(out=ek[:], in_=pE[:, 0:128], func=AF.Exp, scale=-1.0), R=[pE], W=[ek])
                    k.I("act", lambda g: g.activation(out=cmr[:], in_=pE[:, 128:130], func=AF.Exp), R=[pE], W=[cmr])
                    k.I("act", lambda g: g.activation(out=e2[:], in_=pE[:, 130:258], func=AF.Exp), R=[pE], W=[e2])
                    k.I("dve", lambda g: g.tensor_tensor(out=qin[:], in0=pT[:, 0:128], in1=eq[:], op=ALU.mult), R=[pT, eq], W=[qin])
                    k.I("dve", lambda g: g.tensor_tensor(out=kin[:], in0=pT[:, 128:256], in1=ek[:], op=ALU.mult), R=[pT, ek], W=[kin])
                    k.I("pool", lambda g: g.tensor_tensor(out=kout[:], in0=kt[:, hs], in1=e2[:], op=ALU.mult), R=[kt, e2], W=[kout])
                    k.I("dve", lambda g: g.tensor_tensor(out=ct[:], in0=cmr[:, 0:1], in1=cmr[:, 1:2], op=ALU.mult), R=[cmr], W=[ct])
                    k.mm(psA, psA[:], kin, kin[:], qin, qin[:])
                    k.I("dve", lambda g: g.tensor_tensor(out=at[:], in0=psA[:], in1=Um, op=ALU.mult), R=[psA, cst], W=[at])
                    k.I("pool", lambda g: g.tensor_scalar(out=smid[:], in0=Sh[:], scalar1=cmr[:, 0:1], scalar2=None, op0=ALU.mult),
                        R=[Sh, cmr], W=[smid])
                    k.mm(pO, pO[:], at, at[:], vt, vt[:, hs], start=True, stop=False)
                    k.mm(pO, pO[:], qin, qin[:], smid, smid[:], start=False, stop=True)
                    k.mm(psS, psS[:], kout, kout[:], vt, vt[:, hs])
                    k.I("dve", lambda g: g.scalar_tensor_tensor(out=Sh[:], in0=Sh[:], scalar=ct[:, 0:1], in1=psS[:],
                                                               op0=ALU.mult, op1=ALU.add), R=[Sh, ct, psS], W=[Sh])
                    if d == 0:
                        k.I("act", lambda g: g.activation(out=ot[:, hs], in_=pO[:], func=AF.Copy), R=[pO], W=[ot])
                    else:
                        oft = oft_[b2]
                        k.I("dve", lambda g: g.tensor_tensor(out=ot[:, hs], in0=pO[:], in1=oft[:, hs], op=ALU.add),
                            R=[pO, oft], W=[ot])
                    if h != 3:
                        return
                    if d == 0:
                        k.dma(of_.ap[rows, :], ot[:], R=[ot], W=[of_.b[t]], q="act")
                    else:
                        ssq, tmp, mixt, gst = ssq_[b2], tmp_[b2], mixt_[b2], gst_[b2]
                        for h_ in range(4):
                            hs_ = slice(h_ * 128, (h_ + 1) * 128)
                            k.I("act", lambda g: g.activation(out=junk[:], in_=ot[:, hs_], func=AF.Square, accum_out=ssq[:, h_:h_ + 1]),
                                R=[ot], W=[junk, ssq])
                        k.I("act", lambda g: g.activation(out=ssq[:], in_=ssq[:], func=AF.Sqrt, bias=epsT[:], scale=1.0 / 128),
                            R=[ssq, epsT], W=[ssq])
                        k.I("dve", lambda g: g.reciprocal(out=ssq[:], in_=ssq[:]), R=[ssq], W=[ssq])
                        for h_ in range(4):
                            hs_ = slice(h_ * 128, (h_ + 1) * 128)
                            k.I("pool", lambda g: g.scalar_tensor_tensor(out=tmp[:, hs_], in0=ot[:, hs_], scalar=ssq[:, h_:h_ + 1], in1=gain[:],
                                                                        op0=ALU.mult, op1=ALU.mult), R=[ot, ssq, gain], W=[tmp]) if False else \
                                k.I("dve", lambda g: g.scalar_tensor_tensor(out=tmp[:, hs_], in0=ot[:, hs_], scalar=ssq[:, h_:h_ + 1], in1=gain[:],
                                                                           op0=ALU.mult, op1=ALU.mult), R=[ot, ssq, gain], W=[tmp])
                        k.I("pool", lambda g: g.tensor_tensor(out=mixt[:], in0=tmp[:], in1=gst[:], op=ALU.mult), R=[tmp, gst], W=[mixt])
                        k.dma(mix.ap[rows, 0:512], mixt[:], R=[mixt], W=[mix.b[t]])
                units = [(it, t, h) for it, t in enumerate(order) for h in range(4)]
                pre(0, order[0])
                s1(0, *units[0])
                for u, (it, t, h) in enumerate(units):
                    if u + 1 < len(units):
                        nit, nt_, nh = units[u + 1]
                        if nh == 0:
                            pre(nit, nt_)
                        s1(u + 1, nit, nt_, nh)
                    s2(u, it, t, h)
        hgrn_pass(0)
        hgrn_pass(1)
        k.mark("B")
        if stop == "B":
            k.barrier()
            return nc, outs

        with k.scope():
            bias = k.sb("nab", [128, 5, 896])
            kT = k.sb("kT", [64, S], BF16)
            qT = k.sb("qT", [64, S], BF16)
            kcT = k.sb("kcT", [64, 256], BF16)
            qcT = k.sb("qcT", [64, 256], BF16)
            vb = k.sb("vb", [128, NT, 64], BF16)
            vcb = k.sb("vcb", [128, 2, 64], BF16)
            ob = k.sb("ob", [128, NTT, 64], BF16)
            s32_ = [k.sb("s32", [128, 896]) for _ in range(2)]
            p16_ = [k.sb("p16", [128, 896], BF16) for _ in range(2)]
            pT_ = [k.sb("pT", [128, 896], BF16) for _ in range(2)]
            nmx_ = [k.sb("nmx", [128, 1]) for _ in range(2)]
            sm_ = [k.sb("sm", [128, 1]) for _ in range(3)]
            psS0 = [k.ps("psS0", [128, 512]) for _ in range(2)]
            psS1 = [k.ps("psS1", [128, 384]) for _ in range(2)]
            psT = [k.ps("psTn", [128, 896], BF16) for _ in range(2)]
            psO = [k.ps("psOn", [128, 64]) for _ in range(2)]
            n = 0
            for h in range(8):
                fs = slice(h * 64, (h + 1) * 64)
                k.dma(bias[:], nab_d[:, h].rearrange("c q n -> q c n"), W=[bias])
                k.dma(kT[:], knT.ap[fs, CT:], R=knT.b[2:], W=[kT])
                k.dma(qT[:], qnT.ap[fs, CT:], R=qnT.b[2:], W=[qT])
                k.dma(kcT[:], knT.ap[fs, 0:CT], R=knT.b[0:2], W=[kcT])
                k.dma(qcT[:], qnT.ap[fs, 0:CT], R=qnT.b[0:2], W=[qcT])
                for c_ in range(0, NT, 16):
                    ce = min(NT, c_ + 16)
                    k.dma(vb[:, c_:ce, :], vn.ap[CT + c_ * 128:CT + ce * 128, fs].rearrange("(c p) d -> p c d", p=128),
                          R=vn.b[2 + c_:2 + ce], W=[vb])
                k.dma(vcb[:], vn.ap[0:CT, fs].rearrange("(c p) d -> p c d", p=128), R=vn.b[0:2], W=[vcb])
                def info(t):
                    if t < 2:
                        return 256, [(vcb, 0), (vcb, 1)], 0, 0
                    i = t - 2
                    cls = 1 if i == 0 else 2 if i == 1 else 3 if i == NT - 2 else 4 if i == NT - 1 else 0
                    c0 = min(max(i - 2, 0), NT - 5)
                    return 896, [(vb, c0 + c) for c in range(5)] + [(vcb, 0), (vcb, 1)], cls, c0

                def n1(t):
                    i2 = t % 2
                    s32, p16, nmx, sm = s32_[i2], p16_[i2], nmx_[i2], sm_[t % 3]
                    p0, p1 = psS0[i2], psS1[i2]
                    NK, vch, cls, c0 = info(t)
                    if t < 2:
                        k.mm(p1, p1[:, 128:384], qcT, qcT[:, t * 128:(t + 1) * 128], kcT, kcT[:])
                        k.I("dve", lambda g: g.tensor_scalar(out=s32[:, 0:256], in0=p1[:, 128:384], scalar1=0.125, scalar2=None,
                                                             op0=ALU.mult), R=[p1], W=[s32])
                    else:
                        i = t - 2
                        ql = qT[:, i * 128:(i + 1) * 128]
                        k.mm(p0, p0[:], qT, ql, kT, kT[:, c0 * 128:c0 * 128 + 512])
                        k.mm(p1, p1[:, 0:128], qT, ql, kT, kT[:, c0 * 128 + 512:c0 * 128 + 640])
                        k.mm(p1, p1[:, 128:384], qT, ql, kcT, kcT[:])
                        k.I("dve", lambda g: g.scalar_tensor_tensor(out=s32[:, 0:512], in0=p0[:], scalar=0.125, in1=bias[:, cls, 0:512],
                                                                   op0=ALU.mult, op1=ALU.add), R=[p0, bias], W=[s32])
                        k.I("dve", lambda g: g.scalar_tensor_tensor(out=s32[:, 512:896], in0=p1[:], scalar=0.125, in1=bias[:, cls, 512:896],
                                                                   op0=ALU.mult, op1=ALU.add), R=[p1, bias], W=[s32])
                    k.I("dve", lambda g: g.tensor_reduce(out=nmx[:], in_=s32[:, 0:NK], axis=AX.X, op=ALU.max, negate=True), R=[s32], W=[nmx])
                    k.I("act", lambda g: g.activation(out=p16[:, 0:NK], in_=s32[:, 0:NK], func=AF.Exp, bias=nmx[:], scale=1.0,
                                                      accum_out=sm[:]), R=[s32, nmx], W=[p16, sm])

                def n2(t):
                    i2 = t % 2
                    p16, pT, pt = p16_[i2], pT_[i2], psT[i2]
                    NK = info(t)[0]
                    for c in range(NK // 128):
                        k.tr(pt, pt[:, c * 128:(c + 1) * 128], p16, p16[:, c * 128:(c + 1) * 128], identb, identb[:])
                    k.I("act", lambda g: g.activation(out=pT[:, 0:NK], in_=pt[:, 0:NK], func=AF.Copy), R=[pt], W=[pT])

                def n3(t):
                    i2 = t % 2
                    pT, sm, po = pT_[i2], sm_[t % 3], psO[i2]
                    NK, vch, cls, c0 = info(t)
                    nch = NK // 128
                    for c, (vbuf, vc) in enumerate(vch):
                        k.mm(po, po[:], pT, pT[:, c * 128:(c + 1) * 128], vbuf, vbuf[:, vc, :], start=(c == 0), stop=(c == nch - 1))
                    k.I("dve", lambda g: g.reciprocal(out=sm[:], in_=sm[:]), R=[sm], W=[sm])
                    k.I("dve", lambda g: g.tensor_scalar(out=ob[:, t, :], in0=po[:], scalar1=sm[:], scalar2=None, op0=ALU.mult),
                        R=[po, sm], W=[ob])
                for step in range(NTT + 2):
                    if step < NTT:
                        n1(step)
                    if 0 <= step - 1 < NTT:
                        n2(step - 1)
                    if 0 <= step - 2 < NTT:
                        n3(step - 2)
                for c_ in range(0, NTT, 16):
                    ce = min(NTT, c_ + 16)
                    k.dma(mix.ap[c_ * 128:ce * 128, 512 + h * 64:512 + (h + 1) * 64].rearrange("(c p) d -> p c d", p=128), ob[:, c_:ce, :],
                          R=[ob], W=mix.b[c_:ce])
        k.mark("C")
        if stop == "C":
            k.barrier()
            return nc, outs

        class XS:
            pass
        XIN = XS()
        XIN.ap = xin
        XIN.b = [Buf("xin%d" % i) for i in range(NTT)]

        def alloc_post(l, with_ctx):
            P = {}
            P["g2"] = [k.sb("g2row", [128, D]) for _ in range(2)]
            gate_rows(l, 2, 0, P["g2"][0])
            if with_ctx:
                gate_rows(l, 2, 1, P["g2"][1])
            P["rw"] = k.sb("rw", [128, 8, 16])
            k.dma(P["rw"][:], rw_d[l].rearrange("(j p) e -> p j e", p=128), W=[P["rw"]])
            P["xt"] = [k.sb("xt", [128, D]) for _ in range(2)]
            P["tmp"] = k.sb("tmpx", [128, D])
            P["sq"] = k.sb("sqx", [128, D])
            P["ss"] = [k.sb("ssx", [128, 1]) for _ in range(2)]
            P["xn"] = [k.sb("xnx", [128, D]) for _ in range(2)]
            P["tps"] = k.ps("tps32", [128, D])
            P["fT32"] = [k.sb("fT32", [128, 8, 128]) for _ in range(2)]
            P["fTb"] = [k.sb("fTb", [128, 8, 128], BF16) for _ in range(2)]
            P["psR"] = k.ps("psR", [128, 16])
            P["psRT"] = k.ps("psRT", [128, 128])
            P["lg"] = [k.sb("lg", [128, 16]) for _ in range(2)]
            P["aT"] = [k.sb("aT", [128, 128]) for _ in range(2)]
            P["nmx"] = [k.sb("nmxr", [128, 1]) for _ in range(2)]
            P["sm"] = [k.sb("smr", [128, 1]) for _ in range(2)]
            P["A2row"] = [k.sb("A2row", [128, D]) for _ in range(2)]
            P["B2row"] = [k.sb("B2row", [128, D]) for _ in range(2)]
            ngr = k.sb("ngr", [128, D])
            k.dma(ngr[:], ngnat_d[l, 1].partition_broadcast(128), W=[ngr])
            for r in ([0, 1] if with_ctx else [0]):
                gate_rows(l, 3, r, P["B2row"][r])
                gate_rows(l, 4, r, P["A2row"][r])
                a2 = P["A2row"][r]
                k.I("dve", lambda g: g.scalar_tensor_tensor(out=a2[:], in0=a2[:], scalar=1.0, in1=ngr[:], op0=ALU.add, op1=ALU.mult),
                    R=[a2, ngr], W=[a2])
            P["ftk"] = [k.sb("ftk", [128, D], BF16) for _ in range(2)]
            P["tmp2"] = k.sb("tmp2x", [128, D])
            P["n"] = 0
            return P

        def pp2(P, l, t, r, psY, xsrc):
            i2 = t % 2
            rows = slice(t * 128, (t + 1) * 128)
            xt, tmp = P["xt"][i2], P["tmp"]
            g2 = P["g2"][r]
            k.dma(xt[:], xsrc.ap[rows, :], R=[xsrc.b[t]], W=[xt])
            for hf in range(2):
                hfs = slice(hf * 512, (hf + 1) * 512)
                k.I("dve", lambda g: g.tensor_tensor(out=tmp[:, hfs], in0=psY[hf][:], in1=g2[:, hfs], op=ALU.mult),
                    R=[psY[hf], g2], W=[tmp])
            k.I("pool", lambda g: g.tensor_tensor(out=xt[:], in0=xt[:], in1=tmp[:], op=ALU.add), R=[xt, tmp], W=[xt])
            k.dma(xres.ap[rows, :], xt[:], R=[xt], W=[xres.b[t]], q="act")
            sq, ss, xn = P["sq"], P["ss"][i2], P["xn"][i2]
            k.I("act", lambda g: g.activation(out=sq[:], in_=xt[:], func=AF.Square, accum_out=ss[:]), R=[xt], W=[sq, ss])
            k.I("act", lambda g: g.activation(out=ss[:], in_=ss[:], func=AF.Sqrt, bias=epsT[:], scale=1.0 / D),
                R=[ss, epsT], W=[ss])
            k.I("dve", lambda g: g.reciprocal(out=ss[:], in_=ss[:]), R=[ss], W=[ss])
            k.I("dve", lambda g: g.tensor_scalar(out=xn[:], in0=xt[:], scalar1=ss[:], scalar2=None, op0=ALU.mult),
                R=[xt, ss], W=[xn])

        def pp3(P, l, t, r):
            i2 = t % 2
            rows = slice(t * 128, (t + 1) * 128)
            xn, tps, fT32 = P["xn"][i2], P["tps"], P["fT32"][i2]
            for j in range(8):
                k.tr(tps, tps[:, j * 128:(j + 1) * 128], xn, xn[:, j * 128:(j + 1) * 128], cst, C("ident"))
            for j in range(8):
                k.I("act", lambda g: g.activation(out=fT32[:, j, :], in_=tps[:, j * 128:(j + 1) * 128],
                                                  func=AF.Identity, scale=AB[:, l, 2, j, r:r + 1],
                                                  bias=AB[:, l, 3, j, r:r + 1]),
                    R=[tps, AB], W=[fT32])
            ftk, tmp2 = P["ftk"][i2], P["tmp2"]
            k.I("pool", lambda g: g.tensor_tensor(out=tmp2[:], in0=xn[:], in1=P["A2row"][r][:], op=ALU.mult), R=[xn, P["A2row"][r]], W=[tmp2])
            k.I("pool", lambda g: g.tensor_tensor(out=ftk[:], in0=tmp2[:], in1=P["B2row"][r][:], op=ALU.add), R=[tmp2, P["B2row"][r]], W=[ftk])
            k.dma(ftok.ap[rows, :], ftk[:], R=[ftk], W=[ftok.b[t]], q="act")

        def pp4(P, l, t):
            i2 = t % 2
            rows = slice(t * 128, (t + 1) * 128)
            fT32 = P["fT32"][i2]
            psR, psRT, rw = P["psR"], P["psRT"], P["rw"]
            lg, aT, nmx, sm = P["lg"][i2], P["aT"][i2], P["nmx"][i2], P["sm"][i2]
            for j in range(8):
                k.mm(psR, psR[:, 0:16], fT32, fT32[:, j, :], rw, rw[:, j, :], start=(j == 0), stop=(j == 7))
            k.I("dve", lambda g: g.tensor_reduce(out=nmx[:], in_=psR[:, 0:16], axis=AX.X, op=ALU.max, negate=True), R=[psR], W=[nmx])
            k.I("act", lambda g: g.activation(out=lg[:], in_=psR[:, 0:16], func=AF.Exp, bias=nmx[:], scale=1.0, accum_out=sm[:]),
                R=[psR, nmx], W=[lg, sm])
            k.I("dve", lambda g: g.reciprocal(out=sm[:], in_=sm[:]), R=[sm], W=[sm])
            k.I("dve", lambda g: g.tensor_scalar(out=lg[:], in0=lg[:], scalar1=sm[:], scalar2=None, op0=ALU.mult), R=[lg, sm], W=[lg])
            k.dma(aff.ap[rows, :], lg[:], R=[lg], W=[aff.b[t]], q="act")
            k.tr(psRT, psRT[0:16, :], lg, lg[:], cst, C("ident"))
            k.I("act", lambda g: g.activation(out=aT[0:16, :], in_=psRT[0:16, :], func=AF.Copy), R=[psRT], W=[aT])
            k.dma(affT.ap[:, rows], aT[0:16, :], R=[aT], W=[affT.b[t]], q="act")

        def run_pipe(tiles, p1, p2, p3, p4):
            n = len(tiles)
            for s_ in range(n + 3):
                if s_ < n:
                    p1(tiles[s_])
                if 0 <= s_ - 1 < n:
                    p2(tiles[s_ - 1])
                if 0 <= s_ - 2 < n:
                    p3(tiles[s_ - 2])
                if 0 <= s_ - 3 < n:
                    p4(tiles[s_ - 3])

        with k.scope():
            Wo = k.sb("wout", [128, 8, D], BF16)
            load_w_bf16(Wo, abwout_d, 8, D)
            P = alloc_post(0, True)
            mt_ = [k.sb("mt", [128, D], BF16) for _ in range(2)]
            mT_ = [k.sb("mTt", [128, 8, 128], BF16) for _ in range(2)]
            psM = k.ps("psM", [128, D], BF16)
            psY = [k.ps("psY", [128, 512]) for _ in range(2)]
            def d1(t):
                mt, mT = mt_[t % 2], mT_[t % 2]
                k.dma(mt[:], mix.ap[t * 128:(t + 1) * 128, :], R=[mix.b[t]], W=[mt])
                for j in range(8):
                    k.tr(psM, psM[:, j * 128:(j + 1) * 128], mt, mt[:, j * 128:(j + 1) * 128], identb, identb[:])
                k.I("act", lambda g: g.activation(out=mT[:], in_=psM[:], func=AF.Copy), R=[psM], W=[mT])

            def d2(t):
                r = 1 if t < 2 else 0
                mT = mT_[t % 2]
                for hf in range(2):
                    for j in range(8):
                        k.mm(psY[hf], psY[hf][:], mT, mT[:, j, :], Wo, Wo[:, j, hf * 512:(hf + 1) * 512], start=(j == 0), stop=(j == 7))
                pp2(P, 0, t, r, psY, XIN)
            run_pipe(list(range(NTT)), d1, d2, lambda t: pp3(P, 0, t, 1 if t < 2 else 0), lambda t: pp4(P, 0, t))
        k.mark("D")
        if stop == "D":
            k.barrier()
            return nc, outs

        def moe_stage(l, with_ctx, final):
            with k.scope():
                affall = k.sb("affall", [128, NTT, 16])
                for c_ in range(0, NTT, 16):
                    ce = min(NTT, c_ + 16)
                    k.dma(affall[:, c_:ce, :], aff.ap[c_ * 128:ce * 128, :].rearrange("(c p) e -> p c e", p=128), R=aff.b[c_:ce], W=[affall])
                coef = k.sb("coef", [128, NTT, 16])
                thrrow = [k.sb("thrrow", [128, 16]) for _ in range(2)]
                streams = [(0, CT, S, (2 * S) // 16)] + ([(1, 0, CT, (2 * CT) // 16)] if with_ctx else [])
                for (r, tok0, ntok, kk) in streams:
                    with k.scope():
                        n8 = ntok // 8
                        A = k.sb("bisA", [128, n8])
                        junk = k.sb("bisJ", [128, n8])
                        for g8 in range(8):
                            k.dma(A[g8 * 16:(g8 + 1) * 16, :], affT.ap[:, tok0 + g8 * n8:tok0 + (g8 + 1) * n8], R=affT.all, W=[A])
                        lo = k.sb("lo", [128, 1])
                        t_ = k.sb("tt", [128, 1])
                        cp = k.sb("cp", [128, 1])
                        m_ = k.sb("mm", [128, 1])
                        psC = k.ps("psC", [128, 16])
                        k.I("dve", lambda g: g.memset(lo[:], 0.0), W=[lo])
                        w = 0.5
                        for it in range(40):
                            wv = w
                            k.I("dve", lambda g: g.tensor_scalar(out=t_[:], in0=lo[:], scalar1=wv, scalar2=None, op0=ALU.add), R=[lo], W=[t_])
                            k.I("dve", lambda g: g.tensor_scalar(out=junk[:], in0=A[:], scalar1=t_[:, 0:1], scalar2=0.0, op0=ALU.is_ge,
                                                                 op1=ALU.add, accum_out=cp[:]), R=[A, t_], W=[junk, cp])
                            k.mm(psC, psC[:, 0:1], cst, C("G16"), cp, cp[:, 0:1])
                            k.I("dve", lambda g: g.tensor_scalar(out=m_[:], in0=psC[:, 0:1], scalar1=float(kk) - 0.5, scalar2=wv,
                                                                 op0=ALU.is_ge, op1=ALU.mult), R=[psC], W=[m_])
                            k.I("dve", lambda g: g.tensor_tensor(out=lo[:], in0=lo[:], in1=m_[:], op=ALU.add), R=[lo, m_], W=[lo])
                            w *= 0.5
                        thrB = k.sb("thrB", [128, 128])
                        k.I("dve", lambda g: g.tensor_scalar(out=thrB[0:16, :], in0=C("ones")[0:16, :], scalar1=lo[0:16, 0:1], scalar2=None,
                                                             op0=ALU.mult), R=[cst, lo], W=[thrB])
                        k.mm(psC, psC[:, 0:16], thrB, thrB[0:16, :], cst, C("ident")[0:16, 0:16])
                        k.I("act", lambda g: g.activation(out=thrrow[r][:], in_=psC[:, 0:16], func=AF.Copy), R=[psC], W=[thrrow[r]])
                for t in range(NTT):
                    r = 1 if t < 2 else 0
                    if r == 1 and not with_ctx:
                        continue
                    k.I("dve", lambda g: g.tensor_tensor(out=coef[:, t, :], in0=affall[:, t, :], in1=thrrow[r][:], op=ALU.is_ge),
                        R=[affall, thrrow[r]], W=[coef])
                    k.I("dve", lambda g: g.tensor_tensor(out=coef[:, t, :], in0=coef[:, t, :], in1=affall[:, t, :], op=ALU.mult),
                        R=[affall, coef], W=[coef])
                if "coef" in dbg:
                    k.dma(outs["coef"].ap.rearrange("(c p) e -> p c e", p=128), coef[:], R=[coef], W=outs["coef"].all)
                g5 = [k.sb("g5row", [128, D]) for _ in range(2)]
                gate_rows(l, 5, 0, g5[0])
                if with_ctx:
                    gate_rows(l, 5, 1, g5[1])
                if final:
                    fgrow = k.sb("fgrow", [128, D])
                    k.dma(fgrow[:], fing_d.partition_broadcast(128), W=[fgrow])
                fT = k.sb("fTm", [128, 8, 1024], BF16)
                acc = k.sb("acc", [128, 8, D])
                W1 = k.sb("W1", [128, 8, D], BF16)
                W3 = k.sb("W3", [128, 8, D], BF16)
                W2 = k.sb("W2", [128, 8, D], BF16)
                hid = k.sb("hid", [128, 8, 1024], BF16)
                stg = [k.sb("mstg", [128, 1, D]) for _ in range(3)]
                s1_ = [k.sb("s1", [128, 512], BF16) for _ in range(2)]
                ps1 = [k.ps("ps1", [128, 512]) for _ in range(2)]
                ps3 = [k.ps("ps3", [128, 512]) for _ in range(2)]
                psy = [k.ps("psy", [128, 512]) for _ in range(2)]
                xt_ = [k.sb("xtm", [128, D]) for _ in range(2)]
                tmpm = k.sb("tmpm", [128, D])
                sqm = k.sb("sqm", [128, D])
                ssm = [k.sb("ssm", [128, 1]) for _ in range(2)]
                cn = {"stg": 0, "h": 0, "y": 0, "x": 0}

                def loadw(dst, src):
                    for jp in range(8):
                        sb_ = stg[cn["stg"] % 3]
                        cn["stg"] += 1
                        k.dma(sb_[:, 0, :], src[jp * 128:(jp + 1) * 128, :], W=[sb_])
                        k.I("pool", lambda g: g.tensor_copy(out=dst[:, jp, :], in_=sb_[:, 0, :]), R=[sb_], W=[dst])
                sgs = ([(0, 2, 1)] if with_ctx else []) + [(t0, min(8, NTT - t0), 0) for t0 in range(2, NTT, 8)]
                for (t0, nt, r) in sgs:
                    T = nt * 128
                    k.dma(fT[:, :, 0:T], fTd.ap[:, t0 * 128:t0 * 128 + T].rearrange("(j p) t -> p j t", p=128),
                          R=fTd.b[t0:t0 + nt], W=[fT])
                    for e in range(16):
                        loadw(W1, w1_d[l, e])
                        loadw(W3, w3_d[l, e])
                        for g0 in range(0, T, 512):
                            Tg = min(512, T - g0)
                            for ffc in range(8):
                                i2 = cn["h"] % 2
                                cn["h"] += 1
                                p1, p3, s1 = ps1[i2], ps3[i2], s1_[i2]
                                for j in range(8):
                                    k.mm(p1, p1[:, 0:Tg], W1, W1[:, j, ffc * 128:(ffc + 1) * 128], fT, fT[:, j, g0:g0 + Tg],
                                         start=(j == 0), stop=(j == 7))
                                for j in range(8):
                                    k.mm(p3, p3[:, 0:Tg], W3, W3[:, j, ffc * 128:(ffc + 1) * 128], fT, fT[:, j, g0:g0 + Tg],
                                         start=(j == 0), stop=(j == 7))
                                k.I("act", lambda g: g.activation(out=s1[:, 0:Tg], in_=p1[:, 0:Tg], func=AF.Silu), R=[p1], W=[s1])
                                k.I("dve", lambda g: g.tensor_tensor(out=hid[:, ffc, g0:g0 + Tg], in0=s1[:, 0:Tg], in1=p3[:, 0:Tg], op=ALU.mult),
                                    R=[s1, p3], W=[hid])
                        loadw(W2, w2_d[l, e])
                        for ti in range(nt):
                            for hf in range(2):
                                py = psy[cn["y"] % 2]
                                cn["y"] += 1
                                hfs = slice(hf * 512, (hf + 1) * 512)
                                for ffc in range(8):
                                    k.mm(py, py[:], hid, hid[:, ffc, ti * 128:(ti + 1) * 128], W2, W2[:, ffc, hfs],
                                         start=(ffc == 0), stop=(ffc == 7))
                                cf = coef[:, t0 + ti, e:e + 1]
                                if e == 0:
                                    k.I("dve", lambda g: g.tensor_scalar(out=acc[:, ti, hfs], in0=py[:], scalar1=cf, scalar2=None, op0=ALU.mult),
                                        R=[py, coef], W=[acc])
                                else:
                                    k.I("dve", lambda g: g.scalar_tensor_tensor(out=acc[:, ti, hfs], in0=py[:], scalar=cf, in1=acc[:, ti, hfs],
                                                                               op0=ALU.mult, op1=ALU.add), R=[py, coef, acc], W=[acc])
                    for ti in range(nt):
                        t = t0 + ti
                        rows = slice(t * 128, (t + 1) * 128)
                        xt = xt_[cn["x"] % 2]
                        ss = ssm[cn["x"] % 2]
                        cn["x"] += 1
                        k.dma(xt[:], xres.ap[rows, :], R=[xres.b[t]], W=[xt])
                        k.I("pool", lambda g: g.tensor_tensor(out=tmpm[:], in0=acc[:, ti, :], in1=g5[r][:], op=ALU.mult), R=[acc, g5[r]], W=[tmpm])
                        k.I("pool", lambda g: g.tensor_tensor(out=xt[:], in0=xt[:], in1=tmpm[:], op=ALU.add), R=[xt, tmpm], W=[xt])
                        if not final:
                            k.dma(xres.ap[rows, :], xt[:], R=[xt], W=[xres.b[t]])
                        else:
                            k.I("act", lambda g: g.activation(out=sqm[:], in_=xt[:], func=AF.Square, accum_out=ss[:]), R=[xt], W=[sqm, ss])
                            k.I("act", lambda g: g.activation(out=ss[:], in_=ss[:], func=AF.Sqrt, bias=epsT[:], scale=1.0 / D), R=[ss, epsT], W=[ss])
                            k.I("dve", lambda g: g.reciprocal(out=ss[:], in_=ss[:]), R=[ss], W=[ss])
                            k.I("dve", lambda g: g.scalar_tensor_tensor(out=tmpm[:], in0=xt[:], scalar=ss[:, 0:1], in1=fgrow[:],
                                                                       op0=ALU.mult, op1=ALU.mult), R=[xt, ss, fgrow], W=[tmpm])
                            k.dma(out_d.ap[(t - 2) * 128:(t - 1) * 128, :], tmpm[:], R=[tmpm], W=[out_d.b[t - 2]])
        BCREG = {}

        def moe_sparse(l, with_ctx, final):
            IOA = bass.IndirectOffsetOnAxis
            if "bc" not in BCREG:
                BCREG["bc"] = nc.gpsimd.to_reg(NR - 1)
            bcr = BCREG["bc"]
            with k.scope():
                affall = k.sb("affall", [128, NTT, 16])
                for c_ in range(0, NTT, 16):
                    ce = min(NTT, c_ + 16)
                    k.dma(affall[:, c_:ce, :], aff.ap[c_ * 128:ce * 128, :].rearrange("(c p) e -> p c e", p=128), R=aff.b[c_:ce], W=[affall])
                coef = k.sb("coef", [128, NTT, 16])
                selm = k.sb("selm", [128, NTT, 16])
                slots = k.sb("slots", [128, NTT, 16], U32)
                k.I("dve", lambda g: g.memset(selm[:], 0.0), W=[selm])
                k.I("dve", lambda g: g.memset(coef[:], 0.0), W=[coef])
                thrrow = [k.sb("thrrow", [128, 16]) for _ in range(2)]
                streams = [(0, CT, S, CAPL)] + ([(1, 0, CT, (2 * CT) // 16)] if with_ctx else [])
                for (r, tok0, ntok, kk) in streams:
                    with k.scope():
                        n8 = ntok // 8
                        A = k.sb("bisA", [128, n8])
                        junk = k.sb("bisJ", [128, n8])
                        for g8 in range(8):
                            k.dma(A[g8 * 16:(g8 + 1) * 16, :], affT.ap[:, tok0 + g8 * n8:tok0 + (g8 + 1) * n8], R=affT.all, W=[A])
                        lo = k.sb("lo", [128, 1])
                        t_ = k.sb("tt", [128, 1])
                        cp = k.sb("cp", [128, 1])
                        m_ = k.sb("mm", [128, 1])
                        psC = k.ps("psC", [128, 16])
                        k.I("dve", lambda g: g.memset(lo[:], 0.0), W=[lo])
                        w = 0.5
                        for it in range(40):
                            wv = w
                            k.I("dve", lambda g: g.tensor_scalar(out=t_[:], in0=lo[:], scalar1=wv, scalar2=None, op0=ALU.add), R=[lo], W=[t_])
                            k.I("dve", lambda g: g.tensor_scalar(out=junk[:], in0=A[:], scalar1=t_[:, 0:1], scalar2=0.0, op0=ALU.is_ge,
                                                                 op1=ALU.add, accum_out=cp[:]), R=[A, t_], W=[junk, cp])
                            k.mm(psC, psC[:, 0:1], cst, C("G16"), cp, cp[:, 0:1])
                            k.I("dve", lambda g: g.tensor_scalar(out=m_[:], in0=psC[:, 0:1], scalar1=float(kk) - 0.5, scalar2=wv,
                                                                 op0=ALU.is_ge, op1=ALU.mult), R=[psC], W=[m_])
                            k.I("dve", lambda g: g.tensor_tensor(out=lo[:], in0=lo[:], in1=m_[:], op=ALU.add), R=[lo, m_], W=[lo])
                            w *= 0.5
                        thrB = k.sb("thrB", [128, 128])
                        k.I("dve", lambda g: g.tensor_scalar(out=thrB[0:16, :], in0=C("ones")[0:16, :], scalar1=lo[0:16, 0:1], scalar2=None,
                                                             op0=ALU.mult), R=[cst, lo], W=[thrB])
                        k.mm(psC, psC[:, 0:16], thrB, thrB[0:16, :], cst, C("ident")[0:16, 0:16])
                        k.I("act", lambda g: g.activation(out=thrrow[r][:], in_=psC[:, 0:16], func=AF.Copy), R=[psC], W=[thrrow[r]])
                tl = [t for t in range(NTT) if (t >= 2 or with_ctx)]
                for t in tl:
                    r = 1 if t < 2 else 0
                    k.I("dve", lambda g: g.tensor_tensor(out=selm[:, t, :], in0=affall[:, t, :], in1=thrrow[r][:], op=ALU.is_ge),
                        R=[affall, thrrow[r]], W=[selm])
                k.I("dve", lambda g: g.tensor_tensor(out=coef[:], in0=selm[:], in1=affall[:], op=ALU.mult), R=[selm, affall], W=[coef])
                with k.scope():
                    rank = k.sb("rank", [128, NTT, 16])
                    cntb = k.sb("cntb", [128, NTT, 16])
                    offs = k.sb("offs", [128, NTT, 16])
                    onesr = k.sb("onesr", [128, NTT])
                    k.I("dve", lambda g: g.memset(onesr[:], 1.0), W=[onesr])
                    k.I("dve", lambda g: g.memset(offs[:], 0.0), W=[offs])
                    psK = [k.ps("psK", [128, 512]) for _ in range(2)]
                    sf = selm[:].rearrange("p t e -> p (t e)")
                    rf = rank[:].rearrange("p t e -> p (t e)")
                    cf_ = cntb[:].rearrange("p t e -> p (t e)")
                    for c0 in range(0, NTT * 16, 512):
                        c1 = min(NTT * 16, c0 + 512)
                        k.mm(psK[0], psK[0][:, 0:c1 - c0], cst, C("Usb"), selm, sf[:, c0:c1])
                        k.I("act", lambda g: g.activation(out=rf[:, c0:c1], in_=psK[0][:, 0:c1 - c0], func=AF.Copy), R=[psK[0]], W=[rank])
                        k.mm(psK[1], psK[1][:, 0:c1 - c0], cst, C("ones"), selm, sf[:, c0:c1])
                        k.I("act", lambda g: g.activation(out=cf_[:, c0:c1], in_=psK[1][:, 0:c1 - c0], func=AF.Copy), R=[psK[1]], W=[cntb])
                    for e in range(16):
                        k.I("dve", lambda g: g.tensor_tensor_scan(out=offs[:, 2:NTT, e], data0=onesr[:, 2:NTT], data1=cntb[:, 2:NTT, e],
                                                                  initial=0.0, op0=ALU.mult, op1=ALU.add), R=[onesr, cntb], W=[offs])
                    k.I("dve", lambda g: g.tensor_tensor(out=offs[:, 2:NTT, :], in0=offs[:, 2:NTT, :], in1=cntb[:, 2:NTT, :], op=ALU.subtract),
                        R=[offs, cntb], W=[offs])
                    if with_ctx:
                        k.I("dve", lambda g: g.memset(offs[:, 0, :], float(CB)), W=[offs])
                        k.I("dve", lambda g: g.tensor_scalar(out=offs[:, 1, :], in0=cntb[:, 0, :], scalar1=float(CB), scalar2=None, op0=ALU.add),
                            R=[cntb], W=[offs])
                    BIG = 1.0e6
                    k.I("dve", lambda g: g.tensor_tensor(out=rank[:], in0=rank[:], in1=offs[:], op=ALU.add), R=[rank, offs], W=[rank])
                    k.I("dve", lambda g: g.tensor_scalar(out=rank[:], in0=rank[:], scalar1=-BIG, scalar2=None, op0=ALU.add), R=[rank], W=[rank])
                    k.I("dve", lambda g: g.tensor_tensor(out=rank[:], in0=rank[:], in1=selm[:], op=ALU.mult), R=[rank, selm], W=[rank])
                    k.I("dve", lambda g: g.tensor_scalar(out=rank[:], in0=rank[:], scalar1=BIG, scalar2=None, op0=ALU.add), R=[rank], W=[rank])
                    k.I("dve", lambda g: g.tensor_copy(out=slots[:], in_=rank[:]), R=[rank], W=[slots])
                    if "slotsd" in dbg:
                        k.dma(outs["slotsd"].ap.rearrange("(c p) e -> p c e", p=128), slots[:], R=[slots], W=outs["slotsd"].all)
                XZ = [Buf("xz%d" % e) for e in range(16)]
                XW = [[] for e in range(16)]
                YW = [Buf("yw%d" % e) for e in range(16)]
                with k.scope():
                    zt = k.sb("zt", [128, 4, D], BF16)
                    k.I("dve", lambda g: g.memset(zt[:], 0.0), W=[zt])
                    for e in range(16):
                        for r0 in range(0, NR, 512):
                            r1 = min(NR, r0 + 512)
                            nq = (r1 - r0) // 128
                            k.dma(Xsel[e].ap()[r0:r1, :].rearrange("(c p) d -> p c d", p=128), zt[:, 0:nq, :], R=[zt, XZ[e]], W=[])
                            XZ[e].r = {}
                    k.barrier()
                with k.scope():
                    W1 = k.sb("W1", [128, 8, D], BF16)
                    W3 = k.sb("W3", [128, 8, D], BF16)
                    W2 = k.sb("W2", [128, 8, D], BF16)
                    stg = [k.sb("mstg", [128, D]) for _ in range(4)]
                    xl_ = [k.sb("xl", [128, D], BF16) for _ in range(3)]
                    XT_ = [k.sb("XT", [128, 8, 512], BF16) for _ in range(2)]
                    hid_ = [k.sb("hid", [128, 8, 512], BF16) for _ in range(2)]
                    s1_ = [k.sb("s1", [128, 512], BF16) for _ in range(2)]
                    yb_ = [k.sb("yb", [128, D], BF16) for _ in range(2)]
                    psX = k.ps("psX", [128, D], BF16)
                    ps1 = [k.ps("ps1", [128, 512]) for _ in range(2)]
                    ps3 = [k.ps("ps3", [128, 512]) for _ in range(2)]
                    psy = [k.ps("psy", [128, 512]) for _ in range(2)]
                    cn = {"stg": 0, "h": 0, "y": 0, "x": 0, "g": 0, "f": 0}
                    ftl = [k.sb("ftl", [128, D], BF16) for _ in range(4)]

                    def scatter(e):
                        seq = []
                        for t in tl:
                            seq.append((t, cn["f"]))
                            cn["f"] += 1

                        def ld(i):
                            t, fi = seq[i]
                            ft = ftl[fi % 4]
                            k.gdma(ft[:], ftok.ap[t * 128:(t + 1) * 128, :], R=[ftok.b[t]], W=[ft])
                        ld(0)
                        if len(seq) > 1:
                            ld(1)
                        for i, (t, fi) in enumerate(seq):
                            if i + 2 < len(seq):
                                ld(i + 2)
                            ft = ftl[fi % 4]
                            wb = Buf("xw")
                            XW[e].append(wb)
                            k.idma(R=[ft, slots], W=[wb], out=Xsel[e].ap(), out_offset=IOA(ap=slots[:, t, e:e + 1], axis=0),
                                   in_=ft[:], in_offset=None, bounds_check=bcr, oob_is_err=False)

                    def loadw(dst, src):
                        for jp in range(8):
                            sb_ = stg[cn["stg"] % 4]
                            cn["stg"] += 1
                            k.dma(sb_[:], src[jp * 128:(jp + 1) * 128, :], W=[sb_])
                            k.I("dve", lambda g: g.tensor_copy(out=dst[:, jp, :], in_=sb_[:]), R=[sb_], W=[dst])
                    ntl = (CAPL + 128) // 128
                    sgroups = [(g0, min(4, ntl - g0)) for g0 in range(0, ntl, 4)] + ([(CB // 128, 1)] if with_ctx else [])
                    scatter(0)
                    for e in range(16):
                        loadw(W1, w1_d[l, e])
                        loadw(W3, w3_d[l, e])
                        loadw(W2, w2_d[l, e])
                        if e + 1 < 16:
                            scatter(e + 1)
                        for (st0, nst) in sgroups:
                            XT = XT_[cn["g"] % 2]
                            hid = hid_[cn["g"] % 2]
                            cn["g"] += 1
                            Tg = nst * 128
                            for q in range(nst):
                                xl = xl_[cn["x"] % 3]
                                cn["x"] += 1
                                r0 = (st0 + q) * 128
                                k.dma(xl[:], Xsel[e].ap()[r0:r0 + 128, :], R=XW[e], W=[xl])
                                for j in range(8):
                                    k.tr(psX, psX[:, j * 128:(j + 1) * 128], xl, xl[:, j * 128:(j + 1) * 128], identb, identb[:])
                                k.I("act", lambda g: g.activation(out=XT[:, :, q * 128:(q + 1) * 128], in_=psX[:].rearrange("p (j c) -> p j c", j=8),
                                                                  func=AF.Copy), R=[psX], W=[XT])
                            for ffc in range(8):
                                i2 = cn["h"] % 2
                                cn["h"] += 1
                                p1, p3, s1 = ps1[i2], ps3[i2], s1_[i2]
                                for j in range(8):
                                    k.mm(p1, p1[:, 0:Tg], W1, W1[:, j, ffc * 128:(ffc + 1) * 128], XT, XT[:, j, 0:Tg], start=(j == 0), stop=(j == 7))
                                for j in range(8):
                                    k.mm(p3, p3[:, 0:Tg], W3, W3[:, j, ffc * 128:(ffc + 1) * 128], XT, XT[:, j, 0:Tg], start=(j == 0), stop=(j == 7))
                                k.I("act", lambda g: g.activation(out=s1[:, 0:Tg], in_=p1[:, 0:Tg], func=AF.Silu), R=[p1], W=[s1])
                                k.I("dve", lambda g: g.tensor_tensor(out=hid[:, ffc, 0:Tg], in0=s1[:, 0:Tg], in1=p3[:, 0:Tg], op=ALU.mult),
                                    R=[s1, p3], W=[hid])
                            for q in range(nst):
                                yb = yb_[cn["y"] % 2]
                                r0 = (st0 + q) * 128
                                for hf in range(2):
                                    py = psy[hf]
                                    hfs = slice(hf * 512, (hf + 1) * 512)
                                    for ffc in range(8):
                                        k.mm(py, py[:], hid, hid[:, ffc, q * 128:(q + 1) * 128], W2, W2[:, ffc, hfs], start=(ffc == 0), stop=(ffc == 7))
                                    if hf == 0:
                                        k.I("act", lambda g: g.activation(out=yb[:, hfs], in_=py[:], func=AF.Copy), R=[py], W=[yb])
                                    else:
                                        k.I("dve", lambda g: g.tensor_copy(out=yb[:, hfs], in_=py[:]), R=[py], W=[yb])
                                cn["y"] += 1
                                k.dma(Ysel[e].ap()[r0:r0 + 128, :], yb[:], R=[yb], W=[], q="act")
                    k.barrier()
                with k.scope():
                    g5 = [k.sb("g5row", [128, D]) for _ in range(2)]
                    gate_rows(l, 5, 0, g5[0])
                    if with_ctx:
                        gate_rows(l, 5, 1, g5[1])
                    if final:
                        fgrow = k.sb("fgrow", [128, D])
                        k.dma(fgrow[:], fing_d.partition_broadcast(128), W=[fgrow])
                    yg_ = [k.sb("yg", [128, D], BF16) for _ in range(6)]
                    for b_ in yg_:
                        k.I("dve", lambda g: g.memset(b_[:], 0.0), W=[b_])
                    dg_ = [k.sb("dgc", [128, 128], BF16) for _ in range(4)]
                    psg = [[k.ps("psg", [128, 512]) for _ in range(2)] for _ in range(2)]
                    xt_ = [k.sb("xtm", [128, D]) for _ in range(2)]
                    tmpm_ = [k.sb("tmpm", [128, D]) for _ in range(2)]
                    sqm = k.sb("sqm", [128, D])
                    ssm = [k.sb("ssm", [128, 1]) for _ in range(2)]
                    n = 0
                    for it, t in enumerate(tl):
                        r = 1 if t < 2 else 0
                        rows = slice(t * 128, (t + 1) * 128)
                        pg, xt, ss, tmpm = psg[it % 2], xt_[it % 2], ssm[it % 2], tmpm_[it % 2]
                        k.dma(xt[:], xres.ap[rows, :], R=[xres.b[t]], W=[xt])
                        for e in range(16):
                            yg = yg_[n % 6]
                            dg = dg_[n % 4]
                            n += 1
                            k.idma(R=[slots], W=[yg], out=yg[:], out_offset=None, in_=Ysel[e].ap(),
                                   in_offset=IOA(ap=slots[:, t, e:e + 1], axis=0), bounds_check=bcr, oob_is_err=False)
                            k.I("dve", lambda g: g.tensor_scalar(out=dg[:], in0=identb[:], scalar1=coef[:, t, e:e + 1], scalar2=None, op0=ALU.mult),
                                R=[identb, coef], W=[dg])
                            for hf in range(2):
                                k.mm(pg[hf], pg[hf][:], dg, dg[:], yg, yg[:, hf * 512:(hf + 1) * 512], start=(e == 0), stop=(e == 15))
                        for hf in range(2):
                            hfs = slice(hf * 512, (hf + 1) * 512)
                            k.I("dve", lambda g: g.tensor_tensor(out=tmpm[:, hfs], in0=pg[hf][:], in1=g5[r][:, hfs], op=ALU.mult), R=[pg[hf], g5[r]], W=[tmpm])
                        k.I("dve", lambda g: g.tensor_tensor(out=xt[:], in0=xt[:], in1=tmpm[:], op=ALU.add), R=[xt, tmpm], W=[xt])
                        if not final:
                            k.dma(xres.ap[rows, :], xt[:], R=[xt], W=[xres.b[t]], q="act")
                        else:
                            k.I("act", lambda g: g.activation(out=sqm[:], in_=xt[:], func=AF.Square, accum_out=ss[:]), R=[xt], W=[sqm, ss])
                            k.I("act", lambda g: g.activation(out=ss[:], in_=ss[:], func=AF.Sqrt, bias=epsT[:], scale=1.0 / D), R=[ss, epsT], W=[ss])
                            k.I("dve", lambda g: g.reciprocal(out=ss[:], in_=ss[:]), R=[ss], W=[ss])
                            k.I("dve", lambda g: g.scalar_tensor_tensor(out=tmpm[:], in0=xt[:], scalar=ss[:, 0:1], in1=fgrow[:],
                                                                       op0=ALU.mult, op1=ALU.mult), R=[xt, ss, fgrow], W=[tmpm])
                            k.dma(out_d.ap[(t - 2) * 128:(t - 1) * 128, :], tmpm[:], R=[tmpm], W=[out_d.b[t - 2]], q="act")
        moe_stage = moe_sparse
        moe_stage(0, True, stop == "F0final")
        k.mark("F0")
        if stop in ("F", "F0final"):
            k.barrier()
            return nc, outs

        with k.scope():
            W = k.sb("swin", [128, 8, 6208], BF16)
            load_w_bf16(W, swin_d, 8, 6208)
            dtb = k.sb("dtb", [128, 64])
            k.dma(dtb[:], sdtb_d.partition_broadcast(128), W=[dtb])
            arow = k.sb("arow", [128, 64])
            k.dma(arow[:], salog_d.partition_broadcast(128), W=[arow])
            k.I("act", lambda g: g.activation(out=arow[:], in_=arow[:], func=AF.Exp), R=[arow], W=[arow])
            k.I("dve", lambda g: g.tensor_scalar(out=arow[:], in0=arow[:], scalar1=-1.0, scalar2=None, op0=ALU.mult), R=[arow], W=[arow])
            xt_ = [k.sb("xt", [128, D]) for _ in range(2)]
            sq = k.sb("sq", [128, D])
            ss_ = [k.sb("ss", [128, 1]) for _ in range(2)]
            xn_ = [k.sb("xn", [128, D], BF16) for _ in range(2)]
            tps_ = [k.ps("tps", [128, D], BF16) for _ in range(2)]
            hT_ = [k.sb("hT", [128, 8, 512], BF16) for _ in range(2)]
            pj = [k.ps("pj", [128, 512]) for _ in range(4)]
            ob16 = [k.sb("ob16", [128, 512], BF16) for _ in range(4)]
            dts_ = [k.sb("dts", [128, 64]) for _ in range(2)]
            dta_ = [k.sb("dtas", [128, 64]) for _ in range(2)]
            cnt = {"x": 0, "pj": 0, "ob": 0}

            def nxt(lst, key):
                v = lst[cnt[key] % len(lst)]
                cnt[key] += 1
                return v
            for gi, (t0, ng, r) in enumerate(groups()):
                hT = hT_[gi % 2]
                T = ng * 128
                for ti in range(ng):
                    t = t0 + ti
                    xt = nxt(xt_, "x")
                    i2 = cnt["x"] % 2
                    k.dma(xt[:], xres.ap[t * 128:(t + 1) * 128, :], R=[xres.b[t]], W=[xt])
                    norm_tile(xt, 1, 0, r, hT, ti * 128, (sq, ss_[i2], xn_[i2], tps_[i2]), None, identb, identb[:])
                for cc in range(32):
                    p = nxt(pj, "pj")
                    c0 = 2048 + cc * 128
                    for j in range(8):
                        k.mm(p, p[:, 0:T], W, W[:, j, c0:c0 + 128], hT, hT[:, j, 0:T], start=(j == 0), stop=(j == 7))
                    o = nxt(ob16, "ob")
                    if cc % 2 == 0:
                        k.I("act", lambda g: g.activation(out=o[:, 0:T], in_=p[:, 0:T], func=AF.Copy), R=[p], W=[o])
                    else:
                        k.I("dve", lambda g: g.tensor_copy(out=o[:, 0:T], in_=p[:, 0:T]), R=[p], W=[o])
                    k.dma(uT.ap[cc * 128:(cc + 1) * 128, t0 * 128:t0 * 128 + T], o[:, 0:T], R=[o], W=uT.b[t0:t0 + ng])
                for ti in range(ng):
                    t = t0 + ti
                    rows = slice(t * 128, (t + 1) * 128)
                    if r == 0:
                        for cb in range(4):
                            p = nxt(pj, "pj")
                            for j in range(8):
                                k.mm(p, p[:], hT, hT[:, j, ti * 128:(ti + 1) * 128], W, W[:, j, cb * 512:(cb + 1) * 512],
                                     start=(j == 0), stop=(j == 7))
                            o = nxt(ob16, "ob")
                            k.I("act", lambda g: g.activation(out=o[:], in_=p[:], func=AF.Silu), R=[p], W=[o])
                            k.dma(zs.ap[rows, cb * 512:(cb + 1) * 512], o[:], R=[o], W=[zs.b[t]])
                    p = nxt(pj, "pj")
                    for j in range(8):
                        k.mm(p, p[:, 0:64], hT, hT[:, j, ti * 128:(ti + 1) * 128], W, W[:, j, 6144:6208], start=(j == 0), stop=(j == 7))
                    dts, dtas = dts_[t % 2], dta_[t % 2]
                    k.I("dve", lambda g: g.tensor_tensor(out=dts[:], in0=p[:, 0:64], in1=dtb[:], op=ALU.add), R=[p, dtb], W=[dts])
                    k.I("act", lambda g: g.activation(out=dts[:], in_=dts[:], func=AF.Exp), R=[dts], W=[dts])
                    k.I("act", lambda g: g.activation(out=dts[:], in_=dts[:], func=AF.Ln, bias=C("ones")[:, 0:1], scale=1.0), R=[dts, cst], W=[dts])
                    k.I("dve", lambda g: g.tensor_tensor(out=dtas[:], in0=dts[:], in1=arow[:], op=ALU.mult), R=[dts, arow], W=[dtas])
                    k.dma(dtd.ap[rows, :], dts[:], R=[dts], W=[dtd.b[t]])
                    k.dma(dtad.ap[rows, :], dtas[:], R=[dtas], W=[dtad.b[t]])
        k.mark("G")
        if stop == "G":
            k.barrier()
            return nc, outs

        with k.scope():
            cw = k.sb("cw", [128, 32, 5])
            k.dma(cw[:], scw_d, W=[cw])
            cbv = k.sb("cbv", [128, 32])
            k.dma(cbv[:], scb_d, W=[cbv])
            Dg = [k.sb("Dg", [128, 5, 128], BF16) for _ in range(2)]
            ub = [k.sb("ub", [128, 516], BF16) for _ in range(2)]
            yb16 = [k.sb("yb16", [128, 512], BF16) for _ in range(2)]
            tm = [k.sb("tm", [128, 4, 128], BF16) for _ in range(2)]
            psc = [k.ps("psc", [128, 512]) for _ in range(2)]
            pst = [k.ps("pst", [128, 512], BF16) for _ in range(2)]
            its = [(cc, tok0, ntok, g0) for cc in range(32) for (tok0, ntok) in ((0, CT), (CT, S)) for g0 in range(0, ntok, 512)]

            def hload(i):
                cc, tok0, ntok, g0 = its[i]
                u = ub[i % 2]
                Tg = min(512, ntok - g0)
                lo, hi = g0 - 2, g0 + Tg + 2
                clo, chi = max(lo, 0), min(hi, ntok)
                if clo != lo or chi != hi:
                    k.I("pool", lambda g: g.memset(u[:], 0.0), W=[u])
                tl0, tl1 = (tok0 + clo) // 128, (tok0 + chi - 1) // 128 + 1
                k.dma(u[:, clo - lo:chi - lo], uT.ap[cc * 128:(cc + 1) * 128, tok0 + clo:tok0 + chi], R=uT.b[tl0:tl1], W=[u])
            hload(0)
            for i, (cc, tok0, ntok, g0) in enumerate(its):
                Dt = Dg[cc % 2]
                if i == 0 or its[i - 1][0] != cc:
                    for tap in range(5):
                        k.I("dve", lambda g: g.tensor_scalar(out=Dt[:, tap, :], in0=identb[:], scalar1=cw[:, cc, tap:tap + 1], scalar2=None,
                                                             op0=ALU.mult), R=[identb, cw], W=[Dt])
                Tg = min(512, ntok - g0)
                i2 = i % 2
                u, y, tmb, p, pt = ub[i2], yb16[i2], tm[i2], psc[i2], pst[i2]
                for tap in range(5):
                    k.mm(p, p[:, 0:Tg], Dt, Dt[:, tap, :], u, u[:, tap:tap + Tg], start=(tap == 0), stop=(tap == 4))
                k.I("act", lambda g: g.activation(out=y[:, 0:Tg], in_=p[:, 0:Tg], func=AF.Silu, bias=cbv[:, cc:cc + 1], scale=1.0),
                    R=[p, cbv], W=[y])
                tb0, nb = (tok0 + g0) // 128, Tg // 128
                if cc < 24:
                    for q in range(nb):
                        k.tr(pt, pt[:, q * 128:(q + 1) * 128], y, y[:, q * 128:(q + 1) * 128], identb, identb[:])
                    k.I("dve", lambda g: g.tensor_copy(out=tmb[:, 0:nb, :], in_=pt[:, 0:nb * 128].rearrange("p (q c) -> p q c", q=nb)),
                        R=[pt], W=[tmb])
                if i + 1 < len(its):
                    hload(i + 1)
                if cc >= 16:
                    k.dma(bcT.ap[(cc - 16) * 128:(cc - 15) * 128, tok0 + g0:tok0 + g0 + Tg], y[:, 0:Tg], R=[y], W=bcT.b[tb0:tb0 + nb], q="act")
                if cc < 24:
                    k.dma(xB.ap[tok0 + g0:tok0 + g0 + Tg, cc * 128:(cc + 1) * 128].rearrange("(q p) c -> p q c", p=128),
                          tmb[:, 0:nb, :], R=[tmb], W=xB.b[tb0:tb0 + nb])
        k.mark("H")
        if stop == "H":
            k.barrier()
            return nc, outs

        def ssd_pass(d):
            with k.scope():
                Uinc = C("Uf") if d == 0 else C("Ub")
                ds0 = d * 32
                dsl = slice(ds0, ds0 + 32)
                hst = k.sb("hst", [128, 2048])
                hstb = k.sb("hstb", [128, 2048], BF16)
                k.I("pool", lambda g: g.memset(hst[:], 0.0), W=[hst])
                k.I("pool", lambda g: g.memset(hstb[:], 0.0), W=[hstb])
                maskneg = k.sb("maskneg", [128, 128])
                k.I("dve", lambda g: g.tensor_scalar(out=maskneg[:], in0=Uinc, scalar1=-1.0, scalar2=30000.0, op0=ALU.add, op1=ALU.mult),
                    R=[cst], W=[maskneg])
                dta_ = [k.sb("dta", [128, 64]) for _ in range(2)]
                dtt_ = [k.sb("dtt", [128, 64]) for _ in range(2)]
                xBt_ = [k.sb("xBt", [128, 3072], BF16) for _ in range(2)]
                bct_ = [k.sb("bct", [128, 16, 128], BF16) for _ in range(2)]
                yft_ = [k.sb("yft", [128, 2048]) for _ in range(2)]
                ytl_ = [k.sb("ytl", [128, 2048]) for _ in range(2)]
                psA2 = k.ps("psA2", [128, 64])
                psG = [k.ps("psG", [128, 128]) for _ in range(2)]
                psAr = [k.ps("psAr", [128, 512]) for _ in range(2)]
                psY = [k.ps("psYs", [128, 256]) for _ in range(1)]
                psYo = k.ps("psYo", [128, 256])
                psH = k.ps("psH", [128, 256])
                eac_ = [k.sb("eac", [128, 32]) for _ in range(2)]
                yo_ = [k.sb("yo", [128, 256]) for _ in range(2)]
                nac_ = [k.sb("nac", [128, 32]) for _ in range(2)]
                wend_ = [k.sb("wend", [128, 32]) for _ in range(2)]
                dlall_ = [k.sb("dlall", [128, 32]) for _ in range(2)]
                xd_ = [k.sb("xd", [128, 2048], BF16) for _ in range(2)]
                xw_ = [k.sb("xw", [128, 2048], BF16) for _ in range(2)]
                rhs4_ = [k.sb("rhs4", [128, 4, 128]) for _ in range(2)]
                nacM_ = [k.sb("nacM", [128, 4, 128]) for _ in range(2)]
                t4_ = [k.sb("t4", [128, 4, 128]) for _ in range(2)]
                dec_ = [k.sb("dec4", [128, 4, 128]) for _ in range(2)]
                erow_ = [k.sb("erow4", [128, 4, 128]) for _ in range(2)]
                MT_ = [k.sb("MT4", [128, 4, 128], BF16) for _ in range(2)]
                CsT_ = [k.sb("CsT4", [128, 4, 128], BF16) for _ in range(2)]
                htmp_ = [k.sb("htmp", [128, 256]) for _ in range(2)]
                order = list(range(NTT)) if d == 0 else [1, 0] + list(range(NTT - 1, 1, -1))

                def pre(it, t):
                    b2 = it % 2
                    lat = t >= 2
                    rows = slice(t * 128, (t + 1) * 128)
                    dta, dtt, xBt, bct, yft = dta_[b2], dtt_[b2], xBt_[b2], bct_[b2], yft_[b2]
                    nac, wend, dlall, xd, xw = nac_[b2], wend_[b2], dlall_[b2], xd_[b2], xw_[b2]
                    k.dma(dta[:], dtad.ap[rows, :], R=[dtad.b[t]], W=[dta])
                    k.dma(dtt[:], dtd.ap[rows, :], R=[dtd.b[t]], W=[dtt])
                    k.dma(xBt[:], xB.ap[rows, :], R=[xB.b[t]], W=[xBt])
                    if lat:
                        k.dma(bct[:], bcT.ap[:, rows].rearrange("(c p) t -> p c t", p=128), R=[bcT.b[t]], W=[bct])
                    if d == 1 and lat:
                        k.dma(yft[:], yf.ap[rows, :], R=[yf.b[t]], W=[yft])
                    k.mm(psA2, psA2[:, 0:32], cst, Uinc, dta, dta[:, dsl])
                    k.mm(psA2, psA2[:, 32:64], cst, C("ones"), dta, dta[:, dsl])
                    k.I("act", lambda g: g.activation(out=nac[:], in_=psA2[:, 0:32], func=AF.Copy, scale=-1.0), R=[psA2], W=[nac])
                    k.I("dve", lambda g: g.tensor_tensor(out=wend[:], in0=psA2[:, 32:64], in1=nac[:], op=ALU.add), R=[psA2, nac], W=[wend])
                    k.I("act", lambda g: g.activation(out=wend[:], in_=wend[:], func=AF.Exp), R=[wend], W=[wend])
                    k.I("dve", lambda g: g.tensor_tensor(out=wend[:], in0=wend[:], in1=dtt[:, dsl], op=ALU.mult), R=[wend, dtt], W=[wend])
                    k.I("act", lambda g: g.activation(out=dlall[:], in_=psA2[:, 32:64], func=AF.Exp), R=[psA2], W=[dlall])
                    if lat:
                        eac = eac_[b2]
                        k.I("act", lambda g: g.activation(out=eac[:], in_=psA2[:, 0:32], func=AF.Exp), R=[psA2], W=[eac])
                    x3 = xBt[:, 0:2048].rearrange("p (h e) -> p h e", h=32)
                    if lat:
                        k.I("pool", lambda g: g.tensor_tensor(out=xd[:].rearrange("p (h e) -> p h e", h=32), in0=x3,
                                                              in1=dtt[:, dsl].unsqueeze(2).to_broadcast([128, 32, 64]), op=ALU.mult),
                            R=[xBt, dtt], W=[xd])
                    k.I("dve", lambda g: g.tensor_tensor(out=xw[:].rearrange("p (h e) -> p h e", h=32), in0=x3,
                                                         in1=wend[:].unsqueeze(2).to_broadcast([128, 32, 64]), op=ALU.mult),
                        R=[xBt, wend], W=[xw])

                def s1(u, it, t, gr):
                    if t < 2:
                        return
                    b2, i2 = it % 2, u % 2
                    dta, bct, nac = dta_[b2], bct_[b2], nac_[b2]
                    pG, pAr, rhs4, nacM = psG[i2], psAr[i2], rhs4_[i2], nacM_[i2]
                    hs4 = slice(gr * 4, gr * 4 + 4)
                    k.mm(pG, pG[:], bct, bct[:, gr, :], bct, bct[:, 8 + gr, :])
                    k.I("dve", lambda g: g.tensor_tensor(out=rhs4[:], in0=Uinc.unsqueeze(1).to_broadcast([128, 4, 128]),
                                                         in1=dta[:, ds0 + gr * 4:ds0 + gr * 4 + 4].unsqueeze(2).to_broadcast([128, 4, 128]),
                                                         op=ALU.mult), R=[cst, dta], W=[rhs4])
                    k.I("pool", lambda g: g.tensor_tensor(out=nacM[:], in0=maskneg[:].unsqueeze(1).to_broadcast([128, 4, 128]),
                                                          in1=nac[:, hs4].unsqueeze(2).to_broadcast([128, 4, 128]), op=ALU.add),
                        R=[maskneg, nac], W=[nacM])
                    k.mm(pAr, pAr[:], cst, C("ones"), rhs4, rhs4[:].rearrange("p a b -> p (a b)"))

                def s2(u, it, t, gr):
                    b2, i2 = it % 2, u % 2
                    lat = t >= 2
                    rows = slice(t * 128, (t + 1) * 128)
                    xBt, bct, yft, ytl = xBt_[b2], bct_[b2], yft_[b2], ytl_[b2]
                    dlall, xd, xw = dlall_[b2], xd_[b2], xw_[b2]
                    gc = slice(gr * 256, (gr + 1) * 256)
                    hs4 = slice(gr * 4, gr * 4 + 4)
                    if lat:
                        pG, pAr, pY = psG[i2], psAr[i2], psY[0]
                        nacM, t4, dec, MT = nacM_[i2], t4_[i2], dec_[i2], MT_[i2]
                        eac, yo = eac_[b2], yo_[i2]
                        pAr3 = pAr[:].rearrange("p (a b) -> p a b", a=4)
                        k.I("dve", lambda g: g.tensor_tensor(out=t4[:], in0=pAr3, in1=nacM[:], op=ALU.add), R=[pAr, nacM], W=[t4])
                        k.I("act", lambda g: g.activation(out=dec[:], in_=t4[:], func=AF.Exp), R=[t4], W=[dec])
                        k.mm(psYo, psYo[:], bct, bct[:, 8 + gr, :], hstb, hstb[:, gc])
                        k.I("dve", lambda g: g.tensor_tensor(out=yo[:].rearrange("p (a b) -> p a b", a=4),
                                                             in0=psYo[:].rearrange("p (a b) -> p a b", a=4),
                                                             in1=eac[:, hs4].unsqueeze(2).to_broadcast([128, 4, 64]), op=ALU.mult),
                            R=[psYo, eac], W=[yo])
                        if d == 1:
                            k.I("pool", lambda g: g.tensor_tensor(out=yo[:], in0=yo[:], in1=yft[:, gc], op=ALU.add), R=[yo, yft], W=[yo])
                        k.I("dve", lambda g: g.tensor_tensor(out=MT[:], in0=dec[:], in1=pG[:].unsqueeze(1).to_broadcast([128, 4, 128]), op=ALU.mult),
                            R=[dec, pG], W=[MT])
                        for hh in range(4):
                            h = gr * 4 + hh
                            k.mm(pY, pY[:, hh * 64:(hh + 1) * 64], MT, MT[:, hh, :], xd, xd[:, h * 64:(h + 1) * 64], start=True, stop=True)
                        k.I("dve", lambda g: g.tensor_tensor(out=ytl[:, gc], in0=pY[:], in1=yo[:], op=ALU.add), R=[pY, yo], W=[ytl])
                    htmp = htmp_[gr % 2]
                    k.mm(psH, psH[:], xBt, xBt[:, 2048 + gr * 128:2048 + (gr + 1) * 128], xw, xw[:, gc])
                    k.I("pool", lambda g: g.tensor_tensor(out=htmp[:].rearrange("p (a b) -> p a b", a=4),
                                                          in0=hst[:, gc].rearrange("p (a b) -> p a b", a=4),
                                                          in1=dlall[:, hs4].unsqueeze(2).to_broadcast([128, 4, 64]), op=ALU.mult),
                        R=[hst, dlall], W=[htmp])
                    k.I("dve", lambda g: g.tensor_tensor(out=hst[:, gc], in0=htmp[:], in1=psH[:], op=ALU.add), R=[htmp, psH], W=[hst])
                    k.I("act", lambda g: g.activation(out=hstb[:, gc], in_=hst[:, gc], func=AF.Copy), R=[hst], W=[hstb])
                    if lat and gr == 7:
                        k.dma(yf.ap[rows, :], ytl[:], R=[ytl], W=[yf.b[t]], q=("act" if d == 0 else "sp"))
                units = [(it, t, gr) for it, t in enumerate(order) for gr in range(8)]
                pre(0, order[0])
                s1(0, *units[0])
                for u, (it, t, gr) in enumerate(units):
                    if gr == 3 and it + 1 < len(order):
                        pre(it + 1, order[it + 1])
                    if u + 1 < len(units):
                        nit, nt_, ngr = units[u + 1]
                        s1(u + 1, nit, nt_, ngr)
                    s2(u, it, t, gr)
        ssd_pass(0)
        ssd_pass(1)
        k.mark("I")
        if stop == "I":
            k.barrier()
            return nc, outs

        with k.scope():
            Wso = k.sb("wso", [128, 16, D], BF16)
            load_w_bf16(Wso, swout_d, 16, D)
            P = alloc_post(1, False)
            dsk = k.sb("dsk", [128, 32])
            k.dma(dsk[:], sd_d.partition_broadcast(128), W=[dsk])
            ngrow = k.sb("ngrow", [128, 2048])
            k.dma(ngrow[:], sng_d.partition_broadcast(128), W=[ngrow])
            yt_ = [k.sb("yt", [128, 2048]) for _ in range(2)]
            xg_ = [k.sb("xg", [128, 2048], BF16) for _ in range(2)]
            zt_ = [k.sb("zt", [128, 2048], BF16) for _ in range(2)]
            tmpk = k.sb("tmpk", [128, 2048])
            junk = k.sb("junkk", [128, 256])
            ssq_ = [k.sb("ssqk", [128, 8]) for _ in range(2)]
            yn_ = [k.sb("yn", [128, 2048], BF16) for _ in range(2)]
            yT_ = [k.sb("yT", [128, 16, 128], BF16) for _ in range(2)]
            psM_ = [k.ps("psM2", [128, D], BF16) for _ in range(2)]
            psY = [k.ps("psY2", [128, 512]) for _ in range(2)]
            def k1(t):
                b2 = t % 2
                rows = slice(t * 128, (t + 1) * 128)
                yt, xg, zt, ssq, yn, yT = yt_[b2], xg_[b2], zt_[b2], ssq_[b2], yn_[b2], yT_[b2]
                k.dma(yt[:], yf.ap[rows, :], R=[yf.b[t]], W=[yt])
                k.dma(xg[:], xB.ap[rows, 0:2048], R=[xB.b[t]], W=[xg])
                k.dma(zt[:], zs.ap[rows, :], R=[zs.b[t]], W=[zt])
                k.I("dve", lambda g: g.tensor_tensor(out=tmpk[:].rearrange("p (h e) -> p h e", h=32),
                                                     in0=xg[:].rearrange("p (h e) -> p h e", h=32),
                                                     in1=dsk[:].unsqueeze(2).to_broadcast([128, 32, 64]), op=ALU.mult), R=[xg, dsk], W=[tmpk])
                k.I("pool", lambda g: g.tensor_tensor(out=yt[:], in0=yt[:], in1=tmpk[:], op=ALU.add), R=[yt, tmpk], W=[yt])
                k.I("pool", lambda g: g.tensor_tensor(out=yt[:], in0=yt[:], in1=zt[:], op=ALU.mult), R=[yt, zt], W=[yt])
                for g8 in range(8):
                    gc = slice(g8 * 256, (g8 + 1) * 256)
                    k.I("act", lambda g: g.activation(out=junk[:], in_=yt[:, gc], func=AF.Square, accum_out=ssq[:, g8:g8 + 1]), R=[yt], W=[junk, ssq])
                k.I("act", lambda g: g.activation(out=ssq[:], in_=ssq[:], func=AF.Sqrt, bias=epsT[:], scale=1.0 / 256), R=[ssq, epsT], W=[ssq])
                k.I("dve", lambda g: g.reciprocal(out=ssq[:], in_=ssq[:]), R=[ssq], W=[ssq])
                for g8 in range(8):
                    gc = slice(g8 * 256, (g8 + 1) * 256)
                    k.I("dve", lambda g: g.scalar_tensor_tensor(out=yn[:, gc], in0=yt[:, gc], scalar=ssq[:, g8:g8 + 1], in1=ngrow[:, gc],
                                                               op0=ALU.mult, op1=ALU.mult), R=[yt, ssq, ngrow], W=[yn])
                for half in range(2):
                    psM = psM_[half]
                    for j in range(8):
                        jj = half * 8 + j
                        k.tr(psM, psM[:, j * 128:(j + 1) * 128], yn, yn[:, jj * 128:(jj + 1) * 128], identb, identb[:])
                    if half == 0:
                        k.I("act", lambda g: g.activation(out=yT[:, 0:8, :], in_=psM[:].rearrange("p (j c) -> p j c", j=8), func=AF.Copy),
                            R=[psM], W=[yT])
                    else:
                        k.I("dve", lambda g: g.tensor_copy(out=yT[:, 8:16, :], in_=psM[:].rearrange("p (j c) -> p j c", j=8)),
                            R=[psM], W=[yT])

            def k2(t):
                yT = yT_[t % 2]
                for hf in range(2):
                    for j in range(16):
                        k.mm(psY[hf], psY[hf][:], yT, yT[:, j, :], Wso, Wso[:, j, hf * 512:(hf + 1) * 512], start=(j == 0), stop=(j == 15))
                pp2(P, 1, t, 0, psY, xres)
            run_pipe(list(range(2, NTT)), k1, k2, lambda t: pp3(P, 1, t, 0), lambda t: pp4(P, 1, t))
        k.mark("K")
        if stop == "K":
            k.barrier()
            return nc, outs
        moe_stage(1, False, True)

        k.barrier()
        k.mark("F1")
        MARKS[:] = k.marks
        print("instr", k.n_instr, "waits", k.n_wait)
    return nc, outs


def prep_inputs(inp, b, S):
    m = {}
    m["xin"] = np.ascontiguousarray(np.concatenate([inp["ctx"][b], inp["x"][b, :S]], 0), np.float32)
    m["cst"] = CONST_ARR
    m["scol"] = np.ascontiguousarray(np.stack([col_layout(inp["c"][b]), col_layout(inp["c_ctx"])], -1))
    m["ada_w"] = np.ascontiguousarray(inp["ada_w"], np.float32)
    m["adab_col"] = np.ascontiguousarray(np.stack([col_layout(inp["ada_b"][l]) for l in range(2)], 1))
    m["ng_col"] = np.ascontiguousarray(
        np.stack([np.stack([col_layout(inp["norm_g"][l, w]) for w in range(2)], 1) for l in range(2)], 1))
    m["final_g"] = np.ascontiguousarray(inp["final_g"], np.float32)
    m["ab_w_in"] = np.ascontiguousarray(inp["ab_w_in"][0], np.float32)
    m["ab_w_out"] = np.ascontiguousarray(inp["ab_w_out"][0], np.float32)
    m["lb_logits"] = np.ascontiguousarray(inp["hgrn_lb_logits"], np.float32)
    m["onorm_g"] = np.ascontiguousarray(inp["hgrn_onorm_g"][0], np.float32)
    m["na_bias"] = na_bias_tables(np.asarray(inp["na_rpb"][0], np.float32), S // GW)
    m["ssd_w_in"] = np.ascontiguousarray(inp["ssd_w_in"][0], np.float32)
    cw = np.asarray(inp["ssd_conv_w"][0], np.float32)
    m["convw_col"] = np.ascontiguousarray(np.stack([col_layout(cw[t]) for t in range(5)], -1))
    m["convb_col"] = col_layout(inp["ssd_conv_b"][0])
    m["ssd_a_log"] = np.ascontiguousarray(inp["ssd_a_log"][0].reshape(64), np.float32)
    m["ssd_dt_bias"] = np.ascontiguousarray(inp["ssd_dt_bias"][0].reshape(64), np.float32)
    m["ssd_d"] = np.ascontiguousarray(inp["ssd_d"][0], np.float32)
    m["ssd_norm_g"] = np.ascontiguousarray(inp["ssd_norm_g"][0], np.float32)
    m["ssd_w_out"] = np.ascontiguousarray(inp["ssd_w_out"][0], np.float32)
    m["moe_router"] = np.ascontiguousarray(inp["moe_router"], np.float32)
    m["moe_w1"] = np.ascontiguousarray(inp["moe_w1"], np.float32)
    m["moe_w3"] = np.ascontiguousarray(inp["moe_w3"], np.float32)
    m["moe_w2"] = np.ascontiguousarray(inp["moe_w2"], np.float32)
    sel = np.zeros((32, 32, 128), np.float32)
    for h in range(32):
        sel[h, h, :] = 1.0
    m["sel32"] = sel.reshape(32, 32 * 128)
    m["norm_g_nat"] = np.ascontiguousarray(inp["norm_g"], np.float32)
    return m


def kernel(**inputs):
    S = inputs["x"].shape[1]
    nc, outs = build(S)
    maps = [prep_inputs(inputs, c % 2, S) for c in range(2)]
    idle = {kk: (v if kk in ("cst", "sel32") else np.zeros_like(v)) for kk, v in maps[0].items()}
    in_maps = [maps[c] if c < 2 else idle for c in range(8)]
    res = run_bass_kernel_spmd(nc, in_maps, core_ids=list(range(8)))
    return np.stack([res.results[0]["out"], res.results[1]["out"]], 0).astype(np.float32)
```

```python
import contextlib
import numpy as np
import concourse.bass as bass
import concourse.mybir as mybir
from concourse.bass_utils import run_bass_kernel_spmd

F32 = mybir.dt.float32
BF16 = mybir.dt.bfloat16
I32 = mybir.dt.int32
AF = mybir.ActivationFunctionType
ALU = mybir.AluOpType
AX = mybir.AxisListType

D = 1024
CT = 256
GW = 64
EPS = 1e-6
NEG = -1e30


class Buf:
    __slots__ = ("name", "t", "w", "r")

    def __init__(self, name, t=None):
        self.name = name
        self.t = t
        self.w = None
        self.r = {}

    def __getitem__(self, idx):
        return self.t[idx]


class K:
    NQ = 8

    def __init__(self, nc, stack):
        self.nc = nc
        self.stack = stack
        self.cur = stack
        self.eng = {"pe": nc.tensor, "dve": nc.vector, "act": nc.scalar,
                    "pool": nc.gpsimd, "sp": nc.sync}
        self.sem = {}
        self.cnt = {}
        for e in ("pe", "dve", "act", "pool"):
            self.sem[e] = stack.enter_context(nc.semaphore("s_" + e))
            self.cnt[e] = 0
        for i in range(self.NQ):
            self.sem[("d", i)] = stack.enter_context(nc.semaphore("s_d%d" % i))
            self.cnt[("d", i)] = 0
        for i in range(self.NQ):
            self.sem[("g", i)] = stack.enter_context(nc.semaphore("s_g%d" % i))
            self.cnt[("g", i)] = 0
        for i in range(self.NQ):
            self.sem[("a", i)] = stack.enter_context(nc.semaphore("s_a%d" % i))
            self.cnt[("a", i)] = 0
        self.known = {e: {} for e in self.eng}
        self.adma_i = 0
        self.idma_i = 0
        self.dma_i = 0
        self.n_instr = 0
        self.n_wait = 0
        self._uid = 0
        self.marks = []

    def sb(self, name, shape, dt=F32):
        self._uid += 1
        t = self.cur.enter_context(self.nc.sbuf_tensor("%s_%d" % (name, self._uid), list(shape), dt))
        return Buf(name, t)

    def ps(self, name, shape, dt=F32):
        self._uid += 1
        t = self.cur.enter_context(self.nc.psum_tensor("%s_%d" % (name, self._uid), list(shape), dt))
        return Buf(name, t)

    def mark(self, name):
        self.marks.append((name, self.cnt["pe"], self.cnt["act"]))

    @contextlib.contextmanager
    def scope(self):
        prev = self.cur
        with contextlib.ExitStack() as st:
            self.cur = st
            yield
            self.barrier()
        self.cur = prev

    def barrier(self):
        for e in self.eng:
            kn = self.known[e]
            for key, v in self.cnt.items():
                if v > 0 and kn.get(key, 0) < v and not (key == e):
                    self.eng[e].wait_ge(self.sem[key], v)
                    kn[key] = v
                    self.n_wait += 1

    def _need(self, e, R, W):
        need = {}

        def add(ev):
            if ev is None:
                return
            k, v = ev
            if need.get(k, 0) < v:
                need[k] = v
        for b in R:
            add(b.w)
        for b in W:
            add(b.w)
            for k, v in b.r.items():
                add((k, v))
        kn = self.known[e]
        for k, v in need.items():
            if k == e and e == "pe":
                continue
            if kn.get(k, 0) >= v:
                continue
            self.eng[e].wait_ge(self.sem[k], v)
            self.n_wait += 1
            kn[k] = v

    def _done(self, ev, R, W):
        k, v = ev
        for b in R:
            if b.r.get(k, 0) < v:
                b.r[k] = v
        for b in W:
            b.w = ev
            b.r = {}

    def I(self, e, fn, R=(), W=()):
        self._need(e, R, W)
        ins = fn(self.eng[e])
        self.cnt[e] += 1
        ins.then_inc(self.sem[e], 1)
        self.n_instr += 1
        self._done((e, self.cnt[e]), R, W)
        return ins

    def dma(self, out, in_, R=(), W=(), q="sp", **kw):
        e = q
        if q == "sp":
            i = self.dma_i % self.NQ
            self.dma_i += 1
            key = ("d", i)
        else:
            i = self.adma_i % self.NQ
            self.adma_i += 1
            key = ("a", i)
        kn = self.known[e]
        if kn.get(key, 0) < self.cnt[key]:
            self.eng[e].wait_ge(self.sem[key], self.cnt[key])
            kn[key] = self.cnt[key]
        self._need(e, R, W)
        ins = self.eng[e].dma_start(out=out, in_=in_, **kw)
        self.cnt[key] += 16
        ins.then_inc(self.sem[key], 16)
        self.n_instr += 1
        self._done((key, self.cnt[key]), R, W)
        return ins

    def idma(self, R=(), W=(), **kw):
        e = "pool"
        i = self.idma_i % self.NQ
        self.idma_i += 1
        key = ("g", i)
        kn = self.known[e]
        if kn.get(key, 0) < self.cnt[key]:
            self.eng[e].wait_ge(self.sem[key], self.cnt[key])
            kn[key] = self.cnt[key]
        self._need(e, R, W)
        ins = self.eng[e].indirect_dma_start(**kw)
        self.cnt[key] += 16
        ins.then_inc(self.sem[key], 16)
        self.n_instr += 1
        self._done((key, self.cnt[key]), R, W)
        return ins

    def gdma(self, out, in_, R=(), W=()):
        e = "pool"
        i = self.idma_i % self.NQ
        self.idma_i += 1
        key = ("g", i)
        kn = self.known[e]
        if kn.get(key, 0) < self.cnt[key]:
            self.eng[e].wait_ge(self.sem[key], self.cnt[key])
            kn[key] = self.cnt[key]
        self._need(e, R, W)
        ins = self.eng[e].dma_start(out=out, in_=in_)
        self.cnt[key] += 16
        ins.then_inc(self.sem[key], 16)
        self.n_instr += 1
        self._done((key, self.cnt[key]), R, W)
        return ins

    def mm(self, ps, out, lb, lhsT, rb, rhs, start=True, stop=True):
        R = [lb, rb] if lb is not rb else [lb]
        return self.I("pe", lambda g: g.matmul(out, lhsT, rhs, start=start, stop=stop), R=R, W=[ps])

    def tr(self, ps, out, ib, in_, idb, ident):
        return self.I("pe", lambda g: g.transpose(out, in_, ident), R=[ib, idb], W=[ps])


class DT:
    def __init__(self, nc, name, shape, dt, ntile, kind="Internal"):
        self.t = nc.dram_tensor(name, list(shape), dt, kind=kind)
        self.ap = self.t.ap()
        self.b = [Buf("%s@%d" % (name, i)) for i in range(ntile)]
        self.all = self.b


def col_layout(v):
    v = np.asarray(v, np.float32)
    return np.ascontiguousarray(v.reshape(-1, 128).T)


CST = {}
MARKS = []


def make_consts():
    s = np.arange(128)[:, None]
    l = np.arange(128)[None, :]
    c = {}
    c["ident"] = np.eye(128, dtype=np.float32)
    c["Uf"] = (s <= l).astype(np.float32)
    c["Ub"] = (s >= l).astype(np.float32)
    c["Usf"] = (s > l).astype(np.float32)
    c["Usb"] = (s < l).astype(np.float32)
    midf = (s <= 63).astype(np.float32) * np.ones((1, 128), np.float32)
    midb = (s >= 64).astype(np.float32) * np.ones((1, 128), np.float32)
    c["R1f"] = np.concatenate([c["Uf"] - midf, midf[:, :1], 1.0 - midf[:, :1], np.zeros((128, 126), np.float32)], 1)
    c["R1b"] = np.concatenate([c["Ub"] - midb, midb[:, :1], 1.0 - midb[:, :1], np.zeros((128, 126), np.float32)], 1)
    c["ones"] = np.ones((128, 128), np.float32)
    c["G16"] = ((s % 16) == (l % 16)).astype(np.float32)
    names = list(c)
    off = {}
    o = 0
    for n in names:
        off[n] = (o, c[n].shape[1])
        o += c[n].shape[1]
    arr = np.concatenate([c[n] for n in names], 1)
    return arr, off


CONST_ARR, CONST_OFF = make_consts()


def na_bias_tables(rpb, rows):
    nt = rows // 2
    out = np.full((5, 8, 128, 896), NEG, np.float32)
    out[:, :, :, 640:] = 0.0
    qcol = np.arange(64)
    win0 = np.clip(qcol - 8, 0, 48)
    for cls, ti in enumerate([2, 0, 1, nt - 2, nt - 1]):
        c0 = min(max(ti - 2, 0), nt - 5)
        for rr in range(2):
            r = 2 * ti + rr
            r0 = min(max(r - 4, 0), rows - 8)
            for kr in range(10):
                gr = 2 * c0 + kr
                if gr < r0 or gr >= r0 + 8:
                    continue
                dr = gr - r + 7
                for c in range(64):
                    cc = np.arange(win0[c], win0[c] + 16)
                    dc = cc - c + 15
                    out[cls][:, rr * 64 + c, kr * 64 + cc] = rpb[:, dr][:, dc]
    return out


def build(S, stop="end", dbg=()):
    NT = S // 128
    NTT = NT + 2
    TOK = CT + S
    ROWS = S // GW
    nc = bass.Bass("TRN2", target_bir_lowering=False)
    ext = {}

    def ein(name, shape, dt=F32):
        ext[name] = nc.dram_tensor(name, list(shape), dt, kind="ExternalInput")
        return ext[name].ap()

    xin = ein("xin", [TOK, D])
    cst_d = ein("cst", list(CONST_ARR.shape))
    scol_d = ein("scol", [128, 8, 2])
    adaw_d = ein("ada_w", [2, D, 6 * D])
    adab_d = ein("adab_col", [128, 2, 48])
    ng_d = ein("ng_col", [128, 2, 2, 8])
    fing_d = ein("final_g", [D])
    abwin_d = ein("ab_w_in", [D, 4096])
    abwout_d = ein("ab_w_out", [D, D])
    lbl_d = ein("lb_logits", [2, 2, 512])
    ong_d = ein("onorm_g", [128])
    nab_d = ein("na_bias", [5, 8, 128, 896])
    swin_d = ein("ssd_w_in", [D, 6208])
    scw_d = ein("convw_col", [128, 32, 5])
    scb_d = ein("convb_col", [128, 32])
    salog_d = ein("ssd_a_log", [64])
    sdtb_d = ein("ssd_dt_bias", [64])
    sd_d = ein("ssd_d", [32])
    sng_d = ein("ssd_norm_g", [2048])
    swout_d = ein("ssd_w_out", [2048, D])
    rw_d = ein("moe_router", [2, D, 16])
    w1_d = ein("moe_w1", [2, 16, D, D])
    w3_d = ein("moe_w3", [2, 16, D, D])
    w2_d = ein("moe_w2", [2, 16, D, D])
    sel32_d = ein("sel32", [32, 32 * 128])
    ngnat_d = ein("norm_g_nat", [2, 2, D])

    outs = {}

    def dt_(name, shape, dt, ntile=NTT):
        kind = "ExternalOutput" if (name in dbg or name == "out") else "Internal"
        d = DT(nc, name, shape, dt, ntile, kind=kind)
        outs[name] = d
        return d

    out_d = dt_("out", [S, D], F32, NT)
    xres = dt_("xres", [TOK, D], F32)
    qh = dt_("qh", [TOK, 512], BF16)
    vh = dt_("vh", [TOK, 512], BF16)
    gs = dt_("gs", [TOK, 512], BF16)
    kd = [dt_("kf", [TOK, 512], BF16), dt_("kb", [TOK, 512], BF16)]
    gd = [dt_("gf", [TOK, 512], F32), dt_("gb", [TOK, 512], F32)]
    qnT = dt_("qnT", [512, TOK], BF16)
    knT = dt_("knT", [512, TOK], BF16)
    vn = dt_("vn", [TOK, 512], BF16)
    of_ = dt_("of", [TOK, 512], F32)
    mix = dt_("mix", [TOK, D], BF16)
    fTd = dt_("fTd", [D, TOK], BF16)
    aff = dt_("aff", [TOK, 16], F32)
    affT = dt_("affT", [16, TOK], F32)
    modc = dt_("modc", [128, 2 * 48 * 2], F32, 1)
    U32 = mybir.dt.uint32
    CAPL = (2 * S) // 16
    CB = CAPL + 256
    NR = CB + 128
    ftok = dt_("ftok", [TOK, D], BF16)
    Xsel = [nc.dram_tensor("Xsel%d" % e, [NR, D], BF16, kind="Internal") for e in range(16)]
    Ysel = [nc.dram_tensor("Ysel%d" % e, [NR, D], BF16, kind="Internal") for e in range(16)]
    uT = dt_("uT", [4096, TOK], BF16)
    zs = dt_("zs", [TOK, 2048], BF16)
    dtd = dt_("dtd", [TOK, 64], F32)
    dtad = dt_("dtad", [TOK, 64], F32)
    bcT = dt_("bcT", [2048, TOK], BF16)
    xB = dt_("xB", [TOK, 3072], BF16)
    yf = dt_("yf", [TOK, 2048], F32)
    if "coef" in dbg:
        dt_("coef", [TOK, 16], F32)
    if "slotsd" in dbg:
        dt_("slotsd", [TOK, 16], mybir.dt.uint32)

    with contextlib.ExitStack() as st:
        k = K(nc, st)
        NCC = CONST_ARR.shape[1]
        cst = k.sb("cst", [128, NCC])
        k.dma(cst[:], cst_d, W=[cst])

        def C(name, lo=0, hi=None):
            o, n = CONST_OFF[name]
            hi = n if hi is None else hi
            return cst[:, o + lo:o + hi]
        identb = k.sb("identb", [128, 128], BF16)
        k.I("dve", lambda g: g.tensor_copy(out=identb[:], in_=C("ident")), R=[cst], W=[identb])
        epsT = k.sb("eps", [128, 1])
        k.I("dve", lambda g: g.memset(epsT[:], EPS), W=[epsT])
        mod = k.sb("mod", [128, 2, 48, 2])
        AB = k.sb("AB", [128, 2, 4, 8, 2])
        g2row = [[None, None], [None, None]]
        g5row = [[None, None], [None, None]]

        with k.scope():
            scol = k.sb("scol", [128, 8, 2])
            k.dma(scol[:], scol_d, W=[scol])
            ssl = k.sb("ssl", [128, 8, 2])
            k.I("act", lambda g: g.activation(out=ssl[:], in_=scol[:], func=AF.Silu), R=[scol], W=[ssl])
            adab = k.sb("adab", [128, 2, 48])
            k.dma(adab[:], adab_d, W=[adab])
            ngc = k.sb("ngc", [128, 2, 2, 8])
            k.dma(ngc[:], ng_d, W=[ngc])
            wbuf = [k.sb("adaw", [128, 6 * D]) for _ in range(2)]
            pp = [k.ps("pp", [128, 512]) for _ in range(2)]
            n = 0
            for l in range(2):
                for j in range(8):
                    wb = wbuf[n % 2]
                    p = pp[n % 2]
                    n += 1
                    k.dma(wb[:], adaw_d[l, j * 128:(j + 1) * 128, :], W=[wb])
                    for fb in range(48):
                        k.mm(p, p[:, 2 * fb:2 * fb + 2], wb, wb[:, fb * 128:(fb + 1) * 128], ssl, ssl[:, j, :])
                    mv = mod[:, l, :, :]
                    pv = p[:, 0:96]
                    if j == 0:
                        k.I("dve", lambda g: g.tensor_copy(out=mv, in_=pv), R=[p], W=[mod])
                    else:
                        k.I("dve", lambda g: g.tensor_tensor(out=mv, in0=mv, in1=pv, op=ALU.add), R=[p, mod], W=[mod])
            for l in range(2):
                for r in range(2):
                    mv = mod[:, l, :, r]
                    k.I("dve", lambda g: g.tensor_tensor(out=mv, in0=mv, in1=adab[:, l, :], op=ALU.add),
                        R=[mod, adab], W=[mod])
            for l in range(2):
                for r in range(2):
                    for wi, (sh, sc) in enumerate([(0, 1), (3, 4)]):
                        Aout = AB[:, l, 2 * wi, :, r]
                        Bout = AB[:, l, 2 * wi + 1, :, r]
                        scv = mod[:, l, sc * 8:(sc + 1) * 8, r]
                        shv = mod[:, l, sh * 8:(sh + 1) * 8, r]
                        gv = ngc[:, l, wi, :]
                        k.I("dve", lambda g: g.scalar_tensor_tensor(out=Aout, in0=scv, scalar=1.0, in1=gv,
                                                                   op0=ALU.add, op1=ALU.mult),
                            R=[mod, ngc], W=[AB])
                        k.I("dve", lambda g: g.tensor_copy(out=Bout, in_=shv), R=[mod], W=[AB])
            if "modc" in dbg:
                k.dma(modc.ap, mod[:], R=[mod], W=modc.all)

        def gate_rows(l, chunk, r, dst):
            with k.scope():
                tp = k.ps("gtp", [128, 128])
                mT = k.sb("mT", [128, 128])
                bc = k.ps("gbc", [128, 512])
                src = k.sb("gsrc", [128, 8])
                k.I("dve", lambda g: g.tensor_copy(out=src[:], in_=mod[:, l, chunk * 8:(chunk + 1) * 8, r]), R=[mod], W=[src])
                k.tr(tp, tp[0:8, :], src, src[:], cst, C("ident"))
                k.I("act", lambda g: g.activation(out=mT[0:8, :], in_=tp[0:8, :], func=AF.Copy), R=[tp], W=[mT])
                for hf in range(2):
                    for q in range(4):
                        fb = hf * 4 + q
                        selb = k.sb("selb", [128, 128])
                        k.I("dve", lambda g: g.tensor_scalar(out=selb[0:8, :], in0=C("ones")[0:8, :],
                                                             scalar1=C("ident")[0:8, fb:fb + 1], scalar2=None,
                                                             op0=ALU.mult), R=[cst], W=[selb])
                        k.mm(bc, bc[:, q * 128:(q + 1) * 128], selb, selb[0:8, :], mT, mT[0:8, :])
                    k.I("act", lambda g: g.activation(out=dst[:, hf * 512:(hf + 1) * 512], in_=bc[:], func=AF.Copy),
                        R=[bc], W=[dst])

        def norm_tile(xt, l, wi, r, hT, col0, sc, outdt_ps, ident, identb_):
            sq, ss, xn, tps = sc
            k.I("act", lambda g: g.activation(out=sq[:], in_=xt[:], func=AF.Square, accum_out=ss[:]), R=[xt], W=[sq, ss])
            k.I("act", lambda g: g.activation(out=ss[:], in_=ss[:], func=AF.Sqrt, bias=epsT[:], scale=1.0 / D),
                R=[ss, epsT], W=[ss])
            k.I("dve", lambda g: g.reciprocal(out=ss[:], in_=ss[:]), R=[ss], W=[ss])
            k.I("dve", lambda g: g.tensor_scalar(out=xn[:], in0=xt[:], scalar1=ss[:], scalar2=None, op0=ALU.mult),
                R=[xt, ss], W=[xn])
            for j in range(8):
                k.tr(tps, tps[:, j * 128:(j + 1) * 128], xn, xn[:, j * 128:(j + 1) * 128], ident, identb_)
            for j in range(8):
                k.I("act", lambda g: g.activation(out=hT[:, j, col0:col0 + 128], in_=tps[:, j * 128:(j + 1) * 128],
                                                  func=AF.Identity, scale=AB[:, l, 2 * wi, j, r:r + 1],
                                                  bias=AB[:, l, 2 * wi + 1, j, r:r + 1]),
                    R=[tps, AB], W=[hT])

        def groups():
            yield 0, 2, 1
            for t in range(2, NTT, 4):
                yield t, min(4, NTT - t), 0

        def load_w_bf16(dst, src_ap, rows_chunks, cols, eng="pool", piece=2048):
            stg = [k.sb("wstg", [128, piece]) for _ in range(2)]
            n = 0
            for j in range(rows_chunks):
                for c0 in range(0, cols, piece):
                    c1 = min(cols, c0 + piece)
                    sb_ = stg[n % 2]
                    n += 1
                    k.dma(sb_[:, 0:c1 - c0], src_ap[j * 128:(j + 1) * 128, c0:c1], W=[sb_])
                    k.I(eng, lambda g: g.tensor_copy(out=dst[:, j, c0:c1], in_=sb_[:, 0:c1 - c0]), R=[sb_], W=[dst])

        k.mark("prelude")
        if stop == "prelude":
            k.barrier()
            return nc, outs

        with k.scope():
            W = k.sb("abwin", [128, 8, 4096], BF16)
            load_w_bf16(W, abwin_d, 8, 4096)
            lbr = k.sb("lbr", [128, 2, 2, 512])
            k.dma(lbr[:], lbl_d.partition_broadcast(128), W=[lbr])
            lb = k.sb("lb", [128, 2, 512])
            oml = k.sb("oml", [128, 2, 512])
            k.I("dve", lambda g: g.tensor_tensor(out=lb[:], in0=lbr[:, :, 0, :], in1=lbr[:, :, 1, :], op=ALU.subtract),
                R=[lbr], W=[lb])
            k.I("act", lambda g: g.activation(out=lb[:], in_=lb[:], func=AF.Sigmoid), R=[lb], W=[lb])
            k.I("dve", lambda g: g.tensor_scalar(out=oml[:], in0=lb[:], scalar1=-1.0, scalar2=1.0, op0=ALU.mult, op1=ALU.add),
                R=[lb], W=[oml])
            xt_ = [k.sb("xt", [128, D]) for _ in range(2)]
            sq = k.sb("sq", [128, D])
            ss_ = [k.sb("ss", [128, 1]) for _ in range(2)]
            xn_ = [k.sb("xn", [128, D], BF16) for _ in range(2)]
            tps_ = [k.ps("tps", [128, D], BF16) for _ in range(2)]
            hT_ = [k.sb("hT", [128, 8, 512], BF16) for _ in range(2)]
            pj = [k.ps("pj", [128, 512]) for _ in range(4)]
            ob16 = [k.sb("ob16", [128, 512], BF16) for _ in range(4)]
            o32 = [k.sb("o32", [128, 512]) for _ in range(2)]
            sg_ = [k.sb("sg", [128, 512]) for _ in range(2)]
            fT2 = [k.sb("fT2", [128, 512], BF16) for _ in range(2)]
            cnt = {"x": 0, "pj": 0, "ob": 0, "o32": 0, "ft": 0}

            def nxt(lst, key):
                v = lst[cnt[key] % len(lst)]
                cnt[key] += 1
                return v
            for gi, (t0, ng, r) in enumerate(groups()):
                hT = hT_[gi % 2]
                T = ng * 128
                for ti in range(ng):
                    t = t0 + ti
                    xt = nxt(xt_, "x")
                    i2 = cnt["x"] % 2
                    k.dma(xt[:], xin[t * 128:(t + 1) * 128, :], W=[xt])
                    norm_tile(xt, 0, 0, r, hT, ti * 128, (sq, ss_[i2], xn_[i2], tps_[i2]), None, identb, identb[:])
                for fb in range(8):
                    p = nxt(pj, "pj")
                    c0 = 2560 + fb * 128
                    for j in range(8):
                        k.mm(p, p[:, 0:T], W, W[:, j, c0:c0 + 128], hT, hT[:, j, 0:T], start=(j == 0), stop=(j == 7))
                    o = nxt(fT2, "ft")
                    k.I("act", lambda g: g.activation(out=o[:, 0:T], in_=p[:, 0:T], func=AF.Copy), R=[p], W=[o])
                    dst = qnT if fb < 4 else knT
                    rr = (fb % 4) * 128
                    k.dma(dst.ap[rr:rr + 128, t0 * 128:t0 * 128 + T], o[:, 0:T], R=[o], W=dst.b[t0:t0 + ng])
                for ti in range(ng):
                    t = t0 + ti
                    rows = slice(t * 128, (t + 1) * 128)
                    for cb in (0, 1, 2, 3, 4, 7):
                        p = nxt(pj, "pj")
                        for j in range(8):
                            k.mm(p, p[:], hT, hT[:, j, ti * 128:(ti + 1) * 128], W, W[:, j, cb * 512:(cb + 1) * 512],
                                 start=(j == 0), stop=(j == 7))
                        if cb in (0, 4):
                            o = nxt(ob16, "ob")
                            k.I("act", lambda g: g.activation(out=o[:], in_=p[:], func=AF.Silu), R=[p], W=[o])
                            dst = qh if cb == 0 else gs
                            k.dma(dst.ap[rows, :], o[:], R=[o], W=[dst.b[t]])
                        elif cb in (3, 7):
                            o = nxt(ob16, "ob")
                            k.I("act", lambda g: g.activation(out=o[:], in_=p[:], func=AF.Copy), R=[p], W=[o])
                            dst = vh if cb == 3 else vn
                            k.dma(dst.ap[rows, :], o[:], R=[o], W=[dst.b[t]])
                        else:
                            d = cb - 1
                            s_ = sg_[d]
                            k.I("act", lambda g: g.activation(out=s_[:], in_=p[:], func=AF.Sigmoid), R=[p], W=[s_])
                            k.I("dve", lambda g: g.tensor_tensor(out=s_[:], in0=s_[:], in1=oml[:, d, :], op=ALU.mult),
                                R=[s_, oml], W=[s_])
                            k.I("dve", lambda g: g.tensor_tensor(out=s_[:], in0=s_[:], in1=lb[:, d, :], op=ALU.add),
                                R=[s_, lb], W=[s_])
                            ko = nxt(ob16, "ob")
                            k.I("dve", lambda g: g.tensor_scalar(out=ko[:], in0=s_[:], scalar1=-1.0, scalar2=1.0,
                                                                 op0=ALU.mult, op1=ALU.add), R=[s_], W=[ko])
                            k.dma(kd[d].ap[rows, :], ko[:], R=[ko], W=[kd[d].b[t]])
                            go = nxt(o32, "o32")
                            k.I("act", lambda g: g.activation(out=go[:], in_=s_[:], func=AF.Ln), R=[s_], W=[go])
                            k.dma(gd[d].ap[rows, :], go[:], R=[go], W=[gd[d].b[t]])
        k.mark("A")
        if stop == "A":
            k.barrier()
            return nc, outs

        def hgrn_pass(d):
            with k.scope():
                R1 = C("R1f" if d == 0 else "R1b", 0, 130)
                Ust = C("Usf" if d == 0 else "Usb")
                Um = C("Uf" if d == 0 else "Ub")
                Sst = [k.sb("S%d" % h, [128, 128]) for h in range(4)]
                for h in range(4):
                    k.I("pool", lambda g: g.memset(Sst[h][:], 0.0), W=[Sst[h]])
                gain = k.sb("gain", [128, 128])
                k.dma(gain[:], ong_d.partition_broadcast(128), W=[gain])
                gt_ = [k.sb("gt", [128, 512]) for _ in range(2)]
                kt_ = [k.sb("kt", [128, 512], BF16) for _ in range(2)]
                qt_ = [k.sb("qt", [128, 512], BF16) for _ in range(2)]
                vt_ = [k.sb("vt", [128, 512], BF16) for _ in range(2)]
                ot_ = [k.sb("ot", [128, 512]) for _ in range(2)]
                oft_ = [k.sb("oft", [128, 512]) for _ in range(2)]
                gst_ = [k.sb("gst", [128, 512], BF16) for _ in range(2)]
                mixt_ = [k.sb("mixt", [128, 512], BF16) for _ in range(2)]
                psE = [k.ps("psE", [128, 258]) for _ in range(2)]
                psT = [k.ps("psT", [128, 256], BF16) for _ in range(2)]
                psA = k.ps("psA", [128, 128])
                psO = [k.ps("psO", [128, 128]) for _ in range(2)]
                psS = k.ps("psS", [128, 128])
                eq_ = [k.sb("eq", [128, 128]) for _ in range(2)]
                ek_ = [k.sb("ek", [128, 128]) for _ in range(2)]
                e2_ = [k.sb("e2", [128, 128]) for _ in range(2)]
                cmr_ = [k.sb("cmr", [128, 2]) for _ in range(2)]
                ct_ = [k.sb("ct", [128, 1]) for _ in range(2)]
                qin_ = [k.sb("qin", [128, 128], BF16) for _ in range(2)]
                kin_ = [k.sb("kin", [128, 128], BF16) for _ in range(2)]
                kout_ = [k.sb("kout", [128, 128], BF16) for _ in range(2)]
                at_ = [k.sb("at", [128, 128], BF16) for _ in range(2)]
                sm_ = [k.sb("smid", [128, 128], BF16) for _ in range(2)]
                junk = k.sb("junk", [128, 128])
                ssq_ = [k.sb("ssq", [128, 4]) for _ in range(2)]
                tmp_ = [k.sb("tmpo", [128, 512]) for _ in range(2)]
                order = list(range(NTT)) if d == 0 else [1, 0] + list(range(NTT - 1, 1, -1))

                def pre(it, t):
                    b2 = it % 2
                    rows = slice(t * 128, (t + 1) * 128)
                    gt, kt, qt, vt = gt_[b2], kt_[b2], qt_[b2], vt_[b2]
                    k.dma(gt[:], gd[d].ap[rows, :], R=[gd[d].b[t]], W=[gt])
                    k.dma(kt[:], kd[d].ap[rows, :], R=[kd[d].b[t]], W=[kt])
                    k.dma(qt[:], qh.ap[rows, :], R=[qh.b[t]], W=[qt])
                    k.dma(vt[:], vh.ap[rows, :], R=[vh.b[t]], W=[vt])
                    if d == 1:
                        k.dma(oft_[b2][:], of_.ap[rows, :], R=[of_.b[t]], W=[oft_[b2]])
                        k.dma(gst_[b2][:], gs.ap[rows, :], R=[gs.b[t]], W=[gst_[b2]])

                def s1(u, it, t, h):
                    b2, i2 = it % 2, u % 2
                    hs = slice(h * 128, (h + 1) * 128)
                    gt, kt, qt = gt_[b2], kt_[b2], qt_[b2]
                    pE, pT = psE[i2], psT[i2]
                    k.mm(pE, pE[:, 0:130], gt, gt[:, hs], cst, R1)
                    k.mm(pE, pE[:, 130:258], cst, Ust, gt, gt[:, hs])
                    k.tr(pT, pT[:, 0:128], qt, qt[:, hs], identb, identb[:])
                    k.tr(pT, pT[:, 128:256], kt, kt[:, hs], identb, identb[:])

                def s2(u, it, t, h):
                    b2, i2 = it % 2, u % 2
                    rows = slice(t * 128, (t + 1) * 128)
                    hs = slice(h * 128, (h + 1) * 128)
                    kt, vt, ot = kt_[b2], vt_[b2], ot_[b2]
                    pE, pT, pO = psE[i2], psT[i2], psO[i2]
                    eq, ek, e2, cmr, ct = eq_[i2], ek_[i2], e2_[i2], cmr_[i2], ct_[i2]
                    qin, kin, kout, at, smid = qin_[i2], kin_[i2], kout_[i2], at_[i2], sm_[i2]
                    Sh = Sst[h]
                    k.I("act", lambda g: g.activation(out=eq[:], in_=pE[:, 0:128], func=AF.Exp), R=[pE], W=[eq])
                    k.I("act", lambda g: g.activation(out=ek[:], in_=pE[:, 0:128], func=AF.Exp, scale=-1.0), R=[pE], W=[ek])
                    k.I("act", lambda g: g.activation(out=cmr[:], in_=pE[:, 128:130], func=AF.Exp), R=[pE], W=[cmr])
                    k.I("act", lambda g: g.activation(out=e2[:], in_=pE[:, 130:258], func=AF.Exp), R=[pE], W=[e2])
                    k.I("dve", lambda g: g.tensor_tensor(out=qin[:], in0=pT[:, 0:128], in1=eq[:], op=ALU.mult), R=[pT, eq], W=[qin])
                    k.I("dve", lambda g: g.tensor_tensor(out=kin[:], in0=pT[:, 128:256], in1=ek[:], op=ALU.mult), R=[pT, ek], W=[kin])
                    k.I("pool", lambda g: g.tensor_tensor(out=kout[:], in0=kt[:, hs], in1=e2[:], op=ALU.mult), R=[kt, e2], W=[kout])
                    k.I("dve", lambda g: g.tensor_tensor(out=ct[:], in0=cmr[:, 0:1], in1=cmr[:, 1:2], op=ALU.mult), R=[cmr], W=[ct])
                    k.mm(psA, psA[:], kin, kin[:], qin, qin[:])
                    k.I("dve", lambda g: g.tensor_tensor(out=at[:], in0=psA[:], in1=Um, op=ALU.mult), R=[psA, cst], W=[at])
                    k.I("pool", lambda g: g.tensor_scalar(out=smid[:], in0=Sh[:], scalar1=cmr[:, 0:1], scalar2=None, op0=ALU.mult),
                        R=[Sh, cmr], W=[smid])
                    k.mm(pO, pO[:], at, at[:], vt, vt[:, hs], start=True, stop=False)
                    k.mm(pO, pO[:], qin, qin[:], smid, smid[:], start=False, stop=True)
                    k.mm(psS, psS[:], kout, kout[:], vt, vt[:, hs])
                    k.I("dve", lambda g: g.scalar_tensor_tensor(out=Sh[:], in0=Sh[:], scalar=ct[:, 0:1], in1=psS[:],
                                                               op0=ALU.mult, op1=ALU.add), R=[Sh, ct, psS], W=[Sh])
                    if d == 0:
                        k.I("act", lambda g: g.activation(out=ot[:, hs], in_=pO[:], func=AF.Copy), R=[pO], W=[ot])
                    else:
                        oft = oft_[b2]
                        k.I("dve", lambda g: g.tensor_tensor(out=ot[:, hs], in0=pO[:], in1=oft[:, hs], op=ALU.add),
                            R=[pO, oft], W=[ot])
                    if h != 3:
                        return
                    if d == 0:
                        k.dma(of_.ap[rows, :], ot[:], R=[ot], W=[of_.b[t]], q="act")
                    else:
                        ssq, tmp, mixt, gst = ssq_[b2], tmp_[b2], mixt_[b2], gst_[b2]
                        for h_ in range(4):
                            hs_ = slice(h_ * 128, (h_ + 1) * 128)
                            k.I("act", lambda g: g.activation(out=junk[:], in_=ot[:, hs_], func=AF.Square, accum_out=ssq[:, h_:h_ + 1]),
                                R=[ot], W=[junk, ssq])
                        k.I("act", lambda g: g.activation(out=ssq[:], in_=ssq[:], func=AF.Sqrt, bias=epsT[:], scale=1.0 / 128),
                            R=[ssq, epsT], W=[ssq])
                        k.I("dve", lambda g: g.reciprocal(out=ssq[:], in_=ssq[:]), R=[ssq], W=[ssq])
                        for h_ in range(4):
                            hs_ = slice(h_ * 128, (h_ + 1) * 128)
                            k.I("pool", lambda g: g.scalar_tensor_tensor(out=tmp[:, hs_], in0=ot[:, hs_], scalar=ssq[:, h_:h_ + 1], in1=gain[:],
                                                                        op0=ALU.mult, op1=ALU.mult), R=[ot, ssq, gain], W=[tmp]) if False else \
                                k.I("dve", lambda g: g.scalar_tensor_tensor(out=tmp[:, hs_], in0=ot[:, hs_], scalar=ssq[:, h_:h_ + 1], in1=gain[:],
                                                                           op0=ALU.mult, op1=ALU.mult), R=[ot, ssq, gain], W=[tmp])
                        k.I("pool", lambda g: g.tensor_tensor(out=mixt[:], in0=tmp[:], in1=gst[:], op=ALU.mult), R=[tmp, gst], W=[mixt])
                        k.dma(mix.ap[rows, 0:512], mixt[:], R=[mixt], W=[mix.b[t]])
                units = [(it, t, h) for it, t in enumerate(order) for h in range(4)]
                pre(0, order[0])
                s1(0, *units[0])
                for u, (it, t, h) in enumerate(units):
                    if u + 1 < len(units):
                        nit, nt_, nh = units[u + 1]
                        if nh == 0:
                            pre(nit, nt_)
                        s1(u + 1, nit, nt_, nh)
                    s2(u, it, t, h)
        hgrn_pass(0)
        hgrn_pass(1)
        k.mark("B")
        if stop == "B":
            k.barrier()
            return nc, outs

        with k.scope():
            bias = k.sb("nab", [128, 5, 896])
            kT = k.sb("kT", [64, S], BF16)
            qT = k.sb("qT", [64, S], BF16)
            kcT = k.sb("kcT", [64, 256], BF16)
            qcT = k.sb("qcT", [64, 256], BF16)
            vb = k.sb("vb", [128, NT, 64], BF16)
            vcb = k.sb("vcb", [128, 2, 64], BF16)
            ob = k.sb("ob", [128, NTT, 64], BF16)
            s32_ = [k.sb("s32", [128, 896]) for _ in range(2)]
            p16_ = [k.sb("p16", [128, 896], BF16) for _ in range(2)]
            pT_ = [k.sb("pT", [128, 896], BF16) for _ in range(2)]
            nmx_ = [k.sb("nmx", [128, 1]) for _ in range(2)]
            sm_ = [k.sb("sm", [128, 1]) for _ in range(3)]
            psS0 = [k.ps("psS0", [128, 512]) for _ in range(2)]
            psS1 = [k.ps("psS1", [128, 384]) for _ in range(2)]
            psT = [k.ps("psTn", [128, 896], BF16) for _ in range(2)]
            psO = [k.ps("psOn", [128, 64]) for _ in range(2)]
            n = 0
            for h in range(8):
                fs = slice(h * 64, (h + 1) * 64)
                k.dma(bias[:], nab_d[:, h].rearrange("c q n -> q c n"), W=[bias])
                k.dma(kT[:], knT.ap[fs, CT:], R=knT.b[2:], W=[kT])
                k.dma(qT[:], qnT.ap[fs, CT:], R=qnT.b[2:], W=[qT])
                k.dma(kcT[:], knT.ap[fs, 0:CT], R=knT.b[0:2], W=[kcT])
                k.dma(qcT[:], qnT.ap[fs, 0:CT], R=qnT.b[0:2], W=[qcT])
                for c_ in range(0, NT, 16):
                    ce = min(NT, c_ + 16)
                    k.dma(vb[:, c_:ce, :], vn.ap[CT + c_ * 128:CT + ce * 128, fs].rearrange("(c p) d -> p c d", p=128),
                          R=vn.b[2 + c_:2 + ce], W=[vb])
                k.dma(vcb[:], vn.ap[0:CT, fs].rearrange("(c p) d -> p c d", p=128), R=vn.b[0:2], W=[vcb])
                def info(t):
                    if t < 2:
                        return 256, [(vcb, 0), (vcb, 1)], 0, 0
                    i = t - 2
                    cls = 1 if i == 0 else 2 if i == 1 else 3 if i == NT - 2 else 4 if i == NT - 1 else 0
                    c0 = min(max(i - 2, 0), NT - 5)
                    return 896, [(vb, c0 + c) for c in range(5)] + [(vcb, 0), (vcb, 1)], cls, c0

                def n1(t):
                    i2 = t % 2
                    s32, p16, nmx, sm = s32_[i2], p16_[i2], nmx_[i2], sm_[t % 3]
                    p0, p1 = psS0[i2], psS1[i2]
                    NK, vch, cls, c0 = info(t)
                    if t < 2:
                        k.mm(p1, p1[:, 128:384], qcT, qcT[:, t * 128:(t + 1) * 128], kcT, kcT[:])
                        k.I("dve", lambda g: g.tensor_scalar(out=s32[:, 0:256], in0=p1[:, 128:384], scalar1=0.125, scalar2=None,
                                                             op0=ALU.mult), R=[p1], W=[s32])
                    else:
                        i = t - 2
                        ql = qT[:, i * 128:(i + 1) * 128]
                        k.mm(p0, p0[:], qT, ql, kT, kT[:, c0 * 128:c0 * 128 + 512])
                        k.mm(p1, p1[:, 0:128], qT, ql, kT, kT[:, c0 * 128 + 512:c0 * 128 + 640])
                        k.mm(p1, p1[:, 128:384], qT, ql, kcT, kcT[:])
                        k.I("dve", lambda g: g.scalar_tensor_tensor(out=s32[:, 0:512], in0=p0[:], scalar=0.125, in1=bias[:, cls, 0:512],
                                                                   op0=ALU.mult, op1=ALU.add), R=[p0, bias], W=[s32])
                        k.I("dve", lambda g: g.scalar_tensor_tensor(out=s32[:, 512:896], in0=p1[:], scalar=0.125, in1=bias[:, cls, 512:896],
                                                                   op0=ALU.mult, op1=ALU.add), R=[p1, bias], W=[s32])
                    k.I("dve", lambda g: g.tensor_reduce(out=nmx[:], in_=s32[:, 0:NK], axis=AX.X, op=ALU.max, negate=True), R=[s32], W=[nmx])
                    k.I("act", lambda g: g.activation(out=p16[:, 0:NK], in_=s32[:, 0:NK], func=AF.Exp, bias=nmx[:], scale=1.0,
                                                      accum_out=sm[:]), R=[s32, nmx], W=[p16, sm])

                def n2(t):
                    i2 = t % 2
                    p16, pT, pt = p16_[i2], pT_[i2], psT[i2]
                    NK = info(t)[0]
                    for c in range(NK // 128):
                        k.tr(pt, pt[:, c * 128:(c + 1) * 128], p16, p16[:, c * 128:(c + 1) * 128], identb, identb[:])
                    k.I("act", lambda g: g.activation(out=pT[:, 0:NK], in_=pt[:, 0:NK], func=AF.Copy), R=[pt], W=[pT])

                def n3(t):
                    i2 = t % 2
                    pT, sm, po = pT_[i2], sm_[t % 3], psO[i2]
                    NK, vch, cls, c0 = info(t)
                    nch = NK // 128
                    for c, (vbuf, vc) in enumerate(vch):
                        k.mm(po, po[:], pT, pT[:, c * 128:(c + 1) * 128], vbuf, vbuf[:, vc, :], start=(c == 0), stop=(c == nch - 1))
                    k.I("dve", lambda g: g.reciprocal(out=sm[:], in_=sm[:]), R=[sm], W=[sm])
                    k.I("dve", lambda g: g.tensor_scalar(out=ob[:, t, :], in0=po[:], scalar1=sm[:], scalar2=None, op0=ALU.mult),
                        R=[po, sm], W=[ob])
                for step in range(NTT + 2):
                    if step < NTT:
                        n1(step)
                    if 0 <= step - 1 < NTT:
                        n2(step - 1)
                    if 0 <= step - 2 < NTT:
                        n3(step - 2)
                for c_ in range(0, NTT, 16):
                    ce = min(NTT, c_ + 16)
                    k.dma(mix.ap[c_ * 128:ce * 128, 512 + h * 64:512 + (h + 1) * 64].rearrange("(c p) d -> p c d", p=128), ob[:, c_:ce, :],
                          R=[ob], W=mix.b[c_:ce])
        k.mark("C")
        if stop == "C":
            k.barrier()
            return nc, outs

        class XS:
            pass
        XIN = XS()
        XIN.ap = xin
        XIN.b = [Buf("xin%d" % i) for i in range(NTT)]

        def alloc_post(l, with_ctx):
            P = {}
            P["g2"] = [k.sb("g2row", [128, D]) for _ in range(2)]
            gate_rows(l, 2, 0, P["g2"][0])
            if with_ctx:
                gate_rows(l, 2, 1, P["g2"][1])
            P["rw"] = k.sb("rw", [128, 8, 16])
            k.dma(P["rw"][:], rw_d[l].rearrange("(j p) e -> p j e", p=128), W=[P["rw"]])
            P["xt"] = [k.sb("xt", [128, D]) for _ in range(2)]
            P["tmp"] = k.sb("tmpx", [128, D])
            P["sq"] = k.sb("sqx", [128, D])
            P["ss"] = [k.sb("ssx", [128, 1]) for _ in range(2)]
            P["xn"] = [k.sb("xnx", [128, D]) for _ in range(2)]
            P["tps"] = k.ps("tps32", [128, D])
            P["fT32"] = [k.sb("fT32", [128, 8, 128]) for _ in range(2)]
            P["fTb"] = [k.sb("fTb", [128, 8, 128], BF16) for _ in range(2)]
            P["psR"] = k.ps("psR", [128, 16])
            P["psRT"] = k.ps("psRT", [128, 128])
            P["lg"] = [k.sb("lg", [128, 16]) for _ in range(2)]
            P["aT"] = [k.sb("aT", [128, 128]) for _ in range(2)]
            P["nmx"] = [k.sb("nmxr", [128, 1]) for _ in range(2)]
            P["sm"] = [k.sb("smr", [128, 1]) for _ in range(2)]
            P["A2row"] = [k.sb("A2row", [128, D]) for _ in range(2)]
            P["B2row"] = [k.sb("B2row", [128, D]) for _ in range(2)]
            ngr = k.sb("ngr", [128, D])
            k.dma(ngr[:], ngnat_d[l, 1].partition_broadcast(128), W=[ngr])
            for r in ([0, 1] if with_ctx else [0]):
                gate_rows(l, 3, r, P["B2row"][r])
                gate_rows(l, 4, r, P["A2row"][r])
                a2 = P["A2row"][r]
                k.I("dve", lambda g: g.scalar_tensor_tensor(out=a2[:], in0=a2[:], scalar=1.0, in1=ngr[:], op0=ALU.add, op1=ALU.mult),
                    R=[a2, ngr], W=[a2])
            P["ftk"] = [k.sb("ftk", [128, D], BF16) for _ in range(2)]
            P["tmp2"] = k.sb("tmp2x", [128, D])
            P["n"] = 0
            return P

        def pp2(P, l, t, r, psY, xsrc):
            i2 = t % 2
            rows = slice(t * 128, (t + 1) * 128)
            xt, tmp = P["xt"][i2], P["tmp"]
            g2 = P["g2"][r]
            k.dma(xt[:], xsrc.ap[rows, :], R=[xsrc.b[t]], W=[xt])
            for hf in range(2):
                hfs = slice(hf * 512, (hf + 1) * 512)
                k.I("dve", lambda g: g.tensor_tensor(out=tmp[:, hfs], in0=psY[hf][:], in1=g2[:, hfs], op=ALU.mult),
                    R=[psY[hf], g2], W=[tmp])
            k.I("pool", lambda g: g.tensor_tensor(out=xt[:], in0=xt[:], in1=tmp[:], op=ALU.add), R=[xt, tmp], W=[xt])
            k.dma(xres.ap[rows, :], xt[:], R=[xt], W=[xres.b[t]], q="act")
            sq, ss, xn = P["sq"], P["ss"][i2], P["xn"][i2]
            k.I("act", lambda g: g.activation(out=sq[:], in_=xt[:], func=AF.Square, accum_out=ss[:]), R=[xt], W=[sq, ss])
            k.I("act", lambda g: g.activation(out=ss[:], in_=ss[:], func=AF.Sqrt, bias=epsT[:], scale=1.0 / D),
                R=[ss, epsT], W=[ss])
            k.I("dve", lambda g: g.reciprocal(out=ss[:], in_=ss[:]), R=[ss], W=[ss])
            k.I("dve", lambda g: g.tensor_scalar(out=xn[:], in0=xt[:], scalar1=ss[:], scalar2=None, op0=ALU.mult),
                R=[xt, ss], W=[xn])

        def pp3(P, l, t, r):
            i2 = t % 2
            rows = slice(t * 128, (t + 1) * 128)
            xn, tps, fT32 = P["xn"][i2], P["tps"], P["fT32"][i2]
            for j in range(8):
                k.tr(tps, tps[:, j * 128:(j + 1) * 128], xn, xn[:, j * 128:(j + 1) * 128], cst, C("ident"))
            for j in range(8):
                k.I("act", lambda g: g.activation(out=fT32[:, j, :], in_=tps[:, j * 128:(j + 1) * 128],
                                                  func=AF.Identity, scale=AB[:, l, 2, j, r:r + 1],
                                                  bias=AB[:, l, 3, j, r:r + 1]),
                    R=[tps, AB], W=[fT32])
            ftk, tmp2 = P["ftk"][i2], P["tmp2"]
            k.I("pool", lambda g: g.tensor_tensor(out=tmp2[:], in0=xn[:], in1=P["A2row"][r][:], op=ALU.mult), R=[xn, P["A2row"][r]], W=[tmp2])
            k.I("pool", lambda g: g.tensor_tensor(out=ftk[:], in0=tmp2[:], in1=P["B2row"][r][:], op=ALU.add), R=[tmp2, P["B2row"][r]], W=[ftk])
            k.dma(ftok.ap[rows, :], ftk[:], R=[ftk], W=[ftok.b[t]], q="act")

        def pp4(P, l, t):
            i2 = t % 2
            rows = slice(t * 128, (t + 1) * 128)
            fT32 = P["fT32"][i2]
            psR, psRT, rw = P["psR"], P["psRT"], P["rw"]
            lg, aT, nmx, sm = P["lg"][i2], P["aT"][i2], P["nmx"][i2], P["sm"][i2]
            for j in range(8):
                k.mm(psR, psR[:, 0:16], fT32, fT32[:, j, :], rw, rw[:, j, :], start=(j == 0), stop=(j == 7))
            k.I("dve", lambda g: g.tensor_reduce(out=nmx[:], in_=psR[:, 0:16], axis=AX.X, op=ALU.max, negate=True), R=[psR], W=[nmx])
            k.I("act", lambda g: g.activation(out=lg[:], in_=psR[:, 0:16], func=AF.Exp, bias=nmx[:], scale=1.0, accum_out=sm[:]),
                R=[psR, nmx], W=[lg, sm])
            k.I("dve", lambda g: g.reciprocal(out=sm[:], in_=sm[:]), R=[sm], W=[sm])
            k.I("dve", lambda g: g.tensor_scalar(out=lg[:], in0=lg[:], scalar1=sm[:], scalar2=None, op0=ALU.mult), R=[lg, sm], W=[lg])
            k.dma(aff.ap[rows, :], lg[:], R=[lg], W=[aff.b[t]], q="act")
            k.tr(psRT, psRT[0:16, :], lg, lg[:], cst, C("ident"))
            k.I("act", lambda g: g.activation(out=aT[0:16, :], in_=psRT[0:16, :], func=AF.Copy), R=[psRT], W=[aT])
            k.dma(affT.ap[:, rows], aT[0:16, :], R=[aT], W=[affT.b[t]], q="act")

        def run_pipe(tiles, p1, p2, p3, p4):
            n = len(tiles)
            for s_ in range(n + 3):
                if s_ < n:
                    p1(tiles[s_])
                if 0 <= s_ - 1 < n:
                    p2(tiles[s_ - 1])
                if 0 <= s_ - 2 < n:
                    p3(tiles[s_ - 2])
                if 0 <= s_ - 3 < n:
                    p4(tiles[s_ - 3])

        with k.scope():
            Wo = k.sb("wout", [128, 8, D], BF16)
            load_w_bf16(Wo, abwout_d, 8, D)
            P = alloc_post(0, True)
            mt_ = [k.sb("mt", [128, D], BF16) for _ in range(2)]
            mT_ = [k.sb("mTt", [128, 8, 128], BF16) for _ in range(2)]
            psM = k.ps("psM", [128, D], BF16)
            psY = [k.ps("psY", [128, 512]) for _ in range(2)]
            def d1(t):
                mt, mT = mt_[t % 2], mT_[t % 2]
                k.dma(mt[:], mix.ap[t * 128:(t + 1) * 128, :], R=[mix.b[t]], W=[mt])
                for j in range(8):
                    k.tr(psM, psM[:, j * 128:(j + 1) * 128], mt, mt[:, j * 128:(j + 1) * 128], identb, identb[:])
                k.I("act", lambda g: g.activation(out=mT[:], in_=psM[:], func=AF.Copy), R=[psM], W=[mT])

            def d2(t):
                r = 1 if t < 2 else 0
                mT = mT_[t % 2]
                for hf in range(2):
                    for j in range(8):
                        k.mm(psY[hf], psY[hf][:], mT, mT[:, j, :], Wo, Wo[:, j, hf * 512:(hf + 1) * 512], start=(j == 0), stop=(j == 7))
                pp2(P, 0, t, r, psY, XIN)
            run_pipe(list(range(NTT)), d1, d2, lambda t: pp3(P, 0, t, 1 if t < 2 else 0), lambda t: pp4(P, 0, t))
        k.mark("D")
        if stop == "D":
            k.barrier()
            return nc, outs

        def moe_stage(l, with_ctx, final):
            with k.scope():
                affall = k.sb("affall", [128, NTT, 16])
                for c_ in range(0, NTT, 16):
                    ce = min(NTT, c_ + 16)
                    k.dma(affall[:, c_:ce, :], aff.ap[c_ * 128:ce * 128, :].rearrange("(c p) e -> p c e", p=128), R=aff.b[c_:ce], W=[affall])
                coef = k.sb("coef", [128, NTT, 16])
                thrrow = [k.sb("thrrow", [128, 16]) for _ in range(2)]
                streams = [(0, CT, S, (2 * S) // 16)] + ([(1, 0, CT, (2 * CT) // 16)] if with_ctx else [])
                for (r, tok0, ntok, kk) in streams:
                    with k.scope():
                        n8 = ntok // 8
                        A = k.sb("bisA", [128, n8])
                        junk = k.sb("bisJ", [128, n8])
                        for g8 in range(8):
                            k.dma(A[g8 * 16:(g8 + 1) * 16, :], affT.ap[:, tok0 + g8 * n8:tok0 + (g8 + 1) * n8], R=affT.all, W=[A])
                        lo = k.sb("lo", [128, 1])
                        t_ = k.sb("tt", [128, 1])
                        cp = k.sb("cp", [128, 1])
                        m_ = k.sb("mm", [128, 1])
                        psC = k.ps("psC", [128, 16])
                        k.I("dve", lambda g: g.memset(lo[:], 0.0), W=[lo])
                        w = 0.5
                        for it in range(40):
                            wv = w
                            k.I("dve", lambda g: g.tensor_scalar(out=t_[:], in0=lo[:], scalar1=wv, scalar2=None, op0=ALU.add), R=[lo], W=[t_])
                            k.I("dve", lambda g: g.tensor_scalar(out=junk[:], in0=A[:], scalar1=t_[:, 0:1], scalar2=0.0, op0=ALU.is_ge,
                                                                 op1=ALU.add, accum_out=cp[:]), R=[A, t_], W=[junk, cp])
                            k.mm(psC, psC[:, 0:1], cst, C("G16"), cp, cp[:, 0:1])
                            k.I("dve", lambda g: g.tensor_scalar(out=m_[:], in0=psC[:, 0:1], scalar1=float(kk) - 0.5, scalar2=wv,
                                                                 op0=ALU.is_ge, op1=ALU.mult), R=[psC], W=[m_])
                            k.I("dve", lambda g: g.tensor_tensor(out=lo[:], in0=lo[:], in1=m_[:], op=ALU.add), R=[lo, m_], W=[lo])
                            w *= 0.5
                        thrB = k.sb("thrB", [128, 128])
                        k.I("dve", lambda g: g.tensor_scalar(out=thrB[0:16, :], in0=C("ones")[0:16, :], scalar1=lo[0:16, 0:1], scalar2=None,
                                                             op0=ALU.mult), R=[cst, lo], W=[thrB])
                        k.mm(psC, psC[:, 0:16], thrB, thrB[0:16, :], cst, C("ident")[0:16, 0:16])
                        k.I("act", lambda g: g.activation(out=thrrow[r][:], in_=psC[:, 0:16], func=AF.Copy), R=[psC], W=[thrrow[r]])
                for t in range(NTT):
                    r = 1 if t < 2 else 0
                    if r == 1 and not with_ctx:
                        continue
                    k.I("dve", lambda g: g.tensor_tensor(out=coef[:, t, :], in0=affall[:, t, :], in1=thrrow[r][:], op=ALU.is_ge),
                        R=[affall, thrrow[r]], W=[coef])
                    k.I("dve", lambda g: g.tensor_tensor(out=coef[:, t, :], in0=coef[:, t, :], in1=affall[:, t, :], op=ALU.mult),
                        R=[affall, coef], W=[coef])
                if "coef" in dbg:
                    k.dma(outs["coef"].ap.rearrange("(c p) e -> p c e", p=128), coef[:], R=[coef], W=outs["coef"].all)
                g5 = [k.sb("g5row", [128, D]) for _ in range(2)]
                gate_rows(l, 5, 0, g5[0])
                if with_ctx:
                    gate_rows(l, 5, 1, g5[1])
                if final:
                    fgrow = k.sb("fgrow", [128, D])
                    k.dma(fgrow[:], fing_d.partition_broadcast(128), W=[fgrow])
                fT = k.sb("fTm", [128, 8, 1024], BF16)
                acc = k.sb("acc", [128, 8, D])
                W1 = k.sb("W1", [128, 8, D], BF16)
                W3 = k.sb("W3", [128, 8, D], BF16)
                W2 = k.sb("W2", [128, 8, D], BF16)
                hid = k.sb("hid", [128, 8, 1024], BF16)
                stg = [k.sb("mstg", [128, 1, D]) for _ in range(3)]
                s1_ = [k.sb("s1", [128, 512], BF16) for _ in range(2)]
                ps1 = [k.ps("ps1", [128, 512]) for _ in range(2)]
                ps3 = [k.ps("ps3", [128, 512]) for _ in range(2)]
                psy = [k.ps("psy", [128, 512]) for _ in range(2)]
                xt_ = [k.sb("xtm", [128, D]) for _ in range(2)]
                tmpm = k.sb("tmpm", [128, D])
                sqm = k.sb("sqm", [128, D])
                ssm = [k.sb("ssm", [128, 1]) for _ in range(2)]
                cn = {"stg": 0, "h": 0, "y": 0, "x": 0}

                def loadw(dst, src):
                    for jp in range(8):
                        sb_ = stg[cn["stg"] % 3]
                        cn["stg"] += 1
                        k.dma(sb_[:, 0, :], src[jp * 128:(jp + 1) * 128, :], W=[sb_])
                        k.I("pool", lambda g: g.tensor_copy(out=dst[:, jp, :], in_=sb_[:, 0, :]), R=[sb_], W=[dst])
                sgs = ([(0, 2, 1)] if with_ctx else []) + [(t0, min(8, NTT - t0), 0) for t0 in range(2, NTT, 8)]
                for (t0, nt, r) in sgs:
                    T = nt * 128
                    k.dma(fT[:, :, 0:T], fTd.ap[:, t0 * 128:t0 * 128 + T].rearrange("(j p) t -> p j t", p=128),
                          R=fTd.b[t0:t0 + nt], W=[fT])
                    for e in range(16):
                        loadw(W1, w1_d[l, e])
                        loadw(W3, w3_d[l, e])
                        for g0 in range(0, T, 512):
                            Tg = min(512, T - g0)
                            for ffc in range(8):
                                i2 = cn["h"] % 2
                                cn["h"] += 1
                                p1, p3, s1 = ps1[i2], ps3[i2], s1_[i2]
                                for j in range(8):
                                    k.mm(p1, p1[:, 0:Tg], W1, W1[:, j, ffc * 128:(ffc + 1) * 128], fT, fT[:, j, g0:g0 + Tg],
                                         start=(j == 0), stop=(j == 7))
                                for j in range(8):
                                    k.mm(p3, p3[:, 0:Tg], W3, W3[:, j, ffc * 128:(ffc + 1) * 128], fT, fT[:, j, g0:g0 + Tg],
                                         start=(j == 0), stop=(j == 7))
                                k.I("act", lambda g: g.activation(out=s1[:, 0:Tg], in_=p1[:, 0:Tg], func=AF.Silu), R=[p1], W=[s1])
                                k.I("dve", lambda g: g.tensor_tensor(out=hid[:, ffc, g0:g0 + Tg], in0=s1[:, 0:Tg], in1=p3[:, 0:Tg], op=ALU.mult),
                                    R=[s1, p3], W=[hid])
                        loadw(W2, w2_d[l, e])
                        for ti in range(nt):
                            for hf in range(2):
                                py = psy[cn["y"] % 2]
                                cn["y"] += 1
                                hfs = slice(hf * 512, (hf + 1) * 512)
                                for ffc in range(8):
                                    k.mm(py, py[:], hid, hid[:, ffc, ti * 128:(ti + 1) * 128], W2, W2[:, ffc, hfs],
                                         start=(ffc == 0), stop=(ffc == 7))
                                cf = coef[:, t0 + ti, e:e + 1]
                                if e == 0:
                                    k.I("dve", lambda g: g.tensor_scalar(out=acc[:, ti, hfs], in0=py[:], scalar1=cf, scalar2=None, op0=ALU.mult),
                                        R=[py, coef], W=[acc])
                                else:
                                    k.I("dve", lambda g: g.scalar_tensor_tensor(out=acc[:, ti, hfs], in0=py[:], scalar=cf, in1=acc[:, ti, hfs],
                                                                               op0=ALU.mult, op1=ALU.add), R=[py, coef, acc], W=[acc])
                    for ti in range(nt):
                        t = t0 + ti
                        rows = slice(t * 128, (t + 1) * 128)
                        xt = xt_[cn["x"] % 2]
                        ss = ssm[cn["x"] % 2]
                        cn["x"] += 1
                        k.dma(xt[:], xres.ap[rows, :], R=[xres.b[t]], W=[xt])
                        k.I("pool", lambda g: g.tensor_tensor(out=tmpm[:], in0=acc[:, ti, :], in1=g5[r][:], op=ALU.mult), R=[acc, g5[r]], W=[tmpm])
                        k.I("pool", lambda g: g.tensor_tensor(out=xt[:], in0=xt[:], in1=tmpm[:], op=ALU.add), R=[xt, tmpm], W=[xt])
                        if not final:
                            k.dma(xres.ap[rows, :], xt[:], R=[xt], W=[xres.b[t]])
                        else:
                            k.I("act", lambda g: g.activation(out=sqm[:], in_=xt[:], func=AF.Square, accum_out=ss[:]), R=[xt], W=[sqm, ss])
                            k.I("act", lambda g: g.activation(out=ss[:], in_=ss[:], func=AF.Sqrt, bias=epsT[:], scale=1.0 / D), R=[ss, epsT], W=[ss])
                            k.I("dve", lambda g: g.reciprocal(out=ss[:], in_=ss[:]), R=[ss], W=[ss])
                            k.I("dve", lambda g: g.scalar_tensor_tensor(out=tmpm[:], in0=xt[:], scalar=ss[:, 0:1], in1=fgrow[:],
                                                                       op0=ALU.mult, op1=ALU.mult), R=[xt, ss, fgrow], W=[tmpm])
                            k.dma(out_d.ap[(t - 2) * 128:(t - 1) * 128, :], tmpm[:], R=[tmpm], W=[out_d.b[t - 2]])
        BCREG = {}

        def moe_sparse(l, with_ctx, final):
            IOA = bass.IndirectOffsetOnAxis
            if "bc" not in BCREG:
                BCREG["bc"] = nc.gpsimd.to_reg(NR - 1)
            bcr = BCREG["bc"]
            with k.scope():
                affall = k.sb("affall", [128, NTT, 16])
                for c_ in range(0, NTT, 16):
                    ce = min(NTT, c_ + 16)
                    k.dma(affall[:, c_:ce, :], aff.ap[c_ * 128:ce * 128, :].rearrange("(c p) e -> p c e", p=128), R=aff.b[c_:ce], W=[affall])
                coef = k.sb("coef", [128, NTT, 16])
                selm = k.sb("selm", [128, NTT, 16])
                slots = k.sb("slots", [128, NTT, 16], U32)
                k.I("dve", lambda g: g.memset(selm[:], 0.0), W=[selm])
                k.I("dve", lambda g: g.memset(coef[:], 0.0), W=[coef])
                thrrow = [k.sb("thrrow", [128, 16]) for _ in range(2)]
                streams = [(0, CT, S, CAPL)] + ([(1, 0, CT, (2 * CT) // 16)] if with_ctx else [])
                for (r, tok0, ntok, kk) in streams:
                    with k.scope():
                        n8 = ntok // 8
                        A = k.sb("bisA", [128, n8])
                        junk = k.sb("bisJ", [128, n8])
                        for g8 in range(8):
                            k.dma(A[g8 * 16:(g8 + 1) * 16, :], affT.ap[:, tok0 + g8 * n8:tok0 + (g8 + 1) * n8], R=affT.all, W=[A])
                        lo = k.sb("lo", [128, 1])
                        t_ = k.sb("tt", [128, 1])
                        cp = k.sb("cp", [128, 1])
                        m_ = k.sb("mm", [128, 1])
                        psC = k.ps("psC", [128, 16])
                        k.I("dve", lambda g: g.memset(lo[:], 0.0), W=[lo])
                        w = 0.5
                        for it in range(40):
                            wv = w
                            k.I("dve", lambda g: g.tensor_scalar(out=t_[:], in0=lo[:], scalar1=wv, scalar2=None, op0=ALU.add), R=[lo], W=[t_])
                            k.I("dve", lambda g: g.tensor_scalar(out=junk[:], in0=A[:], scalar1=t_[:, 0:1], scalar2=0.0, op0=ALU.is_ge,
                                                                 op1=ALU.add, accum_out=cp[:]), R=[A, t_], W=[junk, cp])
                            k.mm(psC, psC[:, 0:1], cst, C("G16"), cp, cp[:, 0:1])
                            k.I("dve", lambda g: g.tensor_scalar(out=m_[:], in0=psC[:, 0:1], scalar1=float(kk) - 0.5, scalar2=wv,
                                                                 op0=ALU.is_ge, op1=ALU.mult), R=[psC], W=[m_])
                            k.I("dve", lambda g: g.tensor_tensor(out=lo[:], in0=lo[:], in1=m_[:], op=ALU.add), R=[lo, m_], W=[lo])
                            w *= 0.5
                        thrB = k.sb("thrB", [128, 128])
                        k.I("dve", lambda g: g.tensor_scalar(out=thrB[0:16, :], in0=C("ones")[0:16, :], scalar1=lo[0:16, 0:1], scalar2=None,
                                                             op0=ALU.mult), R=[cst, lo], W=[thrB])
                        k.mm(psC, psC[:, 0:16], thrB, thrB[0:16, :], cst, C("ident")[0:16, 0:16])
                        k.I("act", lambda g: g.activation(out=thrrow[r][:], in_=psC[:, 0:16], func=AF.Copy), R=[psC], W=[thrrow[r]])
                tl = [t for t in range(NTT) if (t >= 2 or with_ctx)]
                for t in tl:
                    r = 1 if t < 2 else 0
                    k.I("dve", lambda g: g.tensor_tensor(out=selm[:, t, :], in0=affall[:, t, :], in1=thrrow[r][:], op=ALU.is_ge),
                        R=[affall, thrrow[r]], W=[selm])
                k.I("dve", lambda g: g.tensor_tensor(out=coef[:], in0=selm[:], in1=affall[:], op=ALU.mult), R=[selm, affall], W=[coef])
                with k.scope():
                    rank = k.sb("rank", [128, NTT, 16])
                    cntb = k.sb("cntb", [128, NTT, 16])
                    offs = k.sb("offs", [128, NTT, 16])
                    onesr = k.sb("onesr", [128, NTT])
                    k.I("dve", lambda g: g.memset(onesr[:], 1.0), W=[onesr])
                    k.I("dve", lambda g: g.memset(offs[:], 0.0), W=[offs])
                    psK = [k.ps("psK", [128, 512]) for _ in range(2)]
                    sf = selm[:].rearrange("p t e -> p (t e)")
                    rf = rank[:].rearrange("p t e -> p (t e)")
                    cf_ = cntb[:].rearrange("p t e -> p (t e)")
                    for c0 in range(0, NTT * 16, 512):
                        c1 = min(NTT * 16, c0 + 512)
                        k.mm(psK[0], psK[0][:, 0:c1 - c0], cst, C("Usb"), selm, sf[:, c0:c1])
                        k.I("act", lambda g: g.activation(out=rf[:, c0:c1], in_=psK[0][:, 0:c1 - c0], func=AF.Copy), R=[psK[0]], W=[rank])
                        k.mm(psK[1], psK[1][:, 0:c1 - c0], cst, C("ones"), selm, sf[:, c0:c1])
                        k.I("act", lambda g: g.activation(out=cf_[:, c0:c1], in_=psK[1][:, 0:c1 - c0], func=AF.Copy), R=[psK[1]], W=[cntb])
                    for e in range(16):
                        k.I("dve", lambda g: g.tensor_tensor_scan(out=offs[:, 2:NTT, e], data0=onesr[:, 2:NTT], data1=cntb[:, 2:NTT, e],
                                                                  initial=0.0, op0=ALU.mult, op1=ALU.add), R=[onesr, cntb], W=[offs])
                    k.I("dve", lambda g: g.tensor_tensor(out=offs[:, 2:NTT, :], in0=offs[:, 2:NTT, :], in1=cntb[:, 2:NTT, :], op=ALU.subtract),
                        R=[offs, cntb], W=[offs])
                    if with_ctx:
                        k.I("dve", lambda g: g.memset(offs[:, 0, :], float(CB)), W=[offs])
                        k.I("dve", lambda g: g.tensor_scalar(out=offs[:, 1, :], in0=cntb[:, 0, :], scalar1=float(CB), scalar2=None, op0=ALU.add),
                            R=[cntb], W=[offs])
                    BIG = 1.0e6
                    k.I("dve", lambda g: g.tensor_tensor(out=rank[:], in0=rank[:], in1=offs[:], op=ALU.add), R=[rank, offs], W=[rank])
                    k.I("dve", lambda g: g.tensor_scalar(out=rank[:], in0=rank[:], scalar1=-BIG, scalar2=None, op0=ALU.add), R=[rank], W=[rank])
                    k.I("dve", lambda g: g.tensor_tensor(out=rank[:], in0=rank[:], in1=selm[:], op=ALU.mult), R=[rank, selm], W=[rank])
                    k.I("dve", lambda g: g.tensor_scalar(out=rank[:], in0=rank[:], scalar1=BIG, scalar2=None, op0=ALU.add), R=[rank], W=[rank])
                    k.I("dve", lambda g: g.tensor_copy(out=slots[:], in_=rank[:]), R=[rank], W=[slots])
                    if "slotsd" in dbg:
                        k.dma(outs["slotsd"].ap.rearrange("(c p) e -> p c e", p=128), slots[:], R=[slots], W=outs["slotsd"].all)
                XZ = [Buf("xz%d" % e) for e in range(16)]
                XW = [[] for e in range(16)]
                YW = [Buf("yw%d" % e) for e in range(16)]
                with k.scope():
                    zt = k.sb("zt", [128, 4, D], BF16)
                    k.I("dve", lambda g: g.memset(zt[:], 0.0), W=[zt])
                    for e in range(16):
                        for r0 in range(CAPL, NR, 512):
                            r1 = min(NR, r0 + 512)
                            nq = (r1 - r0) // 128
                            k.dma(Xsel[e].ap()[r0:r1, :].rearrange("(c p) d -> p c d", p=128), zt[:, 0:nq, :], R=[zt, XZ[e]], W=[])
                            XZ[e].r = {}
                    k.barrier()
                with k.scope():
                    W1 = k.sb("W1", [128, 8, D], BF16)
                    W3 = k.sb("W3", [128, 8, D], BF16)
                    W2 = k.sb("W2", [128, 8, D], BF16)
                    stg = [k.sb("mstg", [128, D]) for _ in range(4)]
                    xl_ = [k.sb("xl", [128, D], BF16) for _ in range(3)]
                    XT_ = [k.sb("XT", [128, 8, 512], BF16) for _ in range(2)]
                    hid_ = [k.sb("hid", [128, 8, 512], BF16) for _ in range(2)]
                    s1_ = [k.sb("s1", [128, 512], BF16) for _ in range(2)]
                    yb_ = [k.sb("yb", [128, D], BF16) for _ in range(2)]
                    psX = k.ps("psX", [128, D], BF16)
                    ps1 = [k.ps("ps1", [128, 512]) for _ in range(2)]
                    ps3 = [k.ps("ps3", [128, 512]) for _ in range(2)]
                    psy = [k.ps("psy", [128, 512]) for _ in range(2)]
                    cn = {"stg": 0, "h": 0, "y": 0, "x": 0, "g": 0, "f": 0}
                    ftl = [k.sb("ftl", [128, D], BF16) for _ in range(4)]

                    def scatter(e):
                        seq = []
                        for t in tl:
                            seq.append((t, cn["f"]))
                            cn["f"] += 1

                        def ld(i):
                            t, fi = seq[i]
                            ft = ftl[fi % 4]
                            k.gdma(ft[:], ftok.ap[t * 128:(t + 1) * 128, :], R=[ftok.b[t]], W=[ft])
                        ld(0)
                        if len(seq) > 1:
                            ld(1)
                        for i, (t, fi) in enumerate(seq):
                            if i + 2 < len(seq):
                                ld(i + 2)
                            ft = ftl[fi % 4]
                            wb = Buf("xw")
                            XW[e].append(wb)
                            k.idma(R=[ft, slots], W=[wb], out=Xsel[e].ap(), out_offset=IOA(ap=slots[:, t, e:e + 1], axis=0),
                                   in_=ft[:], in_offset=None, bounds_check=bcr, oob_is_err=False)

                    def loadw(dst, src):
                        for jp in range(8):
                            sb_ = stg[cn["stg"] % 4]
                            cn["stg"] += 1
                            k.dma(sb_[:], src[jp * 128:(jp + 1) * 128, :], W=[sb_])
                            k.I("dve", lambda g: g.tensor_copy(out=dst[:, jp, :], in_=sb_[:]), R=[sb_], W=[dst])
                    ntl = (CAPL + 128) // 128
                    sgroups = [(g0, min(4, ntl - g0)) for g0 in range(0, ntl, 4)] + ([(CB // 128, 1)] if with_ctx else [])
                    scatter(0)
                    for e in range(16):
                        loadw(W1, w1_d[l, e])
                        loadw(W3, w3_d[l, e])
                        loadw(W2, w2_d[l, e])
                        if e + 1 < 16:
                            scatter(e + 1)
                        for (st0, nst) in sgroups:
                            XT = XT_[cn["g"] % 2]
                            hid = hid_[cn["g"] % 2]
                            cn["g"] += 1
                            Tg = nst * 128
                            for q in range(nst):
                                xl = xl_[cn["x"] % 3]
                                cn["x"] += 1
                                r0 = (st0 + q) * 128
                                k.dma(xl[:], Xsel[e].ap()[r0:r0 + 128, :], R=XW[e], W=[xl])
                                for j in range(8):
                                    k.tr(psX, psX[:, j * 128:(j + 1) * 128], xl, xl[:, j * 128:(j + 1) * 128], identb, identb[:])
                                k.I("act", lambda g: g.activation(out=XT[:, :, q * 128:(q + 1) * 128], in_=psX[:].rearrange("p (j c) -> p j c", j=8),
                                                                  func=AF.Copy), R=[psX], W=[XT])
                            for ffc in range(8):
                                i2 = cn["h"] % 2
                                cn["h"] += 1
                                p1, p3, s1 = ps1[i2], ps3[i2], s1_[i2]
                                for j in range(8):
                                    k.mm(p1, p1[:, 0:Tg], W1, W1[:, j, ffc * 128:(ffc + 1) * 128], XT, XT[:, j, 0:Tg], start=(j == 0), stop=(j == 7))
                                for j in range(8):
                                    k.mm(p3, p3[:, 0:Tg], W3, W3[:, j, ffc * 128:(ffc + 1) * 128], XT, XT[:, j, 0:Tg], start=(j == 0), stop=(j == 7))
                                k.I("act", lambda g: g.activation(out=s1[:, 0:Tg], in_=p1[:, 0:Tg], func=AF.Silu), R=[p1], W=[s1])
                                k.I("dve", lambda g: g.tensor_tensor(out=hid[:, ffc, 0:Tg], in0=s1[:, 0:Tg], in1=p3[:, 0:Tg], op=ALU.mult),
                                    R=[s1, p3], W=[hid])
                            for q in range(nst):
                                yb = yb_[cn["y"] % 2]
                                r0 = (st0 + q) * 128
                                for hf in range(2):
                                    py = psy[hf]
                                    hfs = slice(hf * 512, (hf + 1) * 512)
                                    for ffc in range(8):
                                        k.mm(py, py[:], hid, hid[:, ffc, q * 128:(q + 1) * 128], W2, W2[:, ffc, hfs], start=(ffc == 0), stop=(ffc == 7))
                                    if hf == 0:
                                        k.I("act", lambda g: g.activation(out=yb[:, hfs], in_=py[:], func=AF.Copy), R=[py], W=[yb])
                                    else:
                                        k.I("dve", lambda g: g.tensor_copy(out=yb[:, hfs], in_=py[:]), R=[py], W=[yb])
                                cn["y"] += 1
                                k.dma(Ysel[e].ap()[r0:r0 + 128, :], yb[:], R=[yb], W=[], q="act")
                    k.barrier()
                with k.scope():
                    g5 = [k.sb("g5row", [128, D]) for _ in range(2)]
                    gate_rows(l, 5, 0, g5[0])
                    if with_ctx:
                        gate_rows(l, 5, 1, g5[1])
                    if final:
                        fgrow = k.sb("fgrow", [128, D])
                        k.dma(fgrow[:], fing_d.partition_broadcast(128), W=[fgrow])
                    yg_ = [k.sb("yg", [128, D], BF16) for _ in range(6)]
                    for b_ in yg_:
                        k.I("dve", lambda g: g.memset(b_[:], 0.0), W=[b_])
                    dg_ = [k.sb("dgc", [128, 128], BF16) for _ in range(4)]
                    psg = [[k.ps("psg", [128, 512]) for _ in range(2)] for _ in range(2)]
                    xt_ = [k.sb("xtm", [128, D]) for _ in range(2)]
                    tmpm_ = [k.sb("tmpm", [128, D]) for _ in range(2)]
                    sqm = k.sb("sqm", [128, D])
                    ssm = [k.sb("ssm", [128, 1]) for _ in range(2)]
                    n = 0
                    for it, t in enumerate(tl):
                        r = 1 if t < 2 else 0
                        rows = slice(t * 128, (t + 1) * 128)
                        pg, xt, ss, tmpm = psg[it % 2], xt_[it % 2], ssm[it % 2], tmpm_[it % 2]
                        k.dma(xt[:], xres.ap[rows, :], R=[xres.b[t]], W=[xt])
                        for e in range(16):
                            yg = yg_[n % 6]
                            dg = dg_[n % 4]
                            n += 1
                            k.idma(R=[slots], W=[yg], out=yg[:], out_offset=None, in_=Ysel[e].ap(),
                                   in_offset=IOA(ap=slots[:, t, e:e + 1], axis=0), bounds_check=bcr, oob_is_err=False)
                            k.I("dve", lambda g: g.tensor_scalar(out=dg[:], in0=identb[:], scalar1=coef[:, t, e:e + 1], scalar2=None, op0=ALU.mult),
                                R=[identb, coef], W=[dg])
                            for hf in range(2):
                                k.mm(pg[hf], pg[hf][:], dg, dg[:], yg, yg[:, hf * 512:(hf + 1) * 512], start=(e == 0), stop=(e == 15))
                        for hf in range(2):
                            hfs = slice(hf * 512, (hf + 1) * 512)
                            k.I("dve", lambda g: g.tensor_tensor(out=tmpm[:, hfs], in0=pg[hf][:], in1=g5[r][:, hfs], op=ALU.mult), R=[pg[hf], g5[r]], W=[tmpm])
                        k.I("dve", lambda g: g.tensor_tensor(out=xt[:], in0=xt[:], in1=tmpm[:], op=ALU.add), R=[xt, tmpm], W=[xt])
                        if not final:
                            k.dma(xres.ap[rows, :], xt[:], R=[xt], W=[xres.b[t]], q="act")
                        else:
                            k.I("act", lambda g: g.activation(out=sqm[:], in_=xt[:], func=AF.Square, accum_out=ss[:]), R=[xt], W=[sqm, ss])
                            k.I("act", lambda g: g.activation(out=ss[:], in_=ss[:], func=AF.Sqrt, bias=epsT[:], scale=1.0 / D), R=[ss, epsT], W=[ss])
                            k.I("dve", lambda g: g.reciprocal(out=ss[:], in_=ss[:]), R=[ss], W=[ss])
                            k.I("dve", lambda g: g.scalar_tensor_tensor(out=tmpm[:], in0=xt[:], scalar=ss[:, 0:1], in1=fgrow[:],
                                                                       op0=ALU.mult, op1=ALU.mult), R=[xt, ss, fgrow], W=[tmpm])
                            k.dma(out_d.ap[(t - 2) * 128:(t - 1) * 128, :], tmpm[:], R=[tmpm], W=[out_d.b[t - 2]], q="act")
        moe_stage = moe_sparse
        moe_stage(0, True, stop == "F0final")
        k.mark("F0")
        if stop in ("F", "F0final"):
            k.barrier()
            return nc, outs

        with k.scope():
            W = k.sb("swin", [128, 8, 6208], BF16)
            load_w_bf16(W, swin_d, 8, 6208)
            dtb = k.sb("dtb", [128, 64])
            k.dma(dtb[:], sdtb_d.partition_broadcast(128), W=[dtb])
            arow = k.sb("arow", [128, 64])
            k.dma(arow[:], salog_d.partition_broadcast(128), W=[arow])
            k.I("act", lambda g: g.activation(out=arow[:], in_=arow[:], func=AF.Exp), R=[arow], W=[arow])
            k.I("dve", lambda g: g.tensor_scalar(out=arow[:], in0=arow[:], scalar1=-1.0, scalar2=None, op0=ALU.mult), R=[arow], W=[arow])
            xt_ = [k.sb("xt", [128, D]) for _ in range(2)]
            sq = k.sb("sq", [128, D])
            ss_ = [k.sb("ss", [128, 1]) for _ in range(2)]
            xn_ = [k.sb("xn", [128, D], BF16) for _ in range(2)]
            tps_ = [k.ps("tps", [128, D], BF16) for _ in range(2)]
            hT_ = [k.sb("hT", [128, 8, 512], BF16) for _ in range(2)]
            pj = [k.ps("pj", [128, 512]) for _ in range(4)]
            ob16 = [k.sb("ob16", [128, 512], BF16) for _ in range(4)]
            dts_ = [k.sb("dts", [128, 64]) for _ in range(2)]
            dta_ = [k.sb("dtas", [128, 64]) for _ in range(2)]
            cnt = {"x": 0, "pj": 0, "ob": 0}

            def nxt(lst, key):
                v = lst[cnt[key] % len(lst)]
                cnt[key] += 1
                return v
            for gi, (t0, ng, r) in enumerate(groups()):
                hT = hT_[gi % 2]
                T = ng * 128
                for ti in range(ng):
                    t = t0 + ti
                    xt = nxt(xt_, "x")
                    i2 = cnt["x"] % 2
                    k.dma(xt[:], xres.ap[t * 128:(t + 1) * 128, :], R=[xres.b[t]], W=[xt])
                    norm_tile(xt, 1, 0, r, hT, ti * 128, (sq, ss_[i2], xn_[i2], tps_[i2]), None, identb, identb[:])
                for cc in range(32):
                    p = nxt(pj, "pj")
                    c0 = 2048 + cc * 128
                    for j in range(8):
                        k.mm(p, p[:, 0:T], W, W[:, j, c0:c0 + 128], hT, hT[:, j, 0:T], start=(j == 0), stop=(j == 7))
                    o = nxt(ob16, "ob")
                    if cc % 2 == 0:
                        k.I("act", lambda g: g.activation(out=o[:, 0:T], in_=p[:, 0:T], func=AF.Copy), R=[p], W=[o])
                    else:
                        k.I("dve", lambda g: g.tensor_copy(out=o[:, 0:T], in_=p[:, 0:T]), R=[p], W=[o])
                    k.dma(uT.ap[cc * 128:(cc + 1) * 128, t0 * 128:t0 * 128 + T], o[:, 0:T], R=[o], W=uT.b[t0:t0 + ng])
                for ti in range(ng):
                    t = t0 + ti
                    rows = slice(t * 128, (t + 1) * 128)
                    if r == 0:
                        for cb in range(4):
                            p = nxt(pj, "pj")
                            for j in range(8):
                                k.mm(p, p[:], hT, hT[:, j, ti * 128:(ti + 1) * 128], W, W[:, j, cb * 512:(cb + 1) * 512],
                                     start=(j == 0), stop=(j == 7))
                            o = nxt(ob16, "ob")
                            k.I("act", lambda g: g.activation(out=o[:], in_=p[:], func=AF.Silu), R=[p], W=[o])
                            k.dma(zs.ap[rows, cb * 512:(cb + 1) * 512], o[:], R=[o], W=[zs.b[t]])
                    p = nxt(pj, "pj")
                    for j in range(8):
                        k.mm(p, p[:, 0:64], hT, hT[:, j, ti * 128:(ti + 1) * 128], W, W[:, j, 6144:6208], start=(j == 0), stop=(j == 7))
                    dts, dtas = dts_[t % 2], dta_[t % 2]
                    k.I("dve", lambda g: g.tensor_tensor(out=dts[:], in0=p[:, 0:64], in1=dtb[:], op=ALU.add), R=[p, dtb], W=[dts])
                    k.I("act", lambda g: g.activation(out=dts[:], in_=dts[:], func=AF.Exp), R=[dts], W=[dts])
                    k.I("act", lambda g: g.activation(out=dts[:], in_=dts[:], func=AF.Ln, bias=C("ones")[:, 0:1], scale=1.0), R=[dts, cst], W=[dts])
                    k.I("dve", lambda g: g.tensor_tensor(out=dtas[:], in0=dts[:], in1=arow[:], op=ALU.mult), R=[dts, arow], W=[dtas])
                    k.dma(dtd.ap[rows, :], dts[:], R=[dts], W=[dtd.b[t]])
                    k.dma(dtad.ap[rows, :], dtas[:], R=[dtas], W=[dtad.b[t]])
        k.mark("G")
        if stop == "G":
            k.barrier()
            return nc, outs

        with k.scope():
            cw = k.sb("cw", [128, 32, 5])
            k.dma(cw[:], scw_d, W=[cw])
            cbv = k.sb("cbv", [128, 32])
            k.dma(cbv[:], scb_d, W=[cbv])
            Dg = [k.sb("Dg", [128, 5, 128], BF16) for _ in range(2)]
            ub = [k.sb("ub", [128, 516], BF16) for _ in range(2)]
            yb16 = [k.sb("yb16", [128, 512], BF16) for _ in range(2)]
            tm = [k.sb("tm", [128, 4, 128], BF16) for _ in range(2)]
            psc = [k.ps("psc", [128, 512]) for _ in range(2)]
            pst = [k.ps("pst", [128, 512], BF16) for _ in range(2)]
            its = [(cc, tok0, ntok, g0) for cc in range(32) for (tok0, ntok) in ((0, CT), (CT, S)) for g0 in range(0, ntok, 512)]

            def hload(i):
                cc, tok0, ntok, g0 = its[i]
                u = ub[i % 2]
                Tg = min(512, ntok - g0)
                lo, hi = g0 - 2, g0 + Tg + 2
                clo, chi = max(lo, 0), min(hi, ntok)
                if clo != lo or chi != hi:
                    k.I("pool", lambda g: g.memset(u[:], 0.0), W=[u])
                tl0, tl1 = (tok0 + clo) // 128, (tok0 + chi - 1) // 128 + 1
                k.dma(u[:, clo - lo:chi - lo], uT.ap[cc * 128:(cc + 1) * 128, tok0 + clo:tok0 + chi], R=uT.b[tl0:tl1], W=[u])
            hload(0)
            for i, (cc, tok0, ntok, g0) in enumerate(its):
                Dt = Dg[cc % 2]
                if i == 0 or its[i - 1][0] != cc:
                    for tap in range(5):
                        k.I("dve", lambda g: g.tensor_scalar(out=Dt[:, tap, :], in0=identb[:], scalar1=cw[:, cc, tap:tap + 1], scalar2=None,
                                                             op0=ALU.mult), R=[identb, cw], W=[Dt])
                Tg = min(512, ntok - g0)
                i2 = i % 2
                u, y, tmb, p, pt = ub[i2], yb16[i2], tm[i2], psc[i2], pst[i2]
                for tap in range(5):
                    k.mm(p, p[:, 0:Tg], Dt, Dt[:, tap, :], u, u[:, tap:tap + Tg], start=(tap == 0), stop=(tap == 4))
                k.I("act", lambda g: g.activation(out=y[:, 0:Tg], in_=p[:, 0:Tg], func=AF.Silu, bias=cbv[:, cc:cc + 1], scale=1.0),
                    R=[p, cbv], W=[y])
                tb0, nb = (tok0 + g0) // 128, Tg // 128
                if cc < 24:
                    for q in range(nb):
                        k.tr(pt, pt[:, q * 128:(q + 1) * 128], y, y[:, q * 128:(q + 1) * 128], identb, identb[:])
                    k.I("dve", lambda g: g.tensor_copy(out=tmb[:, 0:nb, :], in_=pt[:, 0:nb * 128].rearrange("p (q c) -> p q c", q=nb)),
                        R=[pt], W=[tmb])
                if i + 1 < len(its):
                    hload(i + 1)
                if cc >= 16:
                    k.dma(bcT.ap[(cc - 16) * 128:(cc - 15) * 128, tok0 + g0:tok0 + g0 + Tg], y[:, 0:Tg], R=[y], W=bcT.b[tb0:tb0 + nb], q="act")
                if cc < 24:
                    k.dma(xB.ap[tok0 + g0:tok0 + g0 + Tg, cc * 128:(cc + 1) * 128].rearrange("(q p) c -> p q c", p=128),
                          tmb[:, 0:nb, :], R=[tmb], W=xB.b[tb0:tb0 + nb])
        k.mark("H")
        if stop == "H":
            k.barrier()
            return nc, outs

        def ssd_pass(d):
            with k.scope():
                Uinc = C("Uf") if d == 0 else C("Ub")
                ds0 = d * 32
                dsl = slice(ds0, ds0 + 32)
                hst = k.sb("hst", [128, 2048])
                hstb = k.sb("hstb", [128, 2048], BF16)
                k.I("pool", lambda g: g.memset(hst[:], 0.0), W=[hst])
                k.I("pool", lambda g: g.memset(hstb[:], 0.0), W=[hstb])
                maskneg = k.sb("maskneg", [128, 128])
                k.I("dve", lambda g: g.tensor_scalar(out=maskneg[:], in0=Uinc, scalar1=-1.0, scalar2=30000.0, op0=ALU.add, op1=ALU.mult),
                    R=[cst], W=[maskneg])
                dta_ = [k.sb("dta", [128, 64]) for _ in range(2)]
                dtt_ = [k.sb("dtt", [128, 64]) for _ in range(2)]
                xBt_ = [k.sb("xBt", [128, 3072], BF16) for _ in range(2)]
                bct_ = [k.sb("bct", [128, 16, 128], BF16) for _ in range(2)]
                yft_ = [k.sb("yft", [128, 2048]) for _ in range(2)]
                ytl_ = [k.sb("ytl", [128, 2048]) for _ in range(2)]
                psA2 = k.ps("psA2", [128, 64])
                psG = [k.ps("psG", [128, 128]) for _ in range(2)]
                psAr = [k.ps("psAr", [128, 512]) for _ in range(2)]
                psY = [k.ps("psYs", [128, 256]) for _ in range(1)]
                psYo = k.ps("psYo", [128, 256])
                psH = k.ps("psH", [128, 256])
                eac_ = [k.sb("eac", [128, 32]) for _ in range(2)]
                yo_ = [k.sb("yo", [128, 256]) for _ in range(2)]
                nac_ = [k.sb("nac", [128, 32]) for _ in range(2)]
                wend_ = [k.sb("wend", [128, 32]) for _ in range(2)]
                dlall_ = [k.sb("dlall", [128, 32]) for _ in range(2)]
                xd_ = [k.sb("xd", [128, 2048], BF16) for _ in range(2)]
                xw_ = [k.sb("xw", [128, 2048], BF16) for _ in range(2)]
                rhs4_ = [k.sb("rhs4", [128, 4, 128]) for _ in range(2)]
                nacM_ = [k.sb("nacM", [128, 4, 128]) for _ in range(2)]
                t4_ = [k.sb("t4", [128, 4, 128]) for _ in range(2)]
                dec_ = [k.sb("dec4", [128, 4, 128]) for _ in range(2)]
                erow_ = [k.sb("erow4", [128, 4, 128]) for _ in range(2)]
                MT_ = [k.sb("MT4", [128, 4, 128], BF16) for _ in range(2)]
                CsT_ = [k.sb("CsT4", [128, 4, 128], BF16) for _ in range(2)]
                htmp_ = [k.sb("htmp", [128, 256]) for _ in range(2)]
                order = list(range(NTT)) if d == 0 else [1, 0] + list(range(NTT - 1, 1, -1))

                def pre(it, t):
                    b2 = it % 2
                    lat = t >= 2
                    rows = slice(t * 128, (t + 1) * 128)
                    dta, dtt, xBt, bct, yft = dta_[b2], dtt_[b2], xBt_[b2], bct_[b2], yft_[b2]
                    nac, wend, dlall, xd, xw = nac_[b2], wend_[b2], dlall_[b2], xd_[b2], xw_[b2]
                    k.dma(dta[:], dtad.ap[rows, :], R=[dtad.b[t]], W=[dta])
                    k.dma(dtt[:], dtd.ap[rows, :], R=[dtd.b[t]], W=[dtt])
                    k.dma(xBt[:], xB.ap[rows, :], R=[xB.b[t]], W=[xBt])
                    if lat:
                        k.dma(bct[:], bcT.ap[:, rows].rearrange("(c p) t -> p c t", p=128), R=[bcT.b[t]], W=[bct])
                    if d == 1 and lat:
                        k.dma(yft[:], yf.ap[rows, :], R=[yf.b[t]], W=[yft])
                    k.mm(psA2, psA2[:, 0:32], cst, Uinc, dta, dta[:, dsl])
                    k.mm(psA2, psA2[:, 32:64], cst, C("ones"), dta, dta[:, dsl])
                    k.I("act", lambda g: g.activation(out=nac[:], in_=psA2[:, 0:32], func=AF.Copy, scale=-1.0), R=[psA2], W=[nac])
                    k.I("dve", lambda g: g.tensor_tensor(out=wend[:], in0=psA2[:, 32:64], in1=nac[:], op=ALU.add), R=[psA2, nac], W=[wend])
                    k.I("act", lambda g: g.activation(out=wend[:], in_=wend[:], func=AF.Exp), R=[wend], W=[wend])
                    k.I("dve", lambda g: g.tensor_tensor(out=wend[:], in0=wend[:], in1=dtt[:, dsl], op=ALU.mult), R=[wend, dtt], W=[wend])
                    k.I("act", lambda g: g.activation(out=dlall[:], in_=psA2[:, 32:64], func=AF.Exp), R=[psA2], W=[dlall])
                    if lat:
                        eac = eac_[b2]
                        k.I("act", lambda g: g.activation(out=eac[:], in_=psA2[:, 0:32], func=AF.Exp), R=[psA2], W=[eac])
                    x3 = xBt[:, 0:2048].rearrange("p (h e) -> p h e", h=32)
                    if lat:
                        k.I("pool", lambda g: g.tensor_tensor(out=xd[:].rearrange("p (h e) -> p h e", h=32), in0=x3,
                                                              in1=dtt[:, dsl].unsqueeze(2).to_broadcast([128, 32, 64]), op=ALU.mult),
                            R=[xBt, dtt], W=[xd])
                    k.I("dve", lambda g: g.tensor_tensor(out=xw[:].rearrange("p (h e) -> p h e", h=32), in0=x3,
                                                         in1=wend[:].unsqueeze(2).to_broadcast([128, 32, 64]), op=ALU.mult),
                        R=[xBt, wend], W=[xw])

                def s1(u, it, t, gr):
                    if t < 2:
                        return
                    b2, i2 = it % 2, u % 2
                    dta, bct, nac = dta_[b2], bct_[b2], nac_[b2]
                    pG, pAr, rhs4, nacM = psG[i2], psAr[i2], rhs4_[i2], nacM_[i2]
                    hs4 = slice(gr * 4, gr * 4 + 4)
                    k.mm(pG, pG[:], bct, bct[:, gr, :], bct, bct[:, 8 + gr, :])
                    k.I("dve", lambda g: g.tensor_tensor(out=rhs4[:], in0=Uinc.unsqueeze(1).to_broadcast([128, 4, 128]),
                                                         in1=dta[:, ds0 + gr * 4:ds0 + gr * 4 + 4].unsqueeze(2).to_broadcast([128, 4, 128]),
                                                         op=ALU.mult), R=[cst, dta], W=[rhs4])
                    k.I("pool", lambda g: g.tensor_tensor(out=nacM[:], in0=maskneg[:].unsqueeze(1).to_broadcast([128, 4, 128]),
                                                          in1=nac[:, hs4].unsqueeze(2).to_broadcast([128, 4, 128]), op=ALU.add),
                        R=[maskneg, nac], W=[nacM])
                    k.mm(pAr, pAr[:], cst, C("ones"), rhs4, rhs4[:].rearrange("p a b -> p (a b)"))

                def s2(u, it, t, gr):
                    b2, i2 = it % 2, u % 2
                    lat = t >= 2
                    rows = slice(t * 128, (t + 1) * 128)
                    xBt, bct, yft, ytl = xBt_[b2], bct_[b2], yft_[b2], ytl_[b2]
                    dlall, xd, xw = dlall_[b2], xd_[b2], xw_[b2]
                    gc = slice(gr * 256, (gr + 1) * 256)
                    hs4 = slice(gr * 4, gr * 4 + 4)
                    if lat:
                        pG, pAr, pY = psG[i2], psAr[i2], psY[0]
                        nacM, t4, dec, MT = nacM_[i2], t4_[i2], dec_[i2], MT_[i2]
                        eac, yo = eac_[b2], yo_[i2]
                        pAr3 = pAr[:].rearrange("p (a b) -> p a b", a=4)
                        k.I("dve", lambda g: g.tensor_tensor(out=t4[:], in0=pAr3, in1=nacM[:], op=ALU.add), R=[pAr, nacM], W=[t4])
                        k.I("act", lambda g: g.activation(out=dec[:], in_=t4[:], func=AF.Exp), R=[t4], W=[dec])
                        k.mm(psYo, psYo[:], bct, bct[:, 8 + gr, :], hstb, hstb[:, gc])
                        k.I("dve", lambda g: g.tensor_tensor(out=yo[:].rearrange("p (a b) -> p a b", a=4),
                                                             in0=psYo[:].rearrange("p (a b) -> p a b", a=4),
                                                             in1=eac[:, hs4].unsqueeze(2).to_broadcast([128, 4, 64]), op=ALU.mult),
                            R=[psYo, eac], W=[yo])
                        if d == 1:
                            k.I("pool", lambda g: g.tensor_tensor(out=yo[:], in0=yo[:], in1=yft[:, gc], op=ALU.add), R=[yo, yft], W=[yo])
                        k.I("dve", lambda g: g.tensor_tensor(out=MT[:], in0=dec[:], in1=pG[:].unsqueeze(1).to_broadcast([128, 4, 128]), op=ALU.mult),
                            R=[dec, pG], W=[MT])
                        for hh in range(4):
                            h = gr * 4 + hh
                            k.mm(pY, pY[:, hh * 64:(hh + 1) * 64], MT, MT[:, hh, :], xd, xd[:, h * 64:(h + 1) * 64], start=True, stop=True)
                        k.I("dve", lambda g: g.tensor_tensor(out=ytl[:, gc], in0=pY[:], in1=yo[:], op=ALU.add), R=[pY, yo], W=[ytl])
                    htmp = htmp_[gr % 2]
                    k.mm(psH, psH[:], xBt, xBt[:, 2048 + gr * 128:2048 + (gr + 1) * 128], xw, xw[:, gc])
                    k.I("pool", lambda g: g.tensor_tensor(out=htmp[:].rearrange("p (a b) -> p a b", a=4),
                                                          in0=hst[:, gc].rearrange("p (a b) -> p a b", a=4),
                                                          in1=dlall[:, hs4].unsqueeze(2).to_broadcast([128, 4, 64]), op=ALU.mult),
                        R=[hst, dlall], W=[htmp])
                    k.I("dve", lambda g: g.tensor_tensor(out=hst[:, gc], in0=htmp[:], in1=psH[:], op=ALU.add), R=[htmp, psH], W=[hst])
                    k.I("act", lambda g: g.activation(out=hstb[:, gc], in_=hst[:, gc], func=AF.Copy), R=[hst], W=[hstb])
                    if lat and gr == 7:
                        k.dma(yf.ap[rows, :], ytl[:], R=[ytl], W=[yf.b[t]], q=("act" if d == 0 else "sp"))
                units = [(it, t, gr) for it, t in enumerate(order) for gr in range(8)]
                pre(0, order[0])
                s1(0, *units[0])
                for u, (it, t, gr) in enumerate(units):
                    if gr == 3 and it + 1 < len(order):
                        pre(it + 1, order[it + 1])
                    if u + 1 < len(units):
                        nit, nt_, ngr = units[u + 1]
                        s1(u + 1, nit, nt_, ngr)
                    s2(u, it, t, gr)
        ssd_pass(0)
        ssd_pass(1)
        k.mark("I")
        if stop == "I":
            k.barrier()
            return nc, outs

        with k.scope():
            Wso = k.sb("wso", [128, 16, D], BF16)
            load_w_bf16(Wso, swout_d, 16, D)
            P = alloc_post(1, False)
            dsk = k.sb("dsk", [128, 32])
            k.dma(dsk[:], sd_d.partition_broadcast(128), W=[dsk])
            ngrow = k.sb("ngrow", [128, 2048])
            k.dma(ngrow[:], sng_d.partition_broadcast(128), W=[ngrow])
            yt_ = [k.sb("yt", [128, 2048]) for _ in range(2)]
            xg_ = [k.sb("xg", [128, 2048], BF16) for _ in range(2)]
            zt_ = [k.sb("zt", [128, 2048], BF16) for _ in range(2)]
            tmpk = k.sb("tmpk", [128, 2048])
            junk = k.sb("junkk", [128, 256])
            ssq_ = [k.sb("ssqk", [128, 8]) for _ in range(2)]
            yn_ = [k.sb("yn", [128, 2048], BF16) for _ in range(2)]
            yT_ = [k.sb("yT", [128, 16, 128], BF16) for _ in range(2)]
            psM_ = [k.ps("psM2", [128, D], BF16) for _ in range(2)]
            psY = [k.ps("psY2", [128, 512]) for _ in range(2)]
            def k1(t):
                b2 = t % 2
                rows = slice(t * 128, (t + 1) * 128)
                yt, xg, zt, ssq, yn, yT = yt_[b2], xg_[b2], zt_[b2], ssq_[b2], yn_[b2], yT_[b2]
                k.dma(yt[:], yf.ap[rows, :], R=[yf.b[t]], W=[yt])
                k.dma(xg[:], xB.ap[rows, 0:2048], R=[xB.b[t]], W=[xg])
                k.dma(zt[:], zs.ap[rows, :], R=[zs.b[t]], W=[zt])
                k.I("dve", lambda g: g.tensor_tensor(out=tmpk[:].rearrange("p (h e) -> p h e", h=32),
                                                     in0=xg[:].rearrange("p (h e) -> p h e", h=32),
                                                     in1=dsk[:].unsqueeze(2).to_broadcast([128, 32, 64]), op=ALU.mult), R=[xg, dsk], W=[tmpk])
                k.I("pool", lambda g: g.tensor_tensor(out=yt[:], in0=yt[:], in1=tmpk[:], op=ALU.add), R=[yt, tmpk], W=[yt])
                k.I("pool", lambda g: g.tensor_tensor(out=yt[:], in0=yt[:], in1=zt[:], op=ALU.mult), R=[yt, zt], W=[yt])
                for g8 in range(8):
                    gc = slice(g8 * 256, (g8 + 1) * 256)
                    k.I("act", lambda g: g.activation(out=junk[:], in_=yt[:, gc], func=AF.Square, accum_out=ssq[:, g8:g8 + 1]), R=[yt], W=[junk, ssq])
                k.I("act", lambda g: g.activation(out=ssq[:], in_=ssq[:], func=AF.Sqrt, bias=epsT[:], scale=1.0 / 256), R=[ssq, epsT], W=[ssq])
                k.I("dve", lambda g: g.reciprocal(out=ssq[:], in_=ssq[:]), R=[ssq], W=[ssq])
                for g8 in range(8):
                    gc = slice(g8 * 256, (g8 + 1) * 256)
                    k.I("dve", lambda g: g.scalar_tensor_tensor(out=yn[:, gc], in0=yt[:, gc], scalar=ssq[:, g8:g8 + 1], in1=ngrow[:, gc],
                                                               op0=ALU.mult, op1=ALU.mult), R=[yt, ssq, ngrow], W=[yn])
                for half in range(2):
                    psM = psM_[half]
                    for j in range(8):
                        jj = half * 8 + j
                        k.tr(psM, psM[:, j * 128:(j + 1) * 128], yn, yn[:, jj * 128:(jj + 1) * 128], identb, identb[:])
                    if half == 0:
                        k.I("act", lambda g: g.activation(out=yT[:, 0:8, :], in_=psM[:].rearrange("p (j c) -> p j c", j=8), func=AF.Copy),
                            R=[psM], W=[yT])
                    else:
                        k.I("dve", lambda g: g.tensor_copy(out=yT[:, 8:16, :], in_=psM[:].rearrange("p (j c) -> p j c", j=8)),
                            R=[psM], W=[yT])

            def k2(t):
                yT = yT_[t % 2]
                for hf in range(2):
                    for j in range(16):
                        k.mm(psY[hf], psY[hf][:], yT, yT[:, j, :], Wso, Wso[:, j, hf * 512:(hf + 1) * 512], start=(j == 0), stop=(j == 15))
                pp2(P, 1, t, 0, psY, xres)
            run_pipe(list(range(2, NTT)), k1, k2, lambda t: pp3(P, 1, t, 0), lambda t: pp4(P, 1, t))
        k.mark("K")
        if stop == "K":
            k.barrier()
            return nc, outs
        moe_stage(1, False, True)

        k.barrier()
        k.mark("F1")
        MARKS[:] = k.marks
        print("instr", k.n_instr, "waits", k.n_wait)
    return nc, outs


def prep_inputs(inp, b, S):
    m = {}
    m["xin"] = np.ascontiguousarray(np.concatenate([inp["ctx"][b], inp["x"][b, :S]], 0), np.float32)
    m["cst"] = CONST_ARR
    m["scol"] = np.ascontiguousarray(np.stack([col_layout(inp["c"][b]), col_layout(inp["c_ctx"])], -1))
    m["ada_w"] = np.ascontiguousarray(inp["ada_w"], np.float32)
    m["adab_col"] = np.ascontiguousarray(np.stack([col_layout(inp["ada_b"][l]) for l in range(2)], 1))
    m["ng_col"] = np.ascontiguousarray(
        np.stack([np.stack([col_layout(inp["norm_g"][l, w]) for w in range(2)], 1) for l in range(2)], 1))
    m["final_g"] = np.ascontiguousarray(inp["final_g"], np.float32)
    m["ab_w_in"] = np.ascontiguousarray(inp["ab_w_in"][0], np.float32)
    m["ab_w_out"] = np.ascontiguousarray(inp["ab_w_out"][0], np.float32)
    m["lb_logits"] = np.ascontiguousarray(inp["hgrn_lb_logits"], np.float32)
    m["onorm_g"] = np.ascontiguousarray(inp["hgrn_onorm_g"][0], np.float32)
    m["na_bias"] = na_bias_tables(np.asarray(inp["na_rpb"][0], np.float32), S // GW)
    m["ssd_w_in"] = np.ascontiguousarray(inp["ssd_w_in"][0], np.float32)
    cw = np.asarray(inp["ssd_conv_w"][0], np.float32)
    m["convw_col"] = np.ascontiguousarray(np.stack([col_layout(cw[t]) for t in range(5)], -1))
    m["convb_col"] = col_layout(inp["ssd_conv_b"][0])
    m["ssd_a_log"] = np.ascontiguousarray(inp["ssd_a_log"][0].reshape(64), np.float32)
    m["ssd_dt_bias"] = np.ascontiguousarray(inp["ssd_dt_bias"][0].reshape(64), np.float32)
    m["ssd_d"] = np.ascontiguousarray(inp["ssd_d"][0], np.float32)
    m["ssd_norm_g"] = np.ascontiguousarray(inp["ssd_norm_g"][0], np.float32)
    m["ssd_w_out"] = np.ascontiguousarray(inp["ssd_w_out"][0], np.float32)
    m["moe_router"] = np.ascontiguousarray(inp["moe_router"], np.float32)
    m["moe_w1"] = np.ascontiguousarray(inp["moe_w1"], np.float32)
    m["moe_w3"] = np.ascontiguousarray(inp["moe_w3"], np.float32)
    m["moe_w2"] = np.ascontiguousarray(inp["moe_w2"], np.float32)
    sel = np.zeros((32, 32, 128), np.float32)
    for h in range(32):
        sel[h, h, :] = 1.0
    m["sel32"] = sel.reshape(32, 32 * 128)
    m["norm_g_nat"] = np.ascontiguousarray(inp["norm_g"], np.float32)
    return m


def kernel(**inputs):
    S = inputs["x"].shape[1]
    nc, outs = build(S)
    maps = [prep_inputs(inputs, c % 2, S) for c in range(2)]
    idle = {kk: (v if kk in ("cst", "sel32") else np.zeros_like(v)) for kk, v in maps[0].items()}
    in_maps = [maps[c] if c < 2 else idle for c in range(8)]
    res = run_bass_kernel_spmd(nc, in_maps, core_ids=list(range(8)))
    return np.stack([res.results[0]["out"], res.results[1]["out"]], 0).astype(np.float32)
```
